# Optimizing a Trainium2 kernel written in Bass

```python
import math
import jax, jax.numpy as jnp
from jax import lax
import numpy as np


D_MODEL = 1024
BATCH = 8
SEQ = 2048
DEPTH = 2

CHUNK = 64
N_A_LAYERS = DEPTH // 2
N_B_LAYERS = DEPTH - N_A_LAYERS
CONV_WIDTH = 31
HEAD_DIM = 64
N_HEADS = D_MODEL // (2 * HEAD_DIM)
QK_WIDTH = N_HEADS * 2 * HEAD_DIM
V_WIDTH = N_HEADS * 2 * HEAD_DIM
Q_BLOCK = 128
REL_BUCKETS = 32
REL_MAX_DIST = 128
N_EXPERTS = 32
TOP_K = 4
D_FF = D_MODEL
SWIGLU_LIMIT = 7.0
SWIGLU_ALPHA = 1.702
MOE_BLOCK = 128
LN_EPS = 1e-5
DEEPNORM_ALPHA = (2 * DEPTH) ** 0.25
DEEPNORM_BETA = (8 * DEPTH) ** -0.25

kernel_name = 'yoco_conformer_diffattn_moe_trunk'


def layer_norm(x, g, b):
    xf = x.astype(jnp.float32)
    mu = xf.mean(-1, keepdims=True)
    var = jnp.square(xf - mu).mean(-1, keepdims=True)
    return ((xf - mu) * lax.rsqrt(var + LN_EPS) * g + b).astype(x.dtype)


def modulate(x, shift, scale):
    return x * (1 + scale[:, None, :]) + shift[:, None, :]


def conformer_conv(h, w_pw1, b_pw1, w_dw, b_dw, ln_g, ln_b, w_pw2, b_pw2):
    u = h @ w_pw1 + b_pw1
    a, g = jnp.split(u, 2, axis=-1)
    u = a * jax.nn.sigmoid(g)
    u = lax.conv_general_dilated(
        u, w_dw[:, None, :].astype(u.dtype), window_strides=(1,),
        padding=[(CONV_WIDTH - 1, 0)],
        dimension_numbers=('NWC', 'WIO', 'NWC'),
        feature_group_count=u.shape[-1]) + b_dw
    u = jax.nn.silu(layer_norm(u, ln_g, ln_b))
    return u @ w_pw2 + b_pw2


def t5_bucket(rel):
    nb = REL_BUCKETS // 2
    ret = jnp.where(rel > 0, nb, 0)
    n = jnp.abs(rel)
    max_exact = nb // 2
    large = max_exact + (jnp.log(jnp.maximum(n, 1).astype(jnp.float32) / max_exact)
                         / math.log(REL_MAX_DIST / max_exact) * (nb - max_exact)).astype(jnp.int32)
    large = jnp.minimum(large, nb - 1)
    return ret + jnp.where(n < max_exact, n, large)


def diff_attention(h, k, v, w_q, lam_p, subln_g, w_o, rel_table, lambda_init):
    Bn, Sn, _ = h.shape
    nqb = Sn // Q_BLOCK
    q = (h @ w_q).reshape(Bn, nqb, Q_BLOCK, N_HEADS, 2, HEAD_DIM)
    q = q.transpose(1, 0, 3, 4, 2, 5)
    lp = lam_p.astype(jnp.float32)
    lam = jnp.exp(jnp.sum(lp[0] * lp[1])) - jnp.exp(jnp.sum(lp[2] * lp[3])) + lambda_init
    scale = HEAD_DIM ** -0.5
    kpos = jnp.arange(Sn)

    def block(args):
        qb, i = args
        qpos = i * Q_BLOCK + jnp.arange(Q_BLOCK)
        s = jnp.einsum('bhmqd,bhmkd->bhmqk', qb, k).astype(jnp.float32) * scale
        bias = jnp.take(rel_table, t5_bucket(kpos[None, :] - qpos[:, None]), axis=0)
        s = s + bias.transpose(2, 0, 1).astype(jnp.float32)[None, :, None]
        mask = (kpos[None, :] // CHUNK) <= (qpos[:, None] // CHUNK)
        s = jnp.where(mask, s, -jnp.inf)
        p = jax.nn.softmax(s, axis=-1)
        attn = p[:, :, 0] - lam * p[:, :, 1]
        return jnp.einsum('bhqk,bhke->bqhe', attn.astype(v.dtype), v)

    o = lax.map(block, (q, jnp.arange(nqb)))
    o = o.transpose(1, 0, 2, 3, 4).reshape(Bn, Sn, N_HEADS, 2 * HEAD_DIM)
    of = o.astype(jnp.float32)
    of = of * lax.rsqrt(jnp.mean(jnp.square(of), -1, keepdims=True) + LN_EPS) * subln_g
    of = of * (1.0 - lambda_init)
    return of.astype(h.dtype).reshape(Bn, Sn, N_HEADS * 2 * HEAD_DIM) @ w_o


def clamped_swiglu(gu):
    gate, lin = jnp.split(gu, 2, axis=-1)
    gate = jnp.minimum(gate, SWIGLU_LIMIT)
    lin = jnp.clip(lin, -SWIGLU_LIMIT, SWIGLU_LIMIT)
    return gate * jax.nn.sigmoid(SWIGLU_ALPHA * gate) * (lin + 1)


def moe_ffn(h, w_r, b_r, w_gu, b_gu, w_dn, b_dn):
    Bn, Sn, D = h.shape
    T = Bn * Sn
    hf = h.reshape(T, D)
    logits = (hf @ w_r + b_r).astype(jnp.float32)
    top_logit, top_idx = lax.top_k(logits, TOP_K)
    gates = jax.nn.softmax(top_logit, axis=-1)
    n_assign = T * TOP_K
    flat_e = top_idx.reshape(-1)
    flat_tok = jnp.arange(n_assign, dtype=jnp.int32) // TOP_K
    flat_g = gates.reshape(-1)
    order = jnp.argsort(flat_e)
    sorted_e = flat_e[order]
    counts = jnp.bincount(flat_e, length=N_EXPERTS)
    padded = (counts + MOE_BLOCK - 1) // MOE_BLOCK * MOE_BLOCK
    start = jnp.cumsum(counts) - counts
    pend = jnp.cumsum(padded)
    pstart = pend - padded
    ppos = pstart[sorted_e] + jnp.arange(n_assign) - start[sorted_e]
    n_rows = n_assign + N_EXPERTS * MOE_BLOCK
    n_blocks = n_rows // MOE_BLOCK
    row_tok = jnp.zeros((n_rows,), jnp.int32).at[ppos].set(flat_tok[order])
    row_g = jnp.zeros((n_rows,), h.dtype).at[ppos].set(flat_g[order].astype(h.dtype))
    block_e = jnp.minimum(
        jnp.searchsorted(pend, jnp.arange(n_blocks) * MOE_BLOCK, side='right'), N_EXPERTS - 1)
    xs = hf[row_tok].reshape(n_blocks, MOE_BLOCK, D)

    def expert_block(args):
        xb, e = args
        gu = xb @ w_gu[e] + b_gu[e]
        return clamped_swiglu(gu) @ w_dn[e] + b_dn[e]

    ys = lax.map(expert_block, (xs, block_e)).reshape(n_rows, D) * row_g[:, None]
    out = jnp.zeros((T, D), h.dtype).at[row_tok].add(ys)
    return out.reshape(Bn, Sn, D)


def setup_inputs(seed: int = 0) -> dict:
    key = jax.random.key(seed)
    ks = jax.random.split(key, 32)
    D = D_MODEL
    f32 = jnp.float32

    def nrm(k, shape, s):
        return jax.random.normal(k, shape, f32) * s

    w_k = nrm(ks[14], (D, QK_WIDTH), D ** -0.5)
    w_v = nrm(ks[15], (D, V_WIDTH), D ** -0.5 * DEEPNORM_BETA)
    return {
        'x': nrm(ks[0], (BATCH, SEQ, D), 1.0),
        'c': nrm(ks[1], (BATCH, D), 1.0),
        'ada_w': nrm(ks[2], (DEPTH, D, 6 * D), D ** -0.5),
        'ada_b': nrm(ks[3], (DEPTH, 6 * D), 0.02),
        'post_ln_g': 1.0 + nrm(ks[4], (DEPTH, 2, D), 0.02),
        'post_ln_b': nrm(ks[5], (DEPTH, 2, D), 0.02),
        'conv_w_pw1': nrm(ks[6], (N_A_LAYERS, D, 2 * D), D ** -0.5),
        'conv_b_pw1': nrm(ks[7], (N_A_LAYERS, 2 * D), 0.02),
        'conv_w_dw': nrm(ks[8], (N_A_LAYERS, CONV_WIDTH, D), CONV_WIDTH ** -0.5),
        'conv_b_dw': nrm(ks[9], (N_A_LAYERS, D), 0.02),
        'conv_ln_g': 1.0 + nrm(ks[10], (N_A_LAYERS, D), 0.02),
        'conv_ln_b': nrm(ks[11], (N_A_LAYERS, D), 0.02),
        'conv_w_pw2': nrm(ks[12], (N_A_LAYERS, D, D), D ** -0.5 * DEEPNORM_BETA),
        'conv_b_pw2': nrm(ks[13], (N_A_LAYERS, D), 0.02),
        'w_kv': jnp.concatenate([w_k, w_v], axis=-1),
        'attn_w_q': nrm(ks[16], (N_B_LAYERS, D, QK_WIDTH), D ** -0.5),
        'attn_lambda': nrm(ks[17], (N_B_LAYERS, 4, HEAD_DIM), 0.1),
        'attn_subln_g': 1.0 + nrm(ks[18], (N_B_LAYERS, 2 * HEAD_DIM), 0.02),
        'attn_w_o': nrm(ks[19], (N_B_LAYERS, V_WIDTH, D), V_WIDTH ** -0.5 * DEEPNORM_BETA),
        'rel_bias_table': nrm(ks[20], (REL_BUCKETS, N_HEADS), 0.5),
        'router_w': nrm(ks[21], (DEPTH, D, N_EXPERTS), D ** -0.5),
        'router_b': nrm(ks[22], (DEPTH, N_EXPERTS), 0.01),
        'expert_w_gate_up': nrm(ks[23], (DEPTH, N_EXPERTS, D, 2 * D_FF), D ** -0.5),
        'expert_b_gate_up': nrm(ks[24], (DEPTH, N_EXPERTS, 2 * D_FF), 0.01),
        'expert_w_down': nrm(ks[25], (DEPTH, N_EXPERTS, D_FF, D), D_FF ** -0.5 * DEEPNORM_BETA),
        'expert_b_down': nrm(ks[26], (DEPTH, N_EXPERTS, D), 0.01),
    }


def reference(x, c, ada_w, ada_b, post_ln_g, post_ln_b, conv_w_pw1, conv_b_pw1, conv_w_dw,
              conv_b_dw, conv_ln_g, conv_ln_b, conv_w_pw2, conv_b_pw2, w_kv, attn_w_q,
              attn_lambda, attn_subln_g, attn_w_o, rel_bias_table, router_w, router_b,
              expert_w_gate_up, expert_b_gate_up, expert_w_down, expert_b_down):
    Bn, Sn, _ = x.shape
    cond = jax.nn.silu(c)
    k_sh = None
    v_sh = None
    for l in range(DEPTH):
        mods = cond @ ada_w[l] + ada_b[l]
        sh1, sc1, g1, sh2, sc2, g2 = jnp.split(mods, 6, axis=-1)
        h = modulate(x, sh1, sc1)
        if l < N_A_LAYERS:
            i = l
            y = conformer_conv(h, conv_w_pw1[i], conv_b_pw1[i], conv_w_dw[i], conv_b_dw[i],
                               conv_ln_g[i], conv_ln_b[i], conv_w_pw2[i], conv_b_pw2[i])
        else:
            j = l - N_A_LAYERS
            if j == 0:
                kv = x @ w_kv
                k_sh = kv[..., :QK_WIDTH].reshape(Bn, Sn, N_HEADS, 2, HEAD_DIM).transpose(0, 2, 3, 1, 4)
                v_sh = kv[..., QK_WIDTH:].reshape(Bn, Sn, N_HEADS, 2 * HEAD_DIM).transpose(0, 2, 1, 3)
            lambda_init = 0.8 - 0.6 * math.exp(-0.3 * l)
            y = diff_attention(h, k_sh, v_sh, attn_w_q[j], attn_lambda[j], attn_subln_g[j],
                               attn_w_o[j], rel_bias_table, lambda_init)
        x = layer_norm(DEEPNORM_ALPHA * x + g1[:, None, :] * y, post_ln_g[l, 0], post_ln_b[l, 0])
        h = modulate(x, sh2, sc2)
        y = moe_ffn(h, router_w[l], router_b[l], expert_w_gate_up[l], expert_b_gate_up[l],
                    expert_w_down[l], expert_b_down[l])
        x = layer_norm(DEEPNORM_ALPHA * x + g2[:, None, :] * y, post_ln_g[l, 1], post_ln_b[l, 1])
    return x
```

```python
import math
import numpy as np
import concourse.bass as bass
import concourse.mybir as mybir
from concourse.bass_utils import run_bass_kernel_spmd
from contextlib import ExitStack

F32 = mybir.dt.float32
BF16 = mybir.dt.bfloat16
I32 = mybir.dt.int32
SPARSE = True
AF = mybir.ActivationFunctionType
ALU = mybir.AluOpType
AX = mybir.AxisListType

SAME_ENG_SYNC = True

D = 1024
S = 2048
NE = 32
ALPHA = 4.0 ** 0.25
LN_EPS = 1e-5
BLK = 512
RG = 1152
LAMI = 0.8 - 0.6 * math.exp(-0.3 * 1)
SM_SHIFT = 20.0
NEG = -30000.0
NBLK = S // BLK


class Buf:
    __slots__ = ("name", "w", "r", "rd")

    def __init__(self, name):
        self.name = name
        self.w = None
        self.r = {}
        self.rd = []


class Ins:
    __slots__ = ("eng", "fn", "kind", "cdeps", "dwaits", "sig", "sigidx", "pos", "dsem", "pred", "dord")

    def __init__(self, eng, fn, kind, dsem=None):
        self.pred = None
        self.dord = 0
        self.eng = eng
        self.fn = fn
        self.kind = kind
        self.cdeps = {}
        self.dwaits = {}
        self.sig = False
        self.sigidx = None
        self.pos = None
        self.dsem = dsem


class Prog:
    ENGS = ("pe", "act", "dve", "pool", "sp")

    def __init__(self, nc):
        self.nc = nc
        self.streams = {e: [] for e in self.ENGS}
        self.dma_count = {}
        self.regs = {}

    def _dep(self, ins, p):
        if p is None or p is ins:
            return
        if p.kind == "d":
            s = p.dsem
            ins.dwaits[s] = max(ins.dwaits.get(s, 0), self.dma_count[s])
            return
        if ins.kind == "c" and p.eng == ins.eng:
            if ins.eng == "pe" or not SAME_ENG_SYNC:
                return
        cur = ins.cdeps.get(p.eng)
        if cur is None or cur.pos < p.pos:
            ins.cdeps[p.eng] = p

    def _add(self, ins, reads, writes):
        for b in reads:
            self._dep(ins, b.w)
        for b in writes:
            self._dep(ins, b.w)
            for r in b.r.values():
                self._dep(ins, r)
            for r in b.rd:
                self._dep(ins, r)
        ins.pos = len(self.streams[ins.eng])
        self.streams[ins.eng].append(ins)
        for b in reads:
            if ins.kind == "c":
                b.r[ins.eng] = ins
            else:
                b.rd.append(ins)
        for b in writes:
            b.w = ins
            b.r = {}
            b.rd = []
        return ins

    def op(self, eng, fn, reads=(), writes=(), pred=None):
        ins = Ins(eng, fn, "c")
        ins.pred = pred
        return self._add(ins, reads, writes)

    def dma(self, eng, out, in_, sem, reads=(), writes=(), pred=None, fn=None, **kw):
        self.dma_count.setdefault(sem, 0)
        if fn is None:
            fn = lambda e, out=out, in_=in_, kw=kw: e.dma_start(out=out, in_=in_, **kw)
        ins = Ins(eng, fn, "d", dsem=sem)
        ins.pred = pred
        ins.dord = self.dma_count[sem]
        self._add(ins, reads, writes)
        self.dma_count[sem] += 1
        return ins

    def regload(self, eng, ap, reads=()):
        return self.op(eng, lambda e, ap=ap, eng=eng: e.reg_load(self.regs[eng], ap), reads=reads)

    def final_wait(self, eng, sems):
        ins = Ins(eng, None, "c")
        for s in sems:
            ins.dwaits[s] = self.dma_count[s]
        ins.pos = len(self.streams[eng])
        self.streams[eng].append(ins)

    def emit(self):
        nc = self.nc
        for e in self.ENGS:
            for ins in self.streams[e]:
                for p in ins.cdeps.values():
                    p.sig = True
        for e in self.ENGS:
            n = 0
            for ins in self.streams[e]:
                if ins.sig:
                    n += 1
                    ins.sigidx = n
        with ExitStack() as st:
            csem = {e: st.enter_context(nc.semaphore("c_" + e)) for e in self.ENGS}
            dsem = {s: st.enter_context(nc.semaphore("d_" + s)) for s in self.dma_count}
            block = st.enter_context(nc.Block())

            eobj = {"pe": nc.tensor, "act": nc.scalar, "dve": nc.vector, "pool": nc.gpsimd, "sp": nc.sync}
            for e in self.ENGS:
                self.regs[e] = st.enter_context(eobj[e].register("pr_" + e))

            def run(engname):
                def body(eng):
                    waited = {}

                    def emit_one(ins):
                        for pe_, p in ins.cdeps.items():
                            key = ("c", pe_)
                            if waited.get(key, 0) < p.sigidx:
                                eng.wait_ge(csem[pe_], p.sigidx)
                                waited[key] = p.sigidx
                        for s_, cnt in ins.dwaits.items():
                            key = ("d", s_)
                            if waited.get(key, 0) < cnt:
                                eng.wait_ge(dsem[s_], 16 * cnt)
                                waited[key] = cnt
                        if ins.fn is None:
                            return
                        bi = ins.fn(eng)
                        if ins.kind == "d":
                            bi.then_inc(dsem[ins.dsem], 16)
                        elif ins.sig:
                            bi.then_inc(csem[engname], 1)

                    def body_of(g):
                        for x in g:
                            emit_one(x)

                    def balance(groups):
                        nsig = 0
                        dincs = {}
                        for g in groups:
                            for x in g:
                                if x.kind == "c":
                                    if x.sig:
                                        nsig += 1
                                else:
                                    first, n = dincs.get(x.dsem, (x.dord, 0))
                                    dincs[x.dsem] = (min(first, x.dord), n + 1)
                        if nsig:
                            eng.drain().then_inc(csem[engname], nsig)
                        for s_, (first, n) in dincs.items():
                            if first > 0:
                                eng.wait_ge(dsem[s_], 16 * first)
                            eng.sem_inc(dsem[s_], 16 * n)

                    def emit_seq(groups, lo):
                        i = 0
                        while i < len(groups):
                            g = groups[i]
                            thr = g[0].pred[1]
                            if thr <= lo:
                                body_of(g)
                                i += 1
                                continue
                            k = i
                            while k < len(groups) and groups[k][0].pred[1] >= thr:
                                k += 1
                            run_ = groups[i:k]
                            snap = dict(waited)
                            with eng.If_lt(self.regs[engname], thr + 1):
                                balance(run_)
                            with eng.Else():
                                emit_seq(run_, thr)
                            waited.clear()
                            waited.update(snap)
                            i = k

                    region = []
                    for ins in self.streams[engname]:
                        if ins.pred is None:
                            if region:
                                emit_seq(region, -1)
                                region = []
                            emit_one(ins)
                        else:
                            if region and region[0][0].pred[0] != ins.pred[0]:
                                emit_seq(region, -1)
                                region = []
                            if region and region[-1][0].pred == ins.pred:
                                region[-1].append(ins)
                            else:
                                region.append([ins])
                    if region:
                        emit_seq(region, -1)
                return body

            block.tensor(run("pe"))
            block.scalar(run("act"))
            block.vector(run("dve"))
            block.gpsimd(run("pool"))
            block.sync(run("sp"))


PT = {}
_off = 0


def _pt(name, n):
    global _off
    PT[name] = _off
    _off += n


_pt("ada_b", 96)
_pt("post_g", 32)
_pt("post_b", 32)
_pt("b_pw1", 16)
_pt("w_dw", 248)
_pt("b_dw", 8)
_pt("cln_g", 8)
_pt("cln_b", 8)
_pt("b_pw2", 8)
_pt("b_gu", 1024)
_pt("subln", 8)
NPT = _off


def _cols(v):
    v = np.asarray(v, np.float32).reshape(-1, 128)
    return np.ascontiguousarray(v.T)


def build_pt(inp):
    pt = np.zeros((128, NPT), np.float32)

    def put(name, arr):
        pt[:, PT[name]:PT[name] + arr.shape[1]] = arr

    put("ada_b", np.concatenate([_cols(inp["ada_b"][l]) for l in range(2)], axis=1))
    put("post_g", np.concatenate([_cols(inp["post_ln_g"][l, s]) for l in range(2) for s in range(2)], axis=1))
    put("post_b", np.concatenate([_cols(inp["post_ln_b"][l, s]) for l in range(2) for s in range(2)], axis=1))
    put("b_pw1", _cols(inp["conv_b_pw1"][0]))
    wdw = np.asarray(inp["conv_w_dw"][0], np.float32)
    put("w_dw", np.ascontiguousarray(wdw.reshape(31, 8, 128).transpose(2, 1, 0).reshape(128, 248)))
    put("b_dw", _cols(inp["conv_b_dw"][0]))
    put("cln_g", _cols(inp["conv_ln_g"][0]))
    put("cln_b", _cols(inp["conv_ln_b"][0]))
    put("b_pw2", _cols(inp["conv_b_pw2"][0]))
    put("b_gu", _cols(np.asarray(inp["expert_b_gate_up"], np.float32).reshape(-1)))
    put("subln", np.tile(np.asarray(inp["attn_subln_g"][0], np.float32).reshape(128, 1), (1, 8)))
    return pt


def build(stop_after=None):
    nc = bass.Bass("TRN2", target_bir_lowering=False)

    def din(name, shape, dt=F32):
        return nc.dram_tensor(name, shape, dt, kind="ExternalInput").ap()

    x_d = din("x", [S, D])
    ct_d = din("ct", [128, 8])
    pt_d = din("pt", [128, NPT])
    ident_d = din("ident", [128, 128])
    ada_w_d = din("ada_w", [2, D, 6 * D])
    wpw1_d = din("w_pw1", [D, 2 * D])
    wpw2_d = din("w_pw2", [D, D])
    wr_d = din("w_r", [2, D, NE])
    br_d = din("b_r", [2, 1, NE])
    wgu_d = din("w_gu", [2, NE, D, 2 * D])
    wdn_d = din("w_dn", [2, NE, D, D])
    bdn_d = din("b_dn", [2, NE, D])
    wkv_d = din("w_kv", [D, 2 * D])
    wq_d = din("w_q", [D, D])
    wo_d = din("w_o", [D, D])
    lpt_d = din("lpt", [64, 4])
    tabs_d = din("tabs", [32, 8])
    ohg_d = din("ohg", [32, RG])
    masks_d = din("masks", [4, 128, BLK])
    bgu_d = din("b_gu", [2, NE, 2 * D])
    tri_d = din("tri", [128, 128])
    iota_d = din("iota", [128, NE])
    xs_d = nc.dram_tensor("xs_scratch", [NE * S, D], BF16).ap()
    ys_d = nc.dram_tensor("ys_scratch", [NE * S, D], F32).ap()
    gs_t = nc.dram_tensor("gs_scratch", [8, 128, RG], F32)
    gs_d = gs_t.ap()
    out_d = nc.dram_tensor("out", [S, D], F32, kind="ExternalOutput").ap()

    P = Prog(nc)
    with ExitStack() as st:
        def T(name, shape, dt=F32):
            return st.enter_context(nc.sbuf_tensor("s_" + name, shape, dt))

        region = {}

        def claim(name, new_bufs):
            old = [b for b in region.get(name, []) if b not in new_bufs]
            for nb in new_bufs:
                for ob in old:
                    cands = list(ob.r.values()) + ([ob.w] if ob.w is not None else [])
                    for p in cands:
                        if p.kind == "d":
                            nb.rd.append(p)
                        else:
                            cur = nb.r.get(p.eng)
                            if cur is None or cur.pos < p.pos:
                                nb.r[p.eng] = p
                    nb.rd.extend(ob.rd)
            region[name] = list(new_bufs)

        R_x = T("R_x", [128, 8 * S])
        R_h = T("R_h", [128, 8192])
        R_w = T("R_w", [128, 12288])
        R_a = T("R_a", [128, 2048])
        R_s = T("R_s", [128, 5 * BLK])
        R_m = T("R_m", [128, 6144])
        GT = R_m[0:32, 0:2048]
        gbT = R_m[:, 2048:4096]
        GTb = R_m[0:32, 4096:5120].bitcast(BF16)
        gsel = R_m[0:32, 5120:6144]
        lpT = T("lpT", [64, 4])
        tabs = T("tabs", [32, 8])
        Mall = T("Mall", [128, 16 * NE], BF16)
        posI = T("posI", [128, 64], I32)
        gates4 = T("gates4", [128, 64])
        cntF = T("cntF", [1, NE])
        cntI = T("cntI", [1, NE], I32)
        trib = T("trib", [128, 128], BF16)
        identb = T("identb", [128, 128], BF16)
        iotaT = T("iotaT", [128, NE])
        iotaP = T("iotaP", [128, NE])
        rt = T("rt", [128, 3 * 128 + 16])
        nlam = T("nlam", [128, 4])
        negc = T("negc", [128, 1])
        gsub = T("gsub", [128, 1])
        pt = T("pt", [128, NPT])
        ident = T("ident", [128, 128])
        ones = T("ones", [128, 128])
        onesb = T("onesb", [128, 128], BF16)
        cT = T("cT", [128, 8])
        condb = T("condb", [128, 8], BF16)
        mods = T("mods", [128, 96])
        g1b = T("g1b", [128, 8])
        epsT = T("epsT", [128, 1])
        stage = R_a
        wr_s = T("wr_s", [128, 2 * 8 * NE], BF16)
        br_s = T("br_s", [1, 2 * NE], BF16)
        bdn_s = T("bdn_s", [32, 2 * D], BF16)
        lg = T("lg", [128, 8 * NE])
        mx8 = T("mx8", [128, 16])

        ps = [st.enter_context(nc.psum_tensor(f"ps{i}", [128, 512], F32)) for i in range(8)]
        bps = [Buf(f"ps{i}") for i in range(8)]

        xT = R_x[:].rearrange("p (c t) -> p c t", c=8)
        bxT = [Buf(f"xT{b}") for b in range(NBLK)]
        b_init = Buf("init")
        b_mods = Buf("mods")

        def blk(tb):
            return slice(tb * BLK, (tb + 1) * BLK)

        P.dma("sp", pt[:], pt_d[:, :], "init", writes=[b_init])
        P.dma("sp", ident[:], ident_d[:, :], "init", writes=[b_init])
        P.dma("sp", cT[:], ct_d[:, :], "init", writes=[b_init])
        P.dma("pool", wr_s[:].rearrange("p (l k e) -> p l k e", l=2, k=8), wr_d.rearrange("l (k p) e -> p l k e", p=128), "initp", writes=[b_init])
        P.dma("pool", br_s[:], br_d.rearrange("l o e -> o (l e)"), "initp", writes=[b_init])
        P.dma("pool", bdn_s[:].rearrange("e (l d) -> e l d", l=2), bdn_d.rearrange("l e d -> e l d"), "initp", writes=[b_init])
        b_c = Buf("consts")
        P.op("dve", lambda e: e.memset(ones[:], 1.0), writes=[b_c])
        P.op("dve", lambda e: e.memset(onesb[:], 1.0), writes=[b_c])
        P.op("dve", lambda e: e.memset(epsT[:], LN_EPS), writes=[b_c])
        P.op("dve", lambda e: e.memset(negc[:], -SM_SHIFT), writes=[b_c])
        P.dma("sp", lpT[:], lpt_d[:, :], "init", writes=[b_init])
        P.dma("sp", tabs[:], tabs_d[:, :], "init", writes=[b_init])
        P.dma("pool", trib[:], tri_d[:, :], "initp", writes=[b_init])
        P.dma("sp", iotaT[:], iota_d[:, :], "init", writes=[b_init])
        blin = pt[:, PT["b_gu"]:PT["b_gu"] + 1024].rearrange("p (g j) -> p g j", j=16)[:, :, 8:16]
        P.op("dve", lambda e: e.tensor_scalar_add(out=blin, in0=blin, scalar1=1.0), reads=[b_init], writes=[b_init])
        P.op("act", lambda e: e.activation(out=condb[:], in_=cT[:], func=AF.Silu), reads=[b_init], writes=[b_c])
        P.op("act", lambda e: e.activation(out=identb[:], in_=ident[:], func=AF.Copy), reads=[b_init], writes=[b_c])
        P.op("act", lambda e: e.activation(out=iotaP[:], in_=iotaT[:], func=AF.Identity, bias=1000.0, scale=1.0), reads=[b_init], writes=[b_c])

        units = []
        for u in range(2):
            base = u * 6144
            units.append(dict(
                raw=R_w[:, base:base + 6144],
                buf=Buf(f"unit{u}"), sem=f"wu{u}"))
        ucount = [0]
        claim("Rw", [u["buf"] for u in units])

        def next_unit():
            u = units[ucount[0] % 2]
            ucount[0] += 1
            return u

        for l in range(2):
            for nb in range(6):
                u = next_unit()
                wv = u["raw"][:, 0:4096].bitcast(BF16).rearrange("p (k n) -> p k n", k=8)
                P.dma("pool", wv, ada_w_d[l, :, nb * 1024:(nb + 1) * 1024].rearrange("(k p) n -> p k n", p=128), u["sem"], writes=[u["buf"]])
                for j in range(8):
                    col = nb * 8 + j
                    for k in range(8):
                        P.op("pe", lambda e, wv=wv, j=j, k=k, col=col, l=l: e.matmul(out=ps[l][:, col:col + 1], lhsT=wv[:, k, j * 128:(j + 1) * 128], rhs=condb[:, k:k + 1], start=(k == 0), stop=(k == 7)),
                             reads=[u["buf"], b_c], writes=[bps[l]])
            P.op("dve", lambda e, l=l: e.tensor_tensor(out=mods[:, l * 48:(l + 1) * 48], in0=ps[l][:, 0:48], in1=pt[:, PT["ada_b"] + l * 48:PT["ada_b"] + (l + 1) * 48], op=ALU.add),
                 reads=[bps[l], b_init], writes=[b_mods])
            for o in (8, 32):
                P.op("dve", lambda e, l=l, o=o: e.tensor_scalar_add(out=mods[:, l * 48 + o:l * 48 + o + 8], in0=mods[:, l * 48 + o:l * 48 + o + 8], scalar1=1.0),
                     reads=[b_mods], writes=[b_mods])
        P.op("dve", lambda e: e.tensor_tensor(out=g1b[:], in0=mods[:, 16:24], in1=pt[:, PT["b_pw2"]:PT["b_pw2"] + 8], op=ALU.mult), reads=[b_mods, b_init], writes=[b_mods])

        def mcol(l, o, c):
            return mods[:, l * 48 + o + c:l * 48 + o + c + 1]

        b_stage = [Buf("stage0"), Buf("stage1")]
        claim("Ra", b_stage)
        for i in range(16):
            sl = i % 2
            sv = stage[:, sl * D:(sl + 1) * D]
            P.dma("sp", sv, x_d[i * 128:(i + 1) * 128, :], f"xin{sl}", writes=[b_stage[sl]])
            for g in range(2):
                bank = 2 + g
                for cc in range(4):
                    c = g * 4 + cc
                    P.op("pe", lambda e, sv=sv, c=c, cc=cc, bank=bank: e.transpose(out=ps[bank][:, cc * 128:(cc + 1) * 128], in_=sv[:, c * 128:(c + 1) * 128], identity=ident[:]),
                         reads=[b_stage[sl], b_init], writes=[bps[bank]])
                eng = "act" if g == 0 else "dve"
                if eng == "act":
                    P.op("act", lambda e, g=g, i=i, bank=bank: e.activation(out=xT[:, g * 4:(g + 1) * 4, i * 128:(i + 1) * 128], in_=ps[bank][:].rearrange("p (c t) -> p c t", c=4), func=AF.Copy),
                         reads=[bps[bank]], writes=[bxT[i // 4]])
                else:
                    P.op("dve", lambda e, g=g, i=i, bank=bank: e.tensor_copy(out=xT[:, g * 4:(g + 1) * 4, i * 128:(i + 1) * 128], in_=ps[bank][:].rearrange("p (c t) -> p c t", c=4)),
                         reads=[bps[bank]], writes=[bxT[i // 4]])

        sq = [R_s[:, 0:BLK], R_s[:, BLK:2 * BLK]]
        b_sq = [Buf("sq0"), Buf("sq1")]
        mean_t = R_s[:, 2 * BLK:3 * BLK]
        rstd_t = R_s[:, 3 * BLK:4 * BLK]
        nmr_t = R_s[:, 4 * BLK:5 * BLK]
        b_stat = Buf("stat")
        sqc = [0]

        def _clone(b):
            nb = Buf(b.name + "_c")
            nb.w = b.w
            nb.r = dict(b.r)
            nb.rd = list(b.rd)
            return nb

        def layer_norm_block(src, b_src, dst, b_dst, gcol, bcol, func, bank_s, bank_q):
            inplace = b_src is b_dst
            cbuf = [_clone(b_src) for _ in range(8)]
            dbuf = cbuf if inplace else [_clone(b_dst) for _ in range(8)]
            for c in range(8):
                s = sqc[0] % 2
                sqc[0] += 1
                P.op("act", lambda e, c=c, s=s: e.activation(out=sq[s], in_=src(c), func=AF.Square), reads=[cbuf[c]], writes=[b_sq[s]])
                P.op("pe", lambda e, c=c: e.matmul(out=ps[bank_s][:], lhsT=ones[:], rhs=src(c), start=(c == 0), stop=(c == 7)), reads=[cbuf[c], b_c], writes=[bps[bank_s]])
                P.op("pe", lambda e, c=c, s=s: e.matmul(out=ps[bank_q][:], lhsT=ones[:], rhs=sq[s], start=(c == 0), stop=(c == 7)), reads=[b_sq[s], b_c], writes=[bps[bank_q]])
            P.op("dve", lambda e: e.tensor_scalar_mul(out=mean_t, in0=ps[bank_s][:], scalar1=1.0 / D), reads=[bps[bank_s]], writes=[b_stat])
            P.op("dve", lambda e: e.tensor_tensor(out=nmr_t, in0=mean_t, in1=mean_t, op=ALU.mult), reads=[b_stat], writes=[b_stat])
            P.op("dve", lambda e: e.scalar_tensor_tensor(out=rstd_t, in0=ps[bank_q][:], scalar=1.0 / D, in1=nmr_t, op0=ALU.mult, op1=ALU.subtract), reads=[bps[bank_q], b_stat], writes=[b_stat])
            P.op("act", lambda e: e.activation(out=rstd_t, in_=rstd_t, func=AF.Sqrt, bias=epsT[:, 0:1], scale=1.0), reads=[b_stat, b_c], writes=[b_stat])
            P.op("dve", lambda e: e.reciprocal(out=rstd_t, in_=rstd_t), reads=[b_stat], writes=[b_stat])
            P.op("dve", lambda e: e.scalar_tensor_tensor(out=nmr_t, in0=mean_t, scalar=-1.0, in1=rstd_t, op0=ALU.mult, op1=ALU.mult), reads=[b_stat], writes=[b_stat])
            last = {}
            for c in range(8):
                last["d1"] = P.op("dve", lambda e, c=c: e.tensor_tensor(out=src(c), in0=src(c), in1=rstd_t, op=ALU.mult), reads=[cbuf[c], b_stat], writes=[cbuf[c]])
                last["d2"] = P.op("dve", lambda e, c=c: e.tensor_tensor(out=src(c), in0=src(c), in1=nmr_t, op=ALU.add), reads=[cbuf[c], b_stat], writes=[cbuf[c]])
                last["a"] = P.op("act", lambda e, c=c: e.activation(out=dst(c), in_=src(c), func=func, scale=pt[:, gcol + c:gcol + c + 1], bias=pt[:, bcol + c:bcol + c + 1]),
                                 reads=[cbuf[c], b_init], writes=[dbuf[c]])
            if inplace:
                b_src.w = last["a"]
                b_src.r = {}
                b_src.rd = []
            else:
                b_src.w = last["d2"]
                b_src.r = {"act": last["a"]}
                b_src.rd = []
                b_dst.w = last["a"]
                b_dst.r = {}
                b_dst.rd = []

        def conv_sublayer():
            l = 0
            wpw1 = R_w[:, 0:8192].bitcast(BF16).rearrange("p (k n) -> p k n", k=8)
            wpw2 = R_w[:, 8192:12288].bitcast(BF16).rearrange("p (k n) -> p k n", k=8)
            b_w1 = [Buf(f"wpw1_{i}") for i in range(4)]
            b_w2 = Buf("wpw2")
            claim("Rw", b_w1 + [b_w2])
            for i in range(4):
                P.dma("pool", wpw1[:, :, i * 512:(i + 1) * 512], wpw1_d[:, i * 512:(i + 1) * 512].rearrange("(k p) n -> p k n", p=128), "wconv",
                      writes=[b_w1[i]])
            P.dma("pool", wpw2, wpw2_d.rearrange("(k p) n -> p k n", p=128), "wconv", writes=[b_w2])
            hblk = R_h[:, 0:2048].bitcast(BF16).rearrange("p (c t) -> p c t", c=8)
            sblk = R_h[:, 2048:4096].bitcast(BF16).rearrange("p (c t) -> p c t", c=8)
            vblk = R_h[:, 4096:8192].rearrange("p (c t) -> p c t", c=8)
            ub = [R_a[:, 0:271].bitcast(BF16), R_a[:, 272:543].bitcast(BF16)]
            halo = R_a[:, 1084:1084 + 120].bitcast(BF16).rearrange("p (c t) -> p c t", c=8)
            diag = [R_m[:, 0:1984].bitcast(BF16).rearrange("p (t n) -> p t n", t=31), R_m[:, 2048:2048 + 1984].bitcast(BF16).rearrange("p (t n) -> p t n", t=31)]
            b_diag = [Buf("diag0"), Buf("diag1")]
            claim("Rm", b_diag)
            sgt = [R_a[:, 1324:1324 + 512], R_s[:, 0:BLK]]
            b_h, b_s, b_v = Buf("hblk"), Buf("sblk"), Buf("vblk")
            b_u = [Buf("u0"), Buf("u1")]
            b_halo = Buf("halo")
            b_sg = [Buf("sg0"), b_sq[0]]
            claim("Rs", [b_sq[0], b_sq[1], b_stat])
            claim("Rh", [b_h, b_s, b_v])
            claim("Ra", [b_u[0], b_u[1], b_halo, b_sg[0]])
            P.op("pool", lambda e: e.memset(halo, 0.0), writes=[b_halo])
            wdw0 = PT["w_dw"]
            for tb in range(NBLK):
                for c in range(8):
                    P.op("act", lambda e, c=c, tb=tb: e.activation(out=hblk[:, c, :], in_=xT[:, c, blk(tb)], func=AF.Identity, scale=mcol(l, 8, c), bias=mcol(l, 0, c)),
                         reads=[bxT[tb], b_mods], writes=[b_h])
                def pw1(j):
                    ba, bg = (0, 1) if j % 2 == 0 else (2, 3)
                    s = j % 2
                    dg = diag[s]
                    P.op("dve", lambda e: e.tensor_tensor(out=dg, in0=identb[:].unsqueeze(1).to_broadcast([128, 31, 128]),
                                                          in1=pt[:, wdw0 + j * 31:wdw0 + (j + 1) * 31].unsqueeze(2).to_broadcast([128, 31, 128]), op=ALU.mult),
                         reads=[b_c, b_init], writes=[b_diag[s]])
                    for k in range(8):
                        P.op("pe", lambda e, k=k: e.matmul(out=ps[ba][:], lhsT=wpw1[:, k, j * 128:(j + 1) * 128], rhs=hblk[:, k, :], start=(k == 0), stop=(k == 7)),
                             reads=[b_w1[j // 4], b_h], writes=[bps[ba]])
                    for k in range(8):
                        P.op("pe", lambda e, k=k: e.matmul(out=ps[bg][:], lhsT=wpw1[:, k, 1024 + j * 128:1024 + (j + 1) * 128], rhs=hblk[:, k, :], start=(k == 0), stop=(k == 7)),
                             reads=[b_w1[2 + j // 4], b_h], writes=[bps[bg]])

                def glu_conv(j):
                    ba, bg = (0, 1) if j % 2 == 0 else (2, 3)
                    s = j % 2
                    u_ = ub[s]
                    dg = diag[s]
                    P.op("act", lambda e: e.activation(out=sgt[s], in_=ps[bg][:], func=AF.Sigmoid, bias=pt[:, PT["b_pw1"] + 8 + j:PT["b_pw1"] + 9 + j], scale=1.0),
                         reads=[bps[bg], b_init], writes=[b_sg[s]])
                    P.op("pool", lambda e: e.tensor_copy(out=u_[:, 0:30], in_=halo[:, j, :]), reads=[b_halo], writes=[b_u[s]])
                    P.op("dve", lambda e: e.scalar_tensor_tensor(out=u_[:, 30:542], in0=ps[ba][:], scalar=pt[:, PT["b_pw1"] + j:PT["b_pw1"] + j + 1], in1=sgt[s], op0=ALU.add, op1=ALU.mult),
                         reads=[bps[ba], b_sg[s], b_init], writes=[b_u[s]])
                    P.op("pool", lambda e: e.tensor_copy(out=halo[:, j, :], in_=u_[:, 512:542]), reads=[b_u[s]], writes=[b_halo])
                    cb = 6 + s
                    for tap in range(31):
                        P.op("pe", lambda e, tap=tap: e.matmul(out=ps[cb][:], lhsT=dg[:, tap, :], rhs=u_[:, tap:tap + 512], start=(tap == 0), stop=(tap == 30)),
                             reads=[b_diag[s], b_u[s]], writes=[bps[cb]])
                    P.op("dve", lambda e: e.tensor_scalar(out=vblk[:, j, :], in0=ps[cb][:], scalar1=pt[:, PT["b_dw"] + j:PT["b_dw"] + j + 1], scalar2=None, op0=ALU.add),
                         reads=[bps[cb], b_init], writes=[b_v])

                pw1(0)
                for j in range(8):
                    if j + 1 < 8:
                        pw1(j + 1)
                    glu_conv(j)
                layer_norm_block(lambda c: vblk[:, c, :], b_v, lambda c: sblk[:, c, :], b_s, PT["cln_g"], PT["cln_b"], AF.Silu, 4, 5)
                for d in range(8):
                    bank = 6 + d % 2
                    for k in range(8):
                        P.op("pe", lambda e, d=d, k=k, bank=bank: e.matmul(out=ps[bank][:], lhsT=wpw2[:, k, d * 128:(d + 1) * 128], rhs=sblk[:, k, :], start=(k == 0), stop=(k == 7)),
                             reads=[b_w2, b_s], writes=[bps[bank]])
                    s = d % 2
                    P.op("act", lambda e, d=d, bank=bank, s=s: e.activation(out=sgt[s], in_=ps[bank][:], func=AF.Identity, scale=mcol(l, 16, d), bias=g1b[:, d:d + 1]),
                         reads=[bps[bank], b_mods], writes=[b_sg[s]])
                    P.op("dve", lambda e, d=d, tb=tb, s=s: e.scalar_tensor_tensor(out=xT[:, d, blk(tb)], in0=xT[:, d, blk(tb)], scalar=ALPHA, in1=sgt[s], op0=ALU.mult, op1=ALU.add),
                         reads=[b_sg[s], bxT[tb]], writes=[bxT[tb]])
                pg = PT["post_g"] + (l * 2 + 0) * 8
                pb = PT["post_b"] + (l * 2 + 0) * 8
                layer_norm_block(lambda c, tb=tb: xT[:, c, blk(tb)], bxT[tb], lambda c, tb=tb: xT[:, c, blk(tb)], bxT[tb], pg, pb, AF.Identity, 4, 5)

        def moe_sublayer(l):
            hT = R_h[:].bitcast(BF16).rearrange("p (c t) -> p c t", c=8)
            b_hT = [Buf(f"hT{b}") for b in range(NBLK)]
            b_GT = Buf("GT")
            b_lg = Buf("lg")
            actT = [R_a[:, 0:1024].bitcast(BF16).rearrange("p (c t) -> p c t", c=4), R_a[:, 1024:2048].bitcast(BF16).rearrange("p (c t) -> p c t", c=4)]
            b_act = [Buf("act0"), Buf("act1")]
            gc = R_s[:, 0:BLK]
            sgm = R_s[:, BLK:2 * BLK]
            lin = R_s[:, 2 * BLK:3 * BLK]
            b_gc, b_sgm, b_lin = b_sq[0], b_sq[1], b_stat
            b_gb = Buf("gb")
            b_gsel = [Buf("gsel0"), Buf("gsel1")]
            claim("Rh", b_hT)
            claim("Rm", [b_GT, b_gb] + b_gsel)
            claim("Rs", [b_sq[0], b_sq[1], b_stat])
            claim("Ra", b_act)
            claim("Rw", [u["buf"] for u in units])
            for tb in range(NBLK):
                for c in range(8):
                    P.op("act", lambda e, c=c, tb=tb: e.activation(out=hT[:, c, blk(tb)], in_=xT[:, c, blk(tb)], func=AF.Identity, scale=mcol(l, 32, c), bias=mcol(l, 24, c)),
                         reads=[bxT[tb], b_mods], writes=[b_hT[tb]])
                for c in range(8):
                    P.op("dve", lambda e, c=c, tb=tb: e.tensor_scalar_mul(out=xT[:, c, blk(tb)], in0=xT[:, c, blk(tb)], scalar1=ALPHA),
                         reads=[bxT[tb]], writes=[bxT[tb]])
            wr = wr_s[:].rearrange("p (l k e) -> p l k e", l=2, k=8)
            for i in range(16):
                tb = i // 4
                tsl = slice(i * 128, (i + 1) * 128)
                for k in range(8):
                    P.op("pe", lambda e, k=k, tsl=tsl: e.matmul(out=ps[7][:, 0:NE], lhsT=hT[:, k, tsl], rhs=wr[:, l, k, :], start=(k == 0), stop=False),
                         reads=[b_hT[tb], b_init], writes=[bps[7]])
                P.op("pe", lambda e: e.matmul(out=ps[7][:, 0:NE], lhsT=onesb[0:1, :], rhs=br_s[0:1, l * NE:(l + 1) * NE], start=False, stop=True),
                     reads=[b_c, b_init], writes=[bps[7]])
                L = lg[:, 0:NE]
                M = lg[:, NE:2 * NE]
                E = lg[:, 2 * NE:3 * NE]
                G = lg[:, 3 * NE:4 * NE]
                P.op("dve", lambda e, L=L: e.tensor_copy(out=L, in_=ps[7][:, 0:NE]), reads=[bps[7]], writes=[b_lg])
                P.op("dve", lambda e, L=L: e.max(out=mx8[:, 0:8], in_=L), reads=[b_lg], writes=[b_lg])
                P.op("dve", lambda e, L=L, M=M: e.tensor_scalar(out=M, in0=L, scalar1=mx8[:, 3:4], scalar2=None, op0=ALU.is_ge), reads=[b_lg], writes=[b_lg])
                P.op("dve", lambda e: e.tensor_scalar_mul(out=mx8[:, 8:9], in0=mx8[:, 0:1], scalar1=-1.0), reads=[b_lg], writes=[b_lg])
                P.op("act", lambda e, L=L, E=E: e.activation(out=E, in_=L, func=AF.Exp, bias=mx8[:, 8:9], scale=1.0), reads=[b_lg], writes=[b_lg])
                P.op("dve", lambda e, M=M, E=E: e.tensor_tensor(out=E, in0=E, in1=M, op=ALU.mult), reads=[b_lg], writes=[b_lg])
                P.op("dve", lambda e, E=E: e.reduce_sum(out=mx8[:, 9:10], in_=E, axis=AX.X), reads=[b_lg], writes=[b_lg])
                P.op("dve", lambda e: e.reciprocal(out=mx8[:, 10:11], in_=mx8[:, 9:10]), reads=[b_lg], writes=[b_lg])
                P.op("dve", lambda e, E=E, G=G: e.tensor_scalar(out=G, in0=E, scalar1=mx8[:, 10:11], scalar2=None, op0=ALU.mult), reads=[b_lg], writes=[b_lg])
                P.op("pe", lambda e, G=G: e.transpose(out=ps[6][0:NE, 0:128], in_=G, identity=ident[:]), reads=[b_lg, b_init], writes=[bps[6]])
                P.op("act", lambda e, tsl=tsl: e.activation(out=GT[:, tsl], in_=ps[6][0:NE, 0:128], func=AF.Copy), reads=[bps[6]], writes=[b_GT])
                P.op("dve", lambda e, tsl=tsl: e.tensor_copy(out=GTb[:, tsl], in_=ps[6][0:NE, 0:128]), reads=[bps[6]], writes=[b_GT])

            def issue_unit(uidx):
                e_, half = uidx // 2, uidx % 2
                u = next_unit()
                g = u["raw"][:, 0:2048].bitcast(BF16).rearrange("p (k n) -> p k n", k=8)
                lw = u["raw"][:, 2048:4096].bitcast(BF16).rearrange("p (k n) -> p k n", k=8)
                dw = u["raw"][:, 4096:6144].bitcast(BF16).rearrange("p (f n) -> p f n", f=4)
                P.dma("pool", g, wgu_d[l, e_, :, half * 512:(half + 1) * 512].rearrange("(k p) n -> p k n", p=128), u["sem"], writes=[u["buf"]])
                P.dma("pool", lw, wgu_d[l, e_, :, 1024 + half * 512:1024 + (half + 1) * 512].rearrange("(k p) n -> p k n", p=128), u["sem"], writes=[u["buf"]])
                P.dma("pool", dw, wdn_d[l, e_, half * 512:(half + 1) * 512, :].rearrange("(f p) n -> p f n", p=128), u["sem"], writes=[u["buf"]])
                return (u, g, lw, dw)

            NU = 2 * NE
            pending = [issue_unit(0)]
            actc = [0]
            for uidx in range(NU):
                if uidx + 1 < NU:
                    pending.append(issue_unit(uidx + 1))
                u, g, lw, dw = pending.pop(0)
                e_, half = uidx // 2, uidx % 2
                bcol = (l * NE + e_) * 16
                if half == 0:
                    for tb in range(NBLK):
                        gs = gsel[:, (tb % 2) * BLK:(tb % 2 + 1) * BLK]
                        P.op("dve", lambda e, e_=e_, tb=tb, gs=gs: e.tensor_scalar(out=gs, in0=GT[:, blk(tb)], scalar1=ident[0:NE, e_:e_ + 1], scalar2=None, op0=ALU.mult), reads=[b_GT, b_init], writes=[b_gsel[tb % 2]])
                        P.op("pe", lambda e, gs=gs: e.matmul(out=ps[6][:], lhsT=ones[0:NE, :], rhs=gs, start=True, stop=True), reads=[b_gsel[tb % 2], b_c], writes=[bps[6]])
                        P.op("act", lambda e, tb=tb: e.activation(out=gbT[:, blk(tb)], in_=ps[6][:], func=AF.Copy), reads=[bps[6]], writes=[b_gb])
                for tb in range(NBLK):
                    a = actc[0] % 2
                    actc[0] += 1
                    for fc in range(4):
                        pg_, pl_ = (0, 1) if fc % 2 == 0 else (2, 3)
                        for k in range(8):
                            P.op("pe", lambda e, g=g, k=k, fc=fc, tb=tb, pg_=pg_: e.matmul(out=ps[pg_][:], lhsT=g[:, k, fc * 128:(fc + 1) * 128], rhs=hT[:, k, blk(tb)], start=(k == 0), stop=(k == 7)),
                                 reads=[u["buf"], b_hT[tb]], writes=[bps[pg_]])
                        for k in range(8):
                            P.op("pe", lambda e, lw=lw, k=k, fc=fc, tb=tb, pl_=pl_: e.matmul(out=ps[pl_][:], lhsT=lw[:, k, fc * 128:(fc + 1) * 128], rhs=hT[:, k, blk(tb)], start=(k == 0), stop=(k == 7)),
                                 reads=[u["buf"], b_hT[tb]], writes=[bps[pl_]])
                        jg = bcol + half * 4 + fc
                        jl = bcol + 8 + half * 4 + fc
                        P.op("dve", lambda e, pg_=pg_, jg=jg: e.tensor_scalar(out=gc, in0=ps[pg_][:], scalar1=pt[:, PT["b_gu"] + jg:PT["b_gu"] + jg + 1], scalar2=7.0, op0=ALU.add, op1=ALU.min),
                             reads=[bps[pg_], b_init], writes=[b_gc])
                        P.op("act", lambda e: e.activation(out=sgm, in_=gc, func=AF.Sigmoid, scale=1.702), reads=[b_gc], writes=[b_sgm])
                        P.op("dve", lambda e, pl_=pl_, jl=jl: e.tensor_scalar(out=lin, in0=ps[pl_][:], scalar1=pt[:, PT["b_gu"] + jl:PT["b_gu"] + jl + 1], scalar2=8.0, op0=ALU.add, op1=ALU.min),
                             reads=[bps[pl_], b_init], writes=[b_lin])
                        P.op("dve", lambda e: e.tensor_tensor(out=gc, in0=gc, in1=sgm, op=ALU.mult), reads=[b_gc, b_sgm], writes=[b_gc])
                        P.op("dve", lambda e: e.scalar_tensor_tensor(out=lin, in0=lin, scalar=-6.0, in1=gc, op0=ALU.max, op1=ALU.mult), reads=[b_lin, b_gc], writes=[b_lin])
                        P.op("dve", lambda e, a=a, fc=fc, tb=tb: e.tensor_tensor(out=actT[a][:, fc, :], in0=lin, in1=gbT[:, blk(tb)], op=ALU.mult), reads=[b_lin, b_gb], writes=[b_act[a]])
                    for d in range(8):
                        bank = 4 + d % 2
                        for fc in range(4):
                            last = (fc == 3) and not (uidx == 0)
                            P.op("pe", lambda e, dw=dw, fc=fc, d=d, a=a, bank=bank, last=last: e.matmul(out=ps[bank][:], lhsT=dw[:, fc, d * 128:(d + 1) * 128], rhs=actT[a][:, fc, :], start=(fc == 0), stop=last),
                                 reads=[u["buf"], b_act[a]], writes=[bps[bank]])
                        if uidx == 0:
                            P.op("pe", lambda e, d=d, tb=tb, bank=bank: e.matmul(out=ps[bank][:], lhsT=bdn_s[:, l * D + d * 128:l * D + (d + 1) * 128], rhs=GTb[:, blk(tb)], start=False, stop=True),
                                 reads=[b_init, b_GT], writes=[bps[bank]])
                        P.op("dve", lambda e, d=d, tb=tb, bank=bank: e.scalar_tensor_tensor(out=xT[:, d, blk(tb)], in0=ps[bank][:], scalar=mcol(l, 40, d), in1=xT[:, d, blk(tb)], op0=ALU.mult, op1=ALU.add),
                             reads=[bps[bank], b_mods, bxT[tb]], writes=[bxT[tb]])
            pg = PT["post_g"] + (l * 2 + 1) * 8
            pb = PT["post_b"] + (l * 2 + 1) * 8
            for tb in range(NBLK):
                layer_norm_block(lambda c, tb=tb: xT[:, c, blk(tb)], bxT[tb], lambda c, tb=tb: xT[:, c, blk(tb)], bxT[tb], pg, pb, AF.Identity, 0, 1)

        def attn_sublayer():
            l = 1
            xb = R_h[:].bitcast(BF16).rearrange("p (c t) -> p c t", c=8)
            h1b = R_w[:, 0:8192].bitcast(BF16).rearrange("p (c t) -> p c t", c=8)
            KT = R_w[:, 8192:9216].bitcast(BF16)
            QT = R_w[:, 9216:10240].bitcast(BF16)
            Vh = R_w[:, 10240:11264].bitcast(BF16).rearrange("p (i e) -> p i e", i=16)
            AT = R_w[:, 11264:12288].bitcast(BF16)
            Zs = R_m[:, 4096:5120]
            masks = R_a[:].rearrange("p (o q) -> p o q", o=4)
            PTt = [R_s[:, 0:256].bitcast(BF16), R_s[:, 256:512].bitcast(BF16)]
            tmp = [R_s[:, 512:1024], R_s[:, 1024:1536]]
            o0 = R_s[:, 1536:2048]
            rsc = R_s[:, 2048:2560]
            b_xb = [Buf(f"xb{i}") for i in range(NBLK)]
            b_h1 = [Buf(f"h1b{i}") for i in range(NBLK)]
            b_KT, b_QT, b_V, b_AT = Buf("KT"), Buf("QT"), Buf("Vh"), Buf("AT")
            b_Zs, b_mask = Buf("Zs"), Buf("masks")
            b_PT = [Buf("PT0"), Buf("PT1")]
            b_tmp = [Buf("tmp0"), Buf("tmp1")]
            b_o0, b_rsc = Buf("o0"), Buf("rsc")
            b_wt = [Buf("awt0"), Buf("awt1")]
            b_gs = [Buf(f"gs{h}") for h in range(8)]
            b_lam = Buf("lam")
            claim("Rh", b_xb)
            claim("Rw", b_h1 + [b_KT, b_QT, b_V, b_AT])
            claim("Rm", b_wt + [b_Zs])
            claim("Ra", [b_mask])
            P.dma("sp", masks, masks_d.rearrange("o p q -> p o q"), "amask", writes=[b_mask])
            P.op("dve", lambda e: e.tensor_tensor(out=lpT[:, 0:1], in0=lpT[:, 0:1], in1=lpT[:, 1:2], op=ALU.mult), reads=[b_init], writes=[b_lam])
            P.op("dve", lambda e: e.tensor_tensor(out=lpT[:, 1:2], in0=lpT[:, 2:3], in1=lpT[:, 3:4], op=ALU.mult), reads=[b_init, b_lam], writes=[b_lam])
            P.op("pe", lambda e: e.matmul(out=ps[7][:, 0:2], lhsT=ones[0:64, :], rhs=lpT[:, 0:2], start=True, stop=True), reads=[b_lam, b_c], writes=[bps[7]])
            P.op("act", lambda e: e.activation(out=nlam[:, 0:2], in_=ps[7][:, 0:2], func=AF.Exp), reads=[bps[7]], writes=[b_lam])
            P.op("dve", lambda e: e.tensor_tensor(out=nlam[:, 2:3], in0=nlam[:, 1:2], in1=nlam[:, 0:1], op=ALU.subtract), reads=[b_lam], writes=[b_lam])
            P.op("dve", lambda e: e.tensor_scalar_add(out=nlam[:, 3:4], in0=nlam[:, 2:3], scalar1=-LAMI), reads=[b_lam], writes=[b_lam])
            P.op("dve", lambda e: e.tensor_scalar_mul(out=gsub[:], in0=pt[:, PT["subln"]:PT["subln"] + 1], scalar1=1.0 - LAMI), reads=[b_init], writes=[b_lam])
            gst = R_s[:, 0:RG]
            rep = R_s[0:32, RG:RG + 128]
            ohg = R_s[0:32, RG + 128:RG + 128 + RG]
            b_gst, b_rep = Buf("gst"), Buf("rep")
            claim("Rs", [b_gst, b_rep])
            P.dma("sp", ohg, ohg_d[:, :], "ohg", writes=[b_rep])
            for h in range(8):
                P.op("act", lambda e, h=h: e.activation(out=rep, in_=ones[0:32, :], func=AF.Identity, scale=tabs[:, h:h + 1]), reads=[b_init, b_c], writes=[b_rep])
                for (n0, nn) in ((0, 512), (512, 512), (1024, 128)):
                    P.op("pe", lambda e, n0=n0, nn=nn: e.matmul(out=ps[6][:, 0:nn], lhsT=rep, rhs=ohg[:, n0:n0 + nn], start=True, stop=True), reads=[b_rep, b_init], writes=[bps[6]])
                    P.op("dve", lambda e, n0=n0, nn=nn: e.tensor_copy(out=gst[:, n0:n0 + nn], in_=ps[6][:, 0:nn]), reads=[bps[6]], writes=[b_gst])
                P.dma("sp", gs_d[h, :, :], gst, "gsw", reads=[b_gst], writes=[b_gs[h]])
            for b in b_PT + b_tmp + [b_o0, b_rsc]:
                pass
            claim("Rs", b_PT + b_tmp + [b_o0, b_rsc])
            for tb in range(NBLK):
                for c in range(8):
                    P.op("act", lambda e, c=c, tb=tb: e.activation(out=xb[:, c, blk(tb)], in_=xT[:, c, blk(tb)], func=AF.Copy), reads=[bxT[tb]], writes=[b_xb[tb]])
                    P.op("act", lambda e, c=c, tb=tb: e.activation(out=h1b[:, c, blk(tb)], in_=xT[:, c, blk(tb)], func=AF.Identity, scale=mcol(l, 8, c), bias=mcol(l, 0, c)),
                         reads=[bxT[tb], b_mods], writes=[b_h1[tb]])
                for c in range(8):
                    P.op("dve", lambda e, c=c, tb=tb: e.tensor_scalar_mul(out=xT[:, c, blk(tb)], in0=xT[:, c, blk(tb)], scalar1=ALPHA), reads=[bxT[tb]], writes=[bxT[tb]])
            for h in range(8):
                wsl = h % 2
                wbase = wsl * 2048
                wk = R_m[:, wbase:wbase + 512].bitcast(BF16).rearrange("p (k n) -> p k n", k=8)
                wv = R_m[:, wbase + 512:wbase + 1024].bitcast(BF16).rearrange("p (k n) -> p k n", k=8)
                wq = R_m[:, wbase + 1024:wbase + 1536].bitcast(BF16).rearrange("p (k n) -> p k n", k=8)
                wo = R_m[:, wbase + 1536:wbase + 2048].bitcast(BF16)
                hs = slice(h * 128, (h + 1) * 128)
                P.dma("pool", wk, wkv_d[:, hs].rearrange("(k p) n -> p k n", p=128), f"awt{wsl}", writes=[b_wt[wsl]])
                P.dma("pool", wv, wkv_d[:, D + h * 128:D + (h + 1) * 128].rearrange("(k p) n -> p k n", p=128), f"awt{wsl}", writes=[b_wt[wsl]])
                P.dma("pool", wq, wq_d[:, hs].rearrange("(k p) n -> p k n", p=128), f"awt{wsl}", writes=[b_wt[wsl]])
                P.dma("pool", wo, wo_d[hs, :], f"awt{wsl}", writes=[b_wt[wsl]])
                zsrc = bass.AP(gs_t, h * 128 * RG + 127, [[RG - 1, 128], [1, 1024]])
                P.dma("sp", Zs, zsrc, "zs", reads=[b_gs[h]], writes=[b_Zs])
                for tb in range(NBLK):
                    for k in range(8):
                        P.op("pe", lambda e, k=k, tb=tb, wk=wk: e.matmul(out=ps[6][:], lhsT=wk[:, k, :], rhs=xb[:, k, blk(tb)], start=(k == 0), stop=(k == 7)), reads=[b_wt[wsl], b_xb[tb]], writes=[bps[6]])
                    P.op("act", lambda e, tb=tb: e.activation(out=KT[:, blk(tb)], in_=ps[6][:], func=AF.Copy), reads=[bps[6]], writes=[b_KT])
                    for k in range(8):
                        P.op("pe", lambda e, k=k, tb=tb, wq=wq: e.matmul(out=ps[7][:], lhsT=wq[:, k, :], rhs=h1b[:, k, blk(tb)], start=(k == 0), stop=(k == 7)), reads=[b_wt[wsl], b_h1[tb]], writes=[bps[7]])
                    P.op("dve", lambda e, tb=tb: e.tensor_copy(out=QT[:, blk(tb)], in_=ps[7][:]), reads=[bps[7]], writes=[b_QT])
                for i4 in range(4):
                    bank = 6 + i4 % 2
                    for ii in range(4):
                        i = i4 * 4 + ii
                        for k in range(8):
                            P.op("pe", lambda e, k=k, i=i, ii=ii, wv=wv, bank=bank: e.matmul(out=ps[bank][:, ii * 128:(ii + 1) * 128], lhsT=xb[:, k, i * 128:(i + 1) * 128], rhs=wv[:, k, :], start=(k == 0), stop=(k == 7)),
                                 reads=[b_wt[wsl], b_xb[i // 4]], writes=[bps[bank]])
                    if i4 % 2 == 0:
                        P.op("act", lambda e, i4=i4, bank=bank: e.activation(out=Vh[:, i4 * 4:(i4 + 1) * 4, :], in_=ps[bank][:].rearrange("p (i e) -> p i e", i=4), func=AF.Copy), reads=[bps[bank]], writes=[b_V])
                    else:
                        P.op("dve", lambda e, i4=i4, bank=bank: e.tensor_copy(out=Vh[:, i4 * 4:(i4 + 1) * 4, :], in_=ps[bank][:].rearrange("p (i e) -> p i e", i=4)), reads=[bps[bank]], writes=[b_V])
                tiles = [(qb, m, kt) for qb in range(NBLK) for m in range(2) for kt in range(4 * qb + 4)]
                NTL = len(tiles)
                pending = []

                def stageA(idx):
                    qb, m, kt = tiles[idx]
                    msl = slice(m * 64, (m + 1) * 64)
                    off = kt * 128 - qb * 512
                    sb = idx % 2
                    P.op("pe", lambda e: e.matmul(out=ps[sb][:], lhsT=KT[msl, kt * 128:(kt + 1) * 128], rhs=QT[msl, blk(qb)], start=True, stop=True),
                         reads=[b_KT, b_QT], writes=[bps[sb]])
                    if off <= -256:
                        P.op("act", lambda e: e.activation(out=PTt[sb], in_=ps[sb][:], func=AF.Exp, scale=0.125, bias=negc[:, 0:1]), reads=[bps[sb], b_c], writes=[b_PT[sb]])
                    else:
                        z0 = 384 - off
                        P.op("dve", lambda e: e.scalar_tensor_tensor(out=tmp[sb], in0=ps[sb][:], scalar=0.125, in1=Zs[:, z0:z0 + 512], op0=ALU.mult, op1=ALU.add),
                             reads=[bps[sb], b_Zs], writes=[b_tmp[sb]])
                        if off >= 0:
                            P.op("dve", lambda e: e.tensor_tensor(out=tmp[sb], in0=tmp[sb], in1=masks[:, off // 128, :], op=ALU.add), reads=[b_tmp[sb], b_mask], writes=[b_tmp[sb]])
                        P.op("act", lambda e: e.activation(out=PTt[sb], in_=tmp[sb], func=AF.Exp, scale=1.0, bias=negc[:, 0:1]), reads=[b_tmp[sb], b_c], writes=[b_PT[sb]])

                def seg1(qb, m):
                    po, psm = (2, 3) if m == 0 else (4, 5)
                    P.op("dve", lambda e: e.reciprocal(out=rsc, in_=ps[psm][:]), reads=[bps[psm]], writes=[b_rsc])
                    if m == 0:
                        P.op("dve", lambda e: e.tensor_tensor(out=o0, in0=ps[po][:], in1=rsc, op=ALU.mult), reads=[bps[po], b_rsc], writes=[b_o0])
                    else:
                        P.op("dve", lambda e: e.tensor_tensor(out=rsc, in0=ps[po][:], in1=rsc, op=ALU.mult), reads=[bps[po], b_rsc], writes=[b_rsc])
                        P.op("dve", lambda e: e.scalar_tensor_tensor(out=o0, in0=rsc, scalar=nlam[:, 3:4], in1=o0, op0=ALU.mult, op1=ALU.add), reads=[b_rsc, b_o0, b_lam], writes=[b_o0])
                        P.op("act", lambda e: e.activation(out=rsc, in_=o0, func=AF.Square), reads=[b_o0], writes=[b_rsc])

                def seg2(qb):
                    P.op("pe", lambda e: e.matmul(out=ps[6][:], lhsT=ones[:], rhs=rsc, start=True, stop=True), reads=[b_rsc, b_c], writes=[bps[6]])
                    P.op("act", lambda e: e.activation(out=rsc, in_=ps[6][:], func=AF.Sqrt, scale=1.0 / 128.0, bias=epsT[:, 0:1]), reads=[bps[6], b_c], writes=[b_rsc])
                    P.op("dve", lambda e: e.reciprocal(out=rsc, in_=rsc), reads=[b_rsc], writes=[b_rsc])
                    P.op("dve", lambda e: e.tensor_tensor(out=o0, in0=o0, in1=rsc, op=ALU.mult), reads=[b_o0, b_rsc], writes=[b_o0])
                    P.op("act", lambda e: e.activation(out=AT[:, blk(qb)], in_=o0, func=AF.Identity, scale=gsub[:, 0:1]), reads=[b_o0, b_lam], writes=[b_AT])

                def seg3(qb):
                    for d in range(8):
                        bank = 6 + d % 2
                        P.op("pe", lambda e, d=d, bank=bank, wo=wo: e.matmul(out=ps[bank][:], lhsT=wo[:, d * 128:(d + 1) * 128], rhs=AT[:, blk(qb)], start=True, stop=True), reads=[b_wt[wsl], b_AT], writes=[bps[bank]])
                        P.op("dve", lambda e, d=d, bank=bank: e.scalar_tensor_tensor(out=xT[:, d, blk(qb)], in0=ps[bank][:], scalar=mcol(l, 16, d), in1=xT[:, d, blk(qb)], op0=ALU.mult, op1=ALU.add),
                             reads=[bps[bank], b_mods, bxT[qb]], writes=[bxT[qb]])

                def stageB(idx):
                    qb, m, kt = tiles[idx]
                    ntile = 4 * qb + 4
                    po, psm = (2, 3) if m == 0 else (4, 5)
                    sb = idx % 2
                    P.op("pe", lambda e: e.matmul(out=ps[po][:], lhsT=Vh[:, kt, :], rhs=PTt[sb], start=(kt == 0), stop=(kt == ntile - 1)), reads=[b_V, b_PT[sb]], writes=[bps[po]])
                    P.op("pe", lambda e: e.matmul(out=ps[psm][:], lhsT=onesb[:], rhs=PTt[sb], start=(kt == 0), stop=(kt == ntile - 1)), reads=[b_c, b_PT[sb]], writes=[bps[psm]])
                    if kt == ntile - 1:
                        seg1(qb, m)
                        if m == 1:
                            pending.append((idx + 3, lambda qb=qb: seg2(qb)))
                            pending.append((idx + 5, lambda qb=qb: seg3(qb)))

                for idx in range(NTL + 1):
                    if idx < NTL:
                        stageA(idx)
                    if idx >= 1:
                        stageB(idx - 1)
                    while pending and pending[0][0] <= idx:
                        pending.pop(0)[1]()
                while pending:
                    pending.pop(0)[1]()
            pg = PT["post_g"] + (l * 2 + 0) * 8
            pb = PT["post_b"] + (l * 2 + 0) * 8
            claim("Rs", [b_sq[0], b_sq[1], b_stat])
            for tb in range(NBLK):
                layer_norm_block(lambda c, tb=tb: xT[:, c, blk(tb)], bxT[tb], lambda c, tb=tb: xT[:, c, blk(tb)], bxT[tb], pg, pb, AF.Identity, 0, 1)

        b_XS = [Buf(f"XS{i}") for i in range(8)]
        b_YS = [Buf("YS0"), Buf("YS1")]

        def moe_sparse(l):
            hT = R_h[:].bitcast(BF16).rearrange("p (c t) -> p c t", c=8)
            b_hT = [Buf(f"hT{b}") for b in range(NBLK)]
            b_GT, b_lg, b_rt = Buf("GT"), Buf("lg"), Buf("rt")
            b_M, b_cnt = Buf("Mall"), Buf("cnt")
            b_posl = [Buf(f"posI{i}") for i in range(16)]
            b_gatesl = [Buf(f"gates{i}") for i in range(16)]
            claim("Rh", b_hT)
            claim("Rm", [b_GT])
            claim("Rs", [b_sq[0], b_sq[1], b_stat])
            for tb in range(NBLK):
                for c in range(8):
                    P.op("act", lambda e, c=c, tb=tb: e.activation(out=hT[:, c, blk(tb)], in_=xT[:, c, blk(tb)], func=AF.Identity, scale=mcol(l, 32, c), bias=mcol(l, 24, c)),
                         reads=[bxT[tb], b_mods], writes=[b_hT[tb]])
                for c in range(8):
                    P.op("dve", lambda e, c=c, tb=tb: e.tensor_scalar_mul(out=xT[:, c, blk(tb)], in0=xT[:, c, blk(tb)], scalar1=ALPHA), reads=[bxT[tb]], writes=[bxT[tb]])
            wr = wr_s[:].rearrange("p (l k e) -> p l k e", l=2, k=8)
            Mv = Mall[:].rearrange("p (i e) -> p i e", i=16)
            L = lg[:, 0:NE]
            M = lg[:, NE:2 * NE]
            E = lg[:, 2 * NE:3 * NE]
            G = lg[:, 3 * NE:4 * NE]
            OH = rt[:, 0:128].rearrange("p (k e) -> p k e", k=4)
            TA = rt[:, 128:256].rearrange("p (k e) -> p k e", k=4)
            TB = rt[:, 256:384].rearrange("p (k e) -> p k e", k=4)
            eid4 = rt[:, 384:388]
            rank4 = rt[:, 388:392]
            pos4 = rt[:, 392:396]
            e4 = rt[:, 396:400]
            for i in range(16):
                tb = i // 4
                tsl = slice(i * 128, (i + 1) * 128)
                for k in range(8):
                    P.op("pe", lambda e, k=k, tsl=tsl: e.matmul(out=ps[7][:, 0:NE], lhsT=hT[:, k, tsl], rhs=wr[:, l, k, :], start=(k == 0), stop=False), reads=[b_hT[tb], b_init], writes=[bps[7]])
                P.op("pe", lambda e: e.matmul(out=ps[7][:, 0:NE], lhsT=onesb[0:1, :], rhs=br_s[0:1, l * NE:(l + 1) * NE], start=False, stop=True), reads=[b_c, b_init], writes=[bps[7]])
                P.op("dve", lambda e: e.tensor_copy(out=L, in_=ps[7][:, 0:NE]), reads=[bps[7]], writes=[b_lg])
                P.op("dve", lambda e: e.max(out=mx8[:, 0:8], in_=L), reads=[b_lg], writes=[b_lg])
                P.op("dve", lambda e: e.tensor_copy(out=TA[:, 0, :], in_=L), reads=[b_lg], writes=[b_rt])
                Lw = TA[:, 0, :]
                EQ = TA[:, 1, :]
                TT = TA[:, 2, :]
                for k in range(4):
                    P.op("dve", lambda e, k=k: e.tensor_scalar(out=EQ, in0=Lw, scalar1=mx8[:, k:k + 1], scalar2=None, op0=ALU.is_equal), reads=[b_rt, b_lg], writes=[b_rt])
                    P.op("dve", lambda e: e.scalar_tensor_tensor(out=TT, in0=EQ, scalar=-1000.0, in1=iotaP[:], op0=ALU.mult, op1=ALU.add), reads=[b_rt, b_c], writes=[b_rt])
                    P.op("dve", lambda e, k=k: e.tensor_reduce(out=eid4[:, k:k + 1], in_=TT, axis=AX.X, op=ALU.min), reads=[b_rt], writes=[b_rt])
                    P.op("dve", lambda e, k=k: e.tensor_scalar(out=OH[:, k, :], in0=iotaT[:], scalar1=eid4[:, k:k + 1], scalar2=None, op0=ALU.is_equal), reads=[b_rt, b_init], writes=[b_rt])
                    if k < 3:
                        P.op("dve", lambda e, k=k: e.scalar_tensor_tensor(out=Lw, in0=OH[:, k, :], scalar=-1.0e30, in1=Lw, op0=ALU.mult, op1=ALU.add), reads=[b_rt], writes=[b_rt])
                P.op("dve", lambda e: e.reduce_sum(out=M, in_=OH.rearrange("p k e -> p e k"), axis=AX.X), reads=[b_rt], writes=[b_lg])
                P.op("act", lambda e, i=i: e.activation(out=Mv[:, i, :], in_=M, func=AF.Copy), reads=[b_lg], writes=[b_M])
                P.op("dve", lambda e: e.tensor_scalar_mul(out=mx8[:, 8:9], in0=mx8[:, 0:1], scalar1=-1.0), reads=[b_lg], writes=[b_lg])
                P.op("act", lambda e: e.activation(out=E, in_=L, func=AF.Exp, bias=mx8[:, 8:9], scale=1.0), reads=[b_lg], writes=[b_lg])
                P.op("act", lambda e: e.activation(out=e4, in_=mx8[:, 0:4], func=AF.Exp, bias=mx8[:, 8:9], scale=1.0), reads=[b_lg], writes=[b_rt])
                P.op("dve", lambda e: e.tensor_tensor(out=E, in0=E, in1=M, op=ALU.mult), reads=[b_lg], writes=[b_lg])
                P.op("dve", lambda e: e.reduce_sum(out=mx8[:, 9:10], in_=E, axis=AX.X), reads=[b_lg], writes=[b_lg])
                P.op("dve", lambda e: e.reciprocal(out=mx8[:, 10:11], in_=mx8[:, 9:10]), reads=[b_lg], writes=[b_lg])
                P.op("dve", lambda e: e.tensor_scalar(out=G, in0=E, scalar1=mx8[:, 10:11], scalar2=None, op0=ALU.mult), reads=[b_lg], writes=[b_lg])
                P.op("dve", lambda e, i=i: e.tensor_scalar(out=gates4[:, 4 * i:4 * i + 4], in0=e4, scalar1=mx8[:, 10:11], scalar2=None, op0=ALU.mult), reads=[b_lg, b_rt], writes=[b_gatesl[i]])
                P.op("pe", lambda e: e.transpose(out=ps[6][0:NE, 0:128], in_=G, identity=ident[:]), reads=[b_lg, b_init], writes=[bps[6]])
                P.op("act", lambda e, tsl=tsl: e.activation(out=GTb[:, tsl], in_=ps[6][0:NE, 0:128], func=AF.Copy), reads=[bps[6]], writes=[b_GT])
                for i2 in range(i + 1):
                    lhs = trib if i2 == i else onesb
                    P.op("pe", lambda e, i2=i2, i=i, lhs=lhs: e.matmul(out=ps[5][:, 0:NE], lhsT=lhs[:], rhs=Mv[:, i2, :], start=(i2 == 0), stop=(i2 == i)), reads=[b_M, b_c, b_init], writes=[bps[5]])
                P.op("pe", lambda e, i=i: e.matmul(out=ps[4][0:1, 0:NE], lhsT=onesb[:, 0:1], rhs=Mv[:, i, :], start=(i == 0), stop=(i == 15)), reads=[b_M, b_c], writes=[bps[4]])
                P.op("dve", lambda e: e.tensor_tensor(out=TB, in0=OH, in1=ps[5][:, 0:NE].unsqueeze(1).to_broadcast([128, 4, NE]), op=ALU.mult), reads=[b_rt, bps[5]], writes=[b_rt])
                P.op("dve", lambda e: e.reduce_sum(out=rank4, in_=TB, axis=AX.X), reads=[b_rt], writes=[b_rt])
                P.op("dve", lambda e: e.scalar_tensor_tensor(out=pos4, in0=eid4, scalar=float(S), in1=rank4, op0=ALU.mult, op1=ALU.add), reads=[b_rt], writes=[b_rt])
                P.op("dve", lambda e, i=i: e.tensor_copy(out=posI[:, 4 * i:4 * i + 4], in_=pos4), reads=[b_rt], writes=[b_posl[i]])
            P.op("dve", lambda e: e.tensor_copy(out=cntF[:], in_=ps[4][0:1, 0:NE]), reads=[bps[4]], writes=[b_cnt])
            P.op("dve", lambda e: e.tensor_copy(out=cntI[:], in_=cntF[:]), reads=[b_cnt], writes=[b_cnt])
            for tb in range(NBLK):
                for d in range(8):
                    bank = 2 + d % 2
                    P.op("pe", lambda e, d=d, tb=tb, bank=bank: e.matmul(out=ps[bank][:], lhsT=bdn_s[:, l * D + d * 128:l * D + (d + 1) * 128], rhs=GTb[:, blk(tb)], start=True, stop=True), reads=[b_init, b_GT], writes=[bps[bank]])
                    P.op("dve", lambda e, d=d, tb=tb, bank=bank: e.scalar_tensor_tensor(out=xT[:, d, blk(tb)], in0=ps[bank][:], scalar=mcol(l, 40, d), in1=xT[:, d, blk(tb)], op0=ALU.mult, op1=ALU.add),
                         reads=[bps[bank], b_mods, bxT[tb]], writes=[bxT[tb]])
            htok = [R_a[:, 0:512].bitcast(BF16), R_a[:, 512:1024].bitcast(BF16)]
            b_htok = [Buf("htok0"), Buf("htok1")]
            actT = [R_a[:, 1024:1536].bitcast(BF16).rearrange("p (f s) -> p f s", f=8), R_a[:, 1536:2048].bitcast(BF16).rearrange("p (f s) -> p f s", f=8)]
            b_actT = [Buf("actT0"), Buf("actT1")]
            claim("Ra", b_htok + b_actT)
            for i in range(16):
                sl = i % 2
                bank = sl
                pv = ps[bank][:].bitcast(BF16)
                for c in range(8):
                    P.op("pe", lambda e, c=c, i=i, pv=pv: e.transpose(out=pv[:, c * 128:(c + 1) * 128], in_=hT[:, c, i * 128:(i + 1) * 128], identity=identb[:]), reads=[b_hT[i // 4], b_c], writes=[bps[bank]])
                P.op("act", lambda e, sl=sl, pv=pv: e.activation(out=htok[sl], in_=pv, func=AF.Copy), reads=[bps[bank]], writes=[b_htok[sl]])
                for k in range(4):
                    col = 4 * i + k
                    P.dma("pool", None, None, f"xsc{sl}", reads=[b_htok[sl], b_posl[i]], writes=[b_XS[sl * 4 + k]],
                          fn=lambda e, sl=sl, col=col: e.indirect_dma_start(out=xs_d[:, :], out_offset=bass.IndirectOffsetOnAxis(ap=posI[:, col:col + 1], axis=0), in_=htok[sl], in_offset=None))
            ring = [R_w[:, 0:4096], R_w[:, 4096:8192], R_w[:, 8192:12288], R_h[:, 0:4096], R_h[:, 4096:8192]]
            b_ring = [Buf(f"ring{q}") for q in range(5)]
            claim("Rw", b_ring[0:3])
            xsb = R_s[:, 0:512].bitcast(BF16)
            xsT = [R_s[:, 512:1024].bitcast(BF16).rearrange("p (k s) -> p k s", k=8), R_s[:, 1024:1536].bitcast(BF16).rearrange("p (k s) -> p k s", k=8)]
            act_tt = [R_s[:, 1536:2048].bitcast(BF16), R_s[:, 2048:2560].bitcast(BF16)]
            b_xsb, b_xsT, b_acttt = Buf("xsb"), [Buf("xsT0"), Buf("xsT1")], [Buf("act_t0"), Buf("act_t1")]
            ys = [R_m[:, 0:1024], R_m[:, 1024:2048]]
            b_ys = [Buf("ys0"), Buf("ys1")]
            bgu_row = [R_m[0:1, 2048:3072].bitcast(BF16), R_m[0:1, 3072:4096].bitcast(BF16)]
            b_bgu = [Buf("bgu0"), Buf("bgu1")]
            gc, sg_, ln_, t1 = R_m[:, 4096:4608], R_m[:, 4608:5120], R_m[:, 5120:5632], R_m[:, 5632:6144]
            b_gc, b_sg2, b_ln, b_t1 = Buf("gc"), Buf("sg2"), Buf("ln"), Buf("t1")
            ring_claimed_h = [False]

            def issue_piece(e_, p):
                q = (3 * e_ + p) % 5
                if q >= 3 and not ring_claimed_h[0]:
                    claim("Rh", b_ring[3:5])
                    ring_claimed_h[0] = True
                wv = ring[q].bitcast(BF16).rearrange("p (k n) -> p k n", k=8)
                if p == 0:
                    src = wgu_d[l, e_, :, 0:D]
                elif p == 1:
                    src = wgu_d[l, e_, :, D:2 * D]
                else:
                    src = wdn_d[l, e_, :, :]
                P.dma("pool", wv, src.rearrange("(k p) n -> p k n", p=128), f"rg{q}", writes=[b_ring[q]])
                return q, wv

            def issue_bias(e_):
                P.dma("pool", bgu_row[e_ % 2], bgu_d[l, e_:e_ + 1, :], f"bgu{e_ % 2}", writes=[b_bgu[e_ % 2]])

            claim("Rs", [b_xsb, b_xsT[0], b_xsT[1]] + b_acttt)
            claim("Rm", b_ys + b_bgu + [b_gc, b_sg2, b_ln, b_t1])
            pieces = {}
            pieces[(0, 0)] = issue_piece(0, 0)
            pieces[(0, 1)] = issue_piece(0, 1)
            pieces[(0, 2)] = issue_piece(0, 2)
            issue_bias(0)
            bcnt = [0]
            for e_ in range(NE):
                if e_ + 1 < NE:
                    pieces[(e_ + 1, 0)] = issue_piece(e_ + 1, 0)
                    pieces[(e_ + 1, 1)] = issue_piece(e_ + 1, 1)
                    issue_bias(e_ + 1)
                (qg, wg), (ql, wl), (qd, wd) = pieces[(e_, 0)], pieces[(e_, 1)], pieces[(e_, 2)]
                brow = bgu_row[e_ % 2]
                for eng in ("pe", "act", "dve", "sp"):
                    P.regload(eng, cntI[0:1, e_:e_ + 1], reads=[b_cnt])
                def stage_a(j, e_=e_, wg=wg, wl=wl, brow=brow, qg=qg, ql=ql):
                    pr = ((l, e_), j * 128)
                    s_ = j % 2
                    act_t, b_actt = act_tt[s_], b_acttt[s_]
                    r0 = e_ * S + j * 128
                    P.dma("sp", xsb, xs_d[r0:r0 + 128, :], "xsl", reads=b_XS, writes=[b_xsb], pred=pr)
                    pv = ps[0][:].bitcast(BF16)
                    for k in range(8):
                        P.op("pe", lambda e, k=k, pv=pv: e.transpose(out=pv[:, k * 128:(k + 1) * 128], in_=xsb[:, k * 128:(k + 1) * 128], identity=identb[:]), reads=[b_xsb, b_c], writes=[bps[0]], pred=pr)
                    P.op("dve", lambda e, s_=s_, pv=pv: e.tensor_copy(out=xsT[s_], in_=pv.rearrange("p (k s) -> p k s", k=8)), reads=[bps[0]], writes=[b_xsT[s_]], pred=pr)
                    for hf in range(2):
                        bg_, bl_ = 2 + 2 * hf, 3 + 2 * hf
                        fs = slice(hf * 512, (hf + 1) * 512)
                        for k in range(8):
                            P.op("pe", lambda e, k=k, s_=s_, bg_=bg_, fs=fs: e.matmul(out=ps[bg_][:], lhsT=xsT[s_][:, k, :], rhs=wg[:, k, fs], start=(k == 0), stop=False), reads=[b_ring[qg], b_xsT[s_]], writes=[bps[bg_]], pred=pr)
                        P.op("pe", lambda e, bg_=bg_, hf=hf: e.matmul(out=ps[bg_][:], lhsT=onesb[0:1, :], rhs=brow[0:1, hf * 512:(hf + 1) * 512], start=False, stop=True), reads=[b_bgu[e_ % 2], b_c], writes=[bps[bg_]], pred=pr)
                        for k in range(8):
                            P.op("pe", lambda e, k=k, s_=s_, bl_=bl_, fs=fs: e.matmul(out=ps[bl_][:], lhsT=xsT[s_][:, k, :], rhs=wl[:, k, fs], start=(k == 0), stop=False), reads=[b_ring[ql], b_xsT[s_]], writes=[bps[bl_]], pred=pr)
                        P.op("pe", lambda e, bl_=bl_, hf=hf: e.matmul(out=ps[bl_][:], lhsT=onesb[0:1, :], rhs=brow[0:1, D + hf * 512:D + (hf + 1) * 512], start=False, stop=True), reads=[b_bgu[e_ % 2], b_c], writes=[bps[bl_]], pred=pr)
                        P.op("dve", lambda e, bg_=bg_: e.tensor_scalar_min(out=gc, in0=ps[bg_][:], scalar1=7.0), reads=[bps[bg_]], writes=[b_gc], pred=pr)
                        P.op("act", lambda e: e.activation(out=sg_, in_=gc, func=AF.Sigmoid, scale=1.702), reads=[b_gc], writes=[b_sg2], pred=pr)
                        P.op("dve", lambda e, bl_=bl_: e.tensor_scalar(out=ln_, in0=ps[bl_][:], scalar1=1.0, scalar2=8.0, op0=ALU.add, op1=ALU.min), reads=[bps[bl_]], writes=[b_ln], pred=pr)
                        P.op("dve", lambda e: e.tensor_tensor(out=t1, in0=gc, in1=sg_, op=ALU.mult), reads=[b_gc, b_sg2], writes=[b_t1], pred=pr)
                        P.op("dve", lambda e, fs=fs, act_t=act_t: e.scalar_tensor_tensor(out=act_t[:, fs], in0=ln_, scalar=-6.0, in1=t1, op0=ALU.max, op1=ALU.mult), reads=[b_ln, b_t1], writes=[b_actt], pred=pr)

                def stage_b(j, e_=e_, wd=wd, qd=qd):
                    pr = ((l, e_), j * 128)
                    s_ = j % 2
                    act_t, b_actt = act_tt[s_], b_acttt[s_]
                    r0 = e_ * S + j * 128
                    pv1 = ps[1][:].bitcast(BF16)
                    for f in range(8):
                        P.op("pe", lambda e, f=f, pv1=pv1, act_t=act_t: e.transpose(out=pv1[:, f * 128:(f + 1) * 128], in_=act_t[:, f * 128:(f + 1) * 128], identity=identb[:]), reads=[b_actt, b_c], writes=[bps[1]], pred=pr)
                    P.op("dve", lambda e, s_=s_, pv1=pv1: e.tensor_copy(out=actT[s_], in_=pv1.rearrange("p (f s) -> p f s", f=8)), reads=[bps[1]], writes=[b_actT[s_]], pred=pr)
                    for n in range(2):
                        for f in range(8):
                            P.op("pe", lambda e, f=f, n=n, s_=s_: e.matmul(out=ps[6 + n][:], lhsT=actT[s_][:, f, :], rhs=wd[:, f, n * 512:(n + 1) * 512], start=(f == 0), stop=(f == 7)), reads=[b_ring[qd], b_actT[s_]], writes=[bps[6 + n]], pred=pr)
                        P.op("dve", lambda e, n=n, s_=s_: e.tensor_copy(out=ys[s_][:, n * 512:(n + 1) * 512], in_=ps[6 + n][:]), reads=[bps[6 + n]], writes=[b_ys[s_]], pred=pr)
                    P.dma("sp", ys_d[r0:r0 + 128, :], ys[s_], "yst", reads=[b_ys[s_]], writes=[b_YS[s_]], pred=pr)

                stage_a(0)
                for j in range(16):
                    if j + 1 < 16:
                        stage_a(j + 1)
                    stage_b(j)
                if e_ + 1 < NE:
                    pieces[(e_ + 1, 2)] = issue_piece(e_ + 1, 2)
            yb = [[R_w[:, (s2 * 4 + k) * 1024:(s2 * 4 + k + 1) * 1024] for k in range(4)] for s2 in range(2)]
            acc = [R_w[:, 8192:9216], R_w[:, 9216:10240]]
            b_yb = [[Buf(f"yb{s2}{k}") for k in range(4)] for s2 in range(2)]
            b_acc = [Buf("acc0"), Buf("acc1")]
            claim("Rw", b_yb[0] + b_yb[1] + b_acc)
            for i in range(16):
                s2 = i % 2
                for k in range(4):
                    col = 4 * i + k
                    P.dma("pool", None, None, f"yg{s2}", reads=b_YS + [b_posl[i]], writes=[b_yb[s2][k]],
                          fn=lambda e, s2=s2, k=k, col=col: e.indirect_dma_start(out=yb[s2][k], out_offset=None, in_=ys_d[:, :], in_offset=bass.IndirectOffsetOnAxis(ap=posI[:, col:col + 1], axis=0)))
                P.op("dve", lambda e, s2=s2, i=i: e.tensor_scalar(out=acc[s2], in0=yb[s2][0], scalar1=gates4[:, 4 * i:4 * i + 1], scalar2=None, op0=ALU.mult), reads=[b_yb[s2][0], b_gatesl[i]], writes=[b_acc[s2]])
                for k in range(1, 4):
                    P.op("dve", lambda e, s2=s2, i=i, k=k: e.scalar_tensor_tensor(out=acc[s2], in0=yb[s2][k], scalar=gates4[:, 4 * i + k:4 * i + k + 1], in1=acc[s2], op0=ALU.mult, op1=ALU.add), reads=[b_yb[s2][k], b_gatesl[i], b_acc[s2]], writes=[b_acc[s2]])
                for g in range(2):
                    bank = 2 * s2 + g
                    for cc in range(4):
                        d = g * 4 + cc
                        P.op("pe", lambda e, d=d, cc=cc, s2=s2, bank=bank: e.transpose(out=ps[bank][:, cc * 128:(cc + 1) * 128], in_=acc[s2][:, d * 128:(d + 1) * 128], identity=ident[:]), reads=[b_acc[s2], b_init], writes=[bps[bank]])
                    for cc in range(4):
                        d = g * 4 + cc
                        P.op("dve", lambda e, d=d, cc=cc, i=i, bank=bank: e.scalar_tensor_tensor(out=xT[:, d, i * 128:(i + 1) * 128], in0=ps[bank][:, cc * 128:(cc + 1) * 128], scalar=mcol(l, 40, d), in1=xT[:, d, i * 128:(i + 1) * 128], op0=ALU.mult, op1=ALU.add),
                             reads=[bps[bank], b_mods, bxT[i // 4]], writes=[bxT[i // 4]])
            pg = PT["post_g"] + (l * 2 + 1) * 8
            pb = PT["post_b"] + (l * 2 + 1) * 8
            claim("Rs", [b_sq[0], b_sq[1], b_stat])
            for tb in range(NBLK):
                layer_norm_block(lambda c, tb=tb: xT[:, c, blk(tb)], bxT[tb], lambda c, tb=tb: xT[:, c, blk(tb)], bxT[tb], pg, pb, AF.Identity, 4, 5)

        moe = moe_sparse if SPARSE else moe_sublayer
        phases = [("conv", conv_sublayer), ("moe0", lambda: moe(0)), ("attn", attn_sublayer), ("moe1", lambda: moe(1))]
        for name, fn in phases:
            fn()
            if stop_after == name:
                break

        b_stage = [Buf("ostage0"), Buf("ostage1")]
        claim("Ra", b_stage)
        for i in range(16):
            sl = i % 2
            sv = stage[:, sl * D:(sl + 1) * D]
            for g in range(2):
                bank = 2 + g
                for cc in range(4):
                    c = g * 4 + cc
                    P.op("pe", lambda e, c=c, cc=cc, i=i, bank=bank: e.transpose(out=ps[bank][:, cc * 128:(cc + 1) * 128], in_=xT[:, c, i * 128:(i + 1) * 128], identity=ident[:]),
                         reads=[bxT[i // 4], b_init], writes=[bps[bank]])
                if g == 0:
                    P.op("act", lambda e, sv=sv, bank=bank: e.activation(out=sv[:, 0:512], in_=ps[bank][:], func=AF.Copy), reads=[bps[bank]], writes=[b_stage[sl]])
                else:
                    P.op("dve", lambda e, sv=sv, bank=bank: e.tensor_copy(out=sv[:, 512:1024], in_=ps[bank][:]), reads=[bps[bank]], writes=[b_stage[sl]])
            P.dma("sp", out_d[i * 128:(i + 1) * 128, :], sv, "out", reads=[b_stage[sl]])
        P.final_wait("sp", ["out"])
        P.emit()
    return nc


_CACHE = {}


def _t5_bucket(rel):
    nb = 16
    ret = np.where(rel > 0, nb, 0)
    n = np.abs(rel)
    max_exact = 8
    nf = np.maximum(n, 1).astype(np.float32) / np.float32(max_exact)
    large = max_exact + (np.log(nf) / np.float32(math.log(128 / max_exact)) * np.float32(nb - max_exact)).astype(np.int32)
    large = np.minimum(large, nb - 1)
    return ret + np.where(n < max_exact, n, large)


def _attn_consts():
    rel = 511 - np.arange(RG)
    bk = _t5_bucket(rel)
    ohg = np.zeros((32, RG), np.float32)
    ohg[bk, np.arange(RG)] = 1.0
    ohg[15, :] -= 1.0
    masks = np.zeros((4, 128, BLK), np.float32)
    kl = np.arange(128)[:, None] // 64
    ql = np.arange(BLK)[None, :] // 64
    for o in range(4):
        masks[o] = np.where(kl - ql <= -(o * 128) // 64, 0.0, NEG)
    return ohg, masks


def make_in_maps(inp):
    f = lambda a: np.ascontiguousarray(np.asarray(a, np.float32))
    pt = build_pt(inp)
    shared = {
        "pt": pt,
        "ident": np.eye(128, dtype=np.float32),
        "ada_w": f(inp["ada_w"]),
        "w_pw1": f(inp["conv_w_pw1"][0]),
        "w_pw2": f(inp["conv_w_pw2"][0]),
        "w_r": f(inp["router_w"]),
        "b_r": f(inp["router_b"]).reshape(2, 1, NE),
        "w_gu": f(inp["expert_w_gate_up"]),
        "w_dn": f(inp["expert_w_down"]),
        "b_dn": f(inp["expert_b_down"]),
        "w_kv": f(inp["w_kv"]),
        "w_q": f(inp["attn_w_q"][0]),
        "w_o": f(inp["attn_w_o"][0]),
        "lpt": np.ascontiguousarray(f(inp["attn_lambda"][0]).T),
        "tabs": f(inp["rel_bias_table"]),
        "b_gu": f(inp["expert_b_gate_up"]),
        "tri": np.triu(np.ones((128, 128), np.float32), 1),
        "iota": np.tile(np.arange(NE, dtype=np.float32)[None, :], (128, 1)),
        "ohg": _attn_consts()[0],
        "masks": _attn_consts()[1],
    }
    maps = []
    for b in range(8):
        m = dict(shared)
        m["x"] = f(inp["x"][b])
        m["ct"] = np.ascontiguousarray(f(inp["c"][b]).reshape(8, 128).T)
        maps.append(m)
    return maps


def kernel(**inputs):
    if "nc" not in _CACHE:
        _CACHE["nc"] = build()
    nc = _CACHE["nc"]
    maps = make_in_maps(inputs)
    res = run_bass_kernel_spmd(nc, maps, core_ids=list(range(8)))
    return np.stack([np.asarray(r["out"], np.float32) for r in res.results], axis=0)
```

```python
import math
import numpy as np
import concourse.bass as bass
import concourse.mybir as mybir
from concourse.bass_utils import run_bass_kernel_spmd
from contextlib import ExitStack

F32 = mybir.dt.float32
BF16 = mybir.dt.bfloat16
I32 = mybir.dt.int32
SPARSE = True
AF = mybir.ActivationFunctionType
ALU = mybir.AluOpType
AX = mybir.AxisListType

SAME_ENG_SYNC = True

D = 1024
S = 2048
NE = 32
ALPHA = 4.0 ** 0.25
LN_EPS = 1e-5
BLK = 512
RG = 1152
LAMI = 0.8 - 0.6 * math.exp(-0.3 * 1)
SM_SHIFT = 20.0
NEG = -30000.0
NBLK = S // BLK


class Buf:
    __slots__ = ("name", "w", "r", "rd")

    def __init__(self, name):
        self.name = name
        self.w = None
        self.r = {}
        self.rd = []


class Ins:
    __slots__ = ("eng", "fn", "kind", "cdeps", "dwaits", "sig", "sigidx", "pos", "dsem", "pred", "dord")

    def __init__(self, eng, fn, kind, dsem=None):
        self.pred = None
        self.dord = 0
        self.eng = eng
        self.fn = fn
        self.kind = kind
        self.cdeps = {}
        self.dwaits = {}
        self.sig = False
        self.sigidx = None
        self.pos = None
        self.dsem = dsem


class Prog:
    ENGS = ("pe", "act", "dve", "pool", "sp")

    def __init__(self, nc):
        self.nc = nc
        self.streams = {e: [] for e in self.ENGS}
        self.dma_count = {}
        self.regs = {}

    def _dep(self, ins, p):
        if p is None or p is ins:
            return
        if p.kind == "d":
            s = p.dsem
            ins.dwaits[s] = max(ins.dwaits.get(s, 0), self.dma_count[s])
            return
        if ins.kind == "c" and p.eng == ins.eng:
            if ins.eng == "pe" or not SAME_ENG_SYNC:
                return
        cur = ins.cdeps.get(p.eng)
        if cur is None or cur.pos < p.pos:
            ins.cdeps[p.eng] = p

    def _add(self, ins, reads, writes):
        for b in reads:
            self._dep(ins, b.w)
        for b in writes:
            self._dep(ins, b.w)
            for r in b.r.values():
                self._dep(ins, r)
            for r in b.rd:
                self._dep(ins, r)
        ins.pos = len(self.streams[ins.eng])
        self.streams[ins.eng].append(ins)
        for b in reads:
            if ins.kind == "c":
                b.r[ins.eng] = ins
            else:
                b.rd.append(ins)
        for b in writes:
            b.w = ins
            b.r = {}
            b.rd = []
        return ins

    def op(self, eng, fn, reads=(), writes=(), pred=None):
        ins = Ins(eng, fn, "c")
        ins.pred = pred
        return self._add(ins, reads, writes)

    def dma(self, eng, out, in_, sem, reads=(), writes=(), pred=None, fn=None, **kw):
        self.dma_count.setdefault(sem, 0)
        if fn is None:
            fn = lambda e, out=out, in_=in_, kw=kw: e.dma_start(out=out, in_=in_, **kw)
        ins = Ins(eng, fn, "d", dsem=sem)
        ins.pred = pred
        ins.dord = self.dma_count[sem]
        self._add(ins, reads, writes)
        self.dma_count[sem] += 1
        return ins

    def regload(self, eng, ap, reads=()):
        return self.op(eng, lambda e, ap=ap, eng=eng: e.reg_load(self.regs[eng], ap), reads=reads)

    def final_wait(self, eng, sems):
        ins = Ins(eng, None, "c")
        for s in sems:
            ins.dwaits[s] = self.dma_count[s]
        ins.pos = len(self.streams[eng])
        self.streams[eng].append(ins)

    def emit(self):
        nc = self.nc
        for e in self.ENGS:
            for ins in self.streams[e]:
                for p in ins.cdeps.values():
                    p.sig = True
        for e in self.ENGS:
            n = 0
            for ins in self.streams[e]:
                if ins.sig:
                    n += 1
                    ins.sigidx = n
        with ExitStack() as st:
            csem = {e: st.enter_context(nc.semaphore("c_" + e)) for e in self.ENGS}
            dsem = {s: st.enter_context(nc.semaphore("d_" + s)) for s in self.dma_count}
            block = st.enter_context(nc.Block())

            eobj = {"pe": nc.tensor, "act": nc.scalar, "dve": nc.vector, "pool": nc.gpsimd, "sp": nc.sync}
            for e in self.ENGS:
                self.regs[e] = st.enter_context(eobj[e].register("pr_" + e))

            def run(engname):
                def body(eng):
                    waited = {}

                    def emit_one(ins):
                        for pe_, p in ins.cdeps.items():
                            key = ("c", pe_)
                            if waited.get(key, 0) < p.sigidx:
                                eng.wait_ge(csem[pe_], p.sigidx)
                                waited[key] = p.sigidx
                        for s_, cnt in ins.dwaits.items():
                            key = ("d", s_)
                            if waited.get(key, 0) < cnt:
                                eng.wait_ge(dsem[s_], 16 * cnt)
                                waited[key] = cnt
                        if ins.fn is None:
                            return
                        bi = ins.fn(eng)
                        if ins.kind == "d":
                            bi.then_inc(dsem[ins.dsem], 16)
                        elif ins.sig:
                            bi.then_inc(csem[engname], 1)

                    def body_of(g):
                        for x in g:
                            emit_one(x)

                    def balance(groups):
                        nsig = 0
                        dincs = {}
                        for g in groups:
                            for x in g:
                                if x.kind == "c":
                                    if x.sig:
                                        nsig += 1
                                else:
                                    first, n = dincs.get(x.dsem, (x.dord, 0))
                                    dincs[x.dsem] = (min(first, x.dord), n + 1)
                        if nsig:
                            eng.drain().then_inc(csem[engname], nsig)
                        for s_, (first, n) in dincs.items():
                            if first > 0:
                                eng.wait_ge(dsem[s_], 16 * first)
                            eng.sem_inc(dsem[s_], 16 * n)

                    def emit_seq(groups, lo):
                        i = 0
                        while i < len(groups):
                            g = groups[i]
                            thr = g[0].pred[1]
                            if thr <= lo:
                                body_of(g)
                                i += 1
                                continue
                            k = i
                            while k < len(groups) and groups[k][0].pred[1] >= thr:
                                k += 1
                            run_ = groups[i:k]
                            snap = dict(waited)
                            with eng.If_lt(self.regs[engname], thr + 1):
                                balance(run_)
                            with eng.Else():
                                emit_seq(run_, thr)
                            waited.clear()
                            waited.update(snap)
                            i = k

                    region = []
                    for ins in self.streams[engname]:
                        if ins.pred is None:
                            if region:
                                emit_seq(region, -1)
                                region = []
                            emit_one(ins)
                        else:
                            if region and region[0][0].pred[0] != ins.pred[0]:
                                emit_seq(region, -1)
                                region = []
                            if region and region[-1][0].pred == ins.pred:
                                region[-1].append(ins)
                            else:
                                region.append([ins])
                    if region:
                        emit_seq(region, -1)
                return body

            block.tensor(run("pe"))
            block.scalar(run("act"))
            block.vector(run("dve"))
            block.gpsimd(run("pool"))
            block.sync(run("sp"))


PT = {}
_off = 0


def _pt(name, n):
    global _off
    PT[name] = _off
    _off += n


_pt("ada_b", 96)
_pt("post_g", 32)
_pt("post_b", 32)
_pt("b_pw1", 16)
_pt("w_dw", 248)
_pt("b_dw", 8)
_pt("cln_g", 8)
_pt("cln_b", 8)
_pt("b_pw2", 8)
_pt("b_gu", 1024)
_pt("subln", 8)
NPT = _off


def _cols(v):
    v = np.asarray(v, np.float32).reshape(-1, 128)
    return np.ascontiguousarray(v.T)


def build_pt(inp):
    pt = np.zeros((128, NPT), np.float32)

    def put(name, arr):
        pt[:, PT[name]:PT[name] + arr.shape[1]] = arr

    put("ada_b", np.concatenate([_cols(inp["ada_b"][l]) for l in range(2)], axis=1))
    put("post_g", np.concatenate([_cols(inp["post_ln_g"][l, s]) for l in range(2) for s in range(2)], axis=1))
    put("post_b", np.concatenate([_cols(inp["post_ln_b"][l, s]) for l in range(2) for s in range(2)], axis=1))
    put("b_pw1", _cols(inp["conv_b_pw1"][0]))
    wdw = np.asarray(inp["conv_w_dw"][0], np.float32)
    put("w_dw", np.ascontiguousarray(wdw.reshape(31, 8, 128).transpose(2, 1, 0).reshape(128, 248)))
    put("b_dw", _cols(inp["conv_b_dw"][0]))
    put("cln_g", _cols(inp["conv_ln_g"][0]))
    put("cln_b", _cols(inp["conv_ln_b"][0]))
    put("b_pw2", _cols(inp["conv_b_pw2"][0]))
    put("b_gu", _cols(np.asarray(inp["expert_b_gate_up"], np.float32).reshape(-1)))
    put("subln", np.tile(np.asarray(inp["attn_subln_g"][0], np.float32).reshape(128, 1), (1, 8)))
    return pt


def build(stop_after=None):
    nc = bass.Bass("TRN2", target_bir_lowering=False)

    def din(name, shape, dt=F32):
        return nc.dram_tensor(name, shape, dt, kind="ExternalInput").ap()

    x_d = din("x", [S, D])
    ct_d = din("ct", [128, 8])
    pt_d = din("pt", [128, NPT])
    ident_d = din("ident", [128, 128])
    ada_w_d = din("ada_w", [2, D, 6 * D])
    wpw1_d = din("w_pw1", [D, 2 * D])
    wpw2_d = din("w_pw2", [D, D])
    wr_d = din("w_r", [2, D, NE])
    br_d = din("b_r", [2, 1, NE])
    wgu_d = din("w_gu", [2, NE, D, 2 * D])
    wdn_d = din("w_dn", [2, NE, D, D])
    bdn_d = din("b_dn", [2, NE, D])
    wkv_d = din("w_kv", [D, 2 * D])
    wq_d = din("w_q", [D, D])
    wo_d = din("w_o", [D, D])
    lpt_d = din("lpt", [64, 4])
    tabs_d = din("tabs", [32, 8])
    ohg_d = din("ohg", [32, RG])
    masks_d = din("masks", [4, 128, BLK])
    bgu_d = din("b_gu", [2, NE, 2 * D])
    tri_d = din("tri", [128, 128])
    iota_d = din("iota", [128, NE])
    xs_d = nc.dram_tensor("xs_scratch", [NE * S, D], BF16).ap()
    ys_d = nc.dram_tensor("ys_scratch", [NE * S, D], F32).ap()
    gs_t = nc.dram_tensor("gs_scratch", [8, 128, RG], F32)
    gs_d = gs_t.ap()
    out_d = nc.dram_tensor("out", [S, D], F32, kind="ExternalOutput").ap()

    P = Prog(nc)
    with ExitStack() as st:
        def T(name, shape, dt=F32):
            return st.enter_context(nc.sbuf_tensor("s_" + name, shape, dt))

        region = {}

        def claim(name, new_bufs):
            old = [b for b in region.get(name, []) if b not in new_bufs]
            for nb in new_bufs:
                for ob in old:
                    cands = list(ob.r.values()) + ([ob.w] if ob.w is not None else [])
                    for p in cands:
                        if p.kind == "d":
                            nb.rd.append(p)
                        else:
                            cur = nb.r.get(p.eng)
                            if cur is None or cur.pos < p.pos:
                                nb.r[p.eng] = p
                    nb.rd.extend(ob.rd)
            region[name] = list(new_bufs)

        R_x = T("R_x", [128, 8 * S])
        R_h = T("R_h", [128, 8192])
        R_w = T("R_w", [128, 12288])
        R_a = T("R_a", [128, 2048])
        R_s = T("R_s", [128, 5 * BLK])
        R_m = T("R_m", [128, 6144])
        GT = R_m[0:32, 0:2048]
        gbT = R_m[:, 2048:4096]
        GTb = R_m[0:32, 4096:5120].bitcast(BF16)
        gsel = R_m[0:32, 5120:6144]
        lpT = T("lpT", [64, 4])
        tabs = T("tabs", [32, 8])
        Mall = T("Mall", [128, 16 * NE], BF16)
        posI = T("posI", [128, 64], I32)
        gates4 = T("gates4", [128, 64])
        cntF = T("cntF", [1, NE])
        cntI = T("cntI", [1, NE], I32)
        trib = T("trib", [128, 128], BF16)
        identb = T("identb", [128, 128], BF16)
        iotaT = T("iotaT", [128, NE])
        iotaP = T("iotaP", [128, NE])
        rt = T("rt", [128, 3 * 128 + 16])
        nlam = T("nlam", [128, 4])
        negc = T("negc", [128, 1])
        gsub = T("gsub", [128, 1])
        pt = T("pt", [128, NPT])
        ident = T("ident", [128, 128])
        ones = T("ones", [128, 128])
        onesb = T("onesb", [128, 128], BF16)
        cT = T("cT", [128, 8])
        condb = T("condb", [128, 8], BF16)
        mods = T("mods", [128, 96])
        g1b = T("g1b", [128, 8])
        epsT = T("epsT", [128, 1])
        stage = R_a
        wr_s = T("wr_s", [128, 2 * 8 * NE], BF16)
        br_s = T("br_s", [1, 2 * NE], BF16)
        bdn_s = T("bdn_s", [32, 2 * D], BF16)
        lg = T("lg", [128, 8 * NE])
        mx8 = T("mx8", [128, 16])

        ps = [st.enter_context(nc.psum_tensor(f"ps{i}", [128, 512], F32)) for i in range(8)]
        bps = [Buf(f"ps{i}") for i in range(8)]

        xT = R_x[:].rearrange("p (c t) -> p c t", c=8)
        bxT = [Buf(f"xT{b}") for b in range(NBLK)]
        b_init = Buf("init")
        b_mods = Buf("mods")

        def blk(tb):
            return slice(tb * BLK, (tb + 1) * BLK)

        P.dma("sp", pt[:], pt_d[:, :], "init", writes=[b_init])
        P.dma("sp", ident[:], ident_d[:, :], "init", writes=[b_init])
        P.dma("sp", cT[:], ct_d[:, :], "init", writes=[b_init])
        P.dma("pool", wr_s[:].rearrange("p (l k e) -> p l k e", l=2, k=8), wr_d.rearrange("l (k p) e -> p l k e", p=128), "initp", writes=[b_init])
        P.dma("pool", br_s[:], br_d.rearrange("l o e -> o (l e)"), "initp", writes=[b_init])
        P.dma("pool", bdn_s[:].rearrange("e (l d) -> e l d", l=2), bdn_d.rearrange("l e d -> e l d"), "initp", writes=[b_init])
        b_c = Buf("consts")
        P.op("dve", lambda e: e.memset(ones[:], 1.0), writes=[b_c])
        P.op("dve", lambda e: e.memset(onesb[:], 1.0), writes=[b_c])
        P.op("dve", lambda e: e.memset(epsT[:], LN_EPS), writes=[b_c])
        P.op("dve", lambda e: e.memset(negc[:], -SM_SHIFT), writes=[b_c])
        P.dma("sp", lpT[:], lpt_d[:, :], "init", writes=[b_init])
        P.dma("sp", tabs[:], tabs_d[:, :], "init", writes=[b_init])
        P.dma("pool", trib[:], tri_d[:, :], "initp", writes=[b_init])
        P.dma("sp", iotaT[:], iota_d[:, :], "init", writes=[b_init])
        blin = pt[:, PT["b_gu"]:PT["b_gu"] + 1024].rearrange("p (g j) -> p g j", j=16)[:, :, 8:16]
        P.op("dve", lambda e: e.tensor_scalar_add(out=blin, in0=blin, scalar1=1.0), reads=[b_init], writes=[b_init])
        P.op("act", lambda e: e.activation(out=condb[:], in_=cT[:], func=AF.Silu), reads=[b_init], writes=[b_c])
        P.op("act", lambda e: e.activation(out=identb[:], in_=ident[:], func=AF.Copy), reads=[b_init], writes=[b_c])
        P.op("act", lambda e: e.activation(out=iotaP[:], in_=iotaT[:], func=AF.Identity, bias=1000.0, scale=1.0), reads=[b_init], writes=[b_c])

        units = []
        for u in range(2):
            base = u * 6144
            units.append(dict(
                raw=R_w[:, base:base + 6144],
                buf=Buf(f"unit{u}"), sem=f"wu{u}"))
        ucount = [0]
        claim("Rw", [u["buf"] for u in units])

        def next_unit():
            u = units[ucount[0] % 2]
            ucount[0] += 1
            return u

        for l in range(2):
            for nb in range(6):
                u = next_unit()
                wv = u["raw"][:, 0:4096].bitcast(BF16).rearrange("p (k n) -> p k n", k=8)
                P.dma("pool", wv, ada_w_d[l, :, nb * 1024:(nb + 1) * 1024].rearrange("(k p) n -> p k n", p=128), u["sem"], writes=[u["buf"]])
                for j in range(8):
                    col = nb * 8 + j
                    for k in range(8):
                        P.op("pe", lambda e, wv=wv, j=j, k=k, col=col, l=l: e.matmul(out=ps[l][:, col:col + 1], lhsT=wv[:, k, j * 128:(j + 1) * 128], rhs=condb[:, k:k + 1], start=(k == 0), stop=(k == 7)),
                             reads=[u["buf"], b_c], writes=[bps[l]])
            P.op("dve", lambda e, l=l: e.tensor_tensor(out=mods[:, l * 48:(l + 1) * 48], in0=ps[l][:, 0:48], in1=pt[:, PT["ada_b"] + l * 48:PT["ada_b"] + (l + 1) * 48], op=ALU.add),
                 reads=[bps[l], b_init], writes=[b_mods])
            for o in (8, 32):
                P.op("dve", lambda e, l=l, o=o: e.tensor_scalar_add(out=mods[:, l * 48 + o:l * 48 + o + 8], in0=mods[:, l * 48 + o:l * 48 + o + 8], scalar1=1.0),
                     reads=[b_mods], writes=[b_mods])
        P.op("dve", lambda e: e.tensor_tensor(out=g1b[:], in0=mods[:, 16:24], in1=pt[:, PT["b_pw2"]:PT["b_pw2"] + 8], op=ALU.mult), reads=[b_mods, b_init], writes=[b_mods])

        def mcol(l, o, c):
            return mods[:, l * 48 + o + c:l * 48 + o + c + 1]

        b_stage = [Buf("stage0"), Buf("stage1")]
        claim("Ra", b_stage)
        for i in range(16):
            sl = i % 2
            sv = stage[:, sl * D:(sl + 1) * D]
            P.dma("sp", sv, x_d[i * 128:(i + 1) * 128, :], f"xin{sl}", writes=[b_stage[sl]])
            for g in range(2):
                bank = 2 + g
                for cc in range(4):
                    c = g * 4 + cc
                    P.op("pe", lambda e, sv=sv, c=c, cc=cc, bank=bank: e.transpose(out=ps[bank][:, cc * 128:(cc + 1) * 128], in_=sv[:, c * 128:(c + 1) * 128], identity=ident[:]),
                         reads=[b_stage[sl], b_init], writes=[bps[bank]])
                eng = "act" if g == 0 else "dve"
                if eng == "act":
                    P.op("act", lambda e, g=g, i=i, bank=bank: e.activation(out=xT[:, g * 4:(g + 1) * 4, i * 128:(i + 1) * 128], in_=ps[bank][:].rearrange("p (c t) -> p c t", c=4), func=AF.Copy),
                         reads=[bps[bank]], writes=[bxT[i // 4]])
                else:
                    P.op("dve", lambda e, g=g, i=i, bank=bank: e.tensor_copy(out=xT[:, g * 4:(g + 1) * 4, i * 128:(i + 1) * 128], in_=ps[bank][:].rearrange("p (c t) -> p c t", c=4)),
                         reads=[bps[bank]], writes=[bxT[i // 4]])

        sq = [R_s[:, 0:BLK], R_s[:, BLK:2 * BLK]]
        b_sq = [Buf("sq0"), Buf("sq1")]
        mean_t = R_s[:, 2 * BLK:3 * BLK]
        rstd_t = R_s[:, 3 * BLK:4 * BLK]
        nmr_t = R_s[:, 4 * BLK:5 * BLK]
        b_stat = Buf("stat")
        sqc = [0]

        def _clone(b):
            nb = Buf(b.name + "_c")
            nb.w = b.w
            nb.r = dict(b.r)
            nb.rd = list(b.rd)
            return nb

        def layer_norm_block(src, b_src, dst, b_dst, gcol, bcol, func, bank_s, bank_q):
            inplace = b_src is b_dst
            cbuf = [_clone(b_src) for _ in range(8)]
            dbuf = cbuf if inplace else [_clone(b_dst) for _ in range(8)]
            for c in range(8):
                s = sqc[0] % 2
                sqc[0] += 1
                P.op("act", lambda e, c=c, s=s: e.activation(out=sq[s], in_=src(c), func=AF.Square), reads=[cbuf[c]], writes=[b_sq[s]])
                P.op("pe", lambda e, c=c: e.matmul(out=ps[bank_s][:], lhsT=ones[:], rhs=src(c), start=(c == 0), stop=(c == 7)), reads=[cbuf[c], b_c], writes=[bps[bank_s]])
                P.op("pe", lambda e, c=c, s=s: e.matmul(out=ps[bank_q][:], lhsT=ones[:], rhs=sq[s], start=(c == 0), stop=(c == 7)), reads=[b_sq[s], b_c], writes=[bps[bank_q]])
            P.op("dve", lambda e: e.tensor_scalar_mul(out=mean_t, in0=ps[bank_s][:], scalar1=1.0 / D), reads=[bps[bank_s]], writes=[b_stat])
            P.op("dve", lambda e: e.tensor_tensor(out=nmr_t, in0=mean_t, in1=mean_t, op=ALU.mult), reads=[b_stat], writes=[b_stat])
            P.op("dve", lambda e: e.scalar_tensor_tensor(out=rstd_t, in0=ps[bank_q][:], scalar=1.0 / D, in1=nmr_t, op0=ALU.mult, op1=ALU.subtract), reads=[bps[bank_q], b_stat], writes=[b_stat])
            P.op("act", lambda e: e.activation(out=rstd_t, in_=rstd_t, func=AF.Sqrt, bias=epsT[:, 0:1], scale=1.0), reads=[b_stat, b_c], writes=[b_stat])
            P.op("dve", lambda e: e.reciprocal(out=rstd_t, in_=rstd_t), reads=[b_stat], writes=[b_stat])
            P.op("dve", lambda e: e.scalar_tensor_tensor(out=nmr_t, in0=mean_t, scalar=-1.0, in1=rstd_t, op0=ALU.mult, op1=ALU.mult), reads=[b_stat], writes=[b_stat])
            last = {}
            for c in range(8):
                last["d1"] = P.op("dve", lambda e, c=c: e.tensor_tensor(out=src(c), in0=src(c), in1=rstd_t, op=ALU.mult), reads=[cbuf[c], b_stat], writes=[cbuf[c]])
                last["d2"] = P.op("dve", lambda e, c=c: e.tensor_tensor(out=src(c), in0=src(c), in1=nmr_t, op=ALU.add), reads=[cbuf[c], b_stat], writes=[cbuf[c]])
                last["a"] = P.op("act", lambda e, c=c: e.activation(out=dst(c), in_=src(c), func=func, scale=pt[:, gcol + c:gcol + c + 1], bias=pt[:, bcol + c:bcol + c + 1]),
                                 reads=[cbuf[c], b_init], writes=[dbuf[c]])
            if inplace:
                b_src.w = last["a"]
                b_src.r = {}
                b_src.rd = []
            else:
                b_src.w = last["d2"]
                b_src.r = {"act": last["a"]}
                b_src.rd = []
                b_dst.w = last["a"]
                b_dst.r = {}
                b_dst.rd = []

        def conv_sublayer():
            l = 0
            wpw1 = R_w[:, 0:8192].bitcast(BF16).rearrange("p (k n) -> p k n", k=8)
            wpw2 = R_w[:, 8192:12288].bitcast(BF16).rearrange("p (k n) -> p k n", k=8)
            b_w1 = [Buf(f"wpw1_{i}") for i in range(4)]
            b_w2 = Buf("wpw2")
            claim("Rw", b_w1 + [b_w2])
            for i in range(4):
                P.dma("pool", wpw1[:, :, i * 512:(i + 1) * 512], wpw1_d[:, i * 512:(i + 1) * 512].rearrange("(k p) n -> p k n", p=128), "wconv",
                      writes=[b_w1[i]])
            P.dma("pool", wpw2, wpw2_d.rearrange("(k p) n -> p k n", p=128), "wconv", writes=[b_w2])
            hblk = R_h[:, 0:2048].bitcast(BF16).rearrange("p (c t) -> p c t", c=8)
            sblk = R_h[:, 2048:4096].bitcast(BF16).rearrange("p (c t) -> p c t", c=8)
            vblk = R_h[:, 4096:8192].rearrange("p (c t) -> p c t", c=8)
            ub = [R_a[:, 0:271].bitcast(BF16), R_a[:, 272:543].bitcast(BF16)]
            halo = R_a[:, 1084:1084 + 120].bitcast(BF16).rearrange("p (c t) -> p c t", c=8)
            diag = [R_m[:, 0:1984].bitcast(BF16).rearrange("p (t n) -> p t n", t=31), R_m[:, 2048:2048 + 1984].bitcast(BF16).rearrange("p (t n) -> p t n", t=31)]
            b_diag = [Buf("diag0"), Buf("diag1")]
            claim("Rm", b_diag)
            sgt = [R_a[:, 1324:1324 + 512], R_s[:, 0:BLK]]
            b_h, b_s, b_v = Buf("hblk"), Buf("sblk"), Buf("vblk")
            b_u = [Buf("u0"), Buf("u1")]
            b_halo = Buf("halo")
            b_sg = [Buf("sg0"), b_sq[0]]
            claim("Rs", [b_sq[0], b_sq[1], b_stat])
            claim("Rh", [b_h, b_s, b_v])
            claim("Ra", [b_u[0], b_u[1], b_halo, b_sg[0]])
            P.op("pool", lambda e: e.memset(halo, 0.0), writes=[b_halo])
            wdw0 = PT["w_dw"]
            for tb in range(NBLK):
                for c in range(8):
                    P.op("act", lambda e, c=c, tb=tb: e.activation(out=hblk[:, c, :], in_=xT[:, c, blk(tb)], func=AF.Identity, scale=mcol(l, 8, c), bias=mcol(l, 0, c)),
                         reads=[bxT[tb], b_mods], writes=[b_h])
                def pw1(j):
                    ba, bg = (0, 1) if j % 2 == 0 else (2, 3)
                    s = j % 2
                    dg = diag[s]
                    P.op("dve", lambda e: e.tensor_tensor(out=dg, in0=identb[:].unsqueeze(1).to_broadcast([128, 31, 128]),
                                                          in1=pt[:, wdw0 + j * 31:wdw0 + (j + 1) * 31].unsqueeze(2).to_broadcast([128, 31, 128]), op=ALU.mult),
                         reads=[b_c, b_init], writes=[b_diag[s]])
                    for k in range(8):
                        P.op("pe", lambda e, k=k: e.matmul(out=ps[ba][:], lhsT=wpw1[:, k, j * 128:(j + 1) * 128], rhs=hblk[:, k, :], start=(k == 0), stop=(k == 7)),
                             reads=[b_w1[j // 4], b_h], writes=[bps[ba]])
                    for k in range(8):
                        P.op("pe", lambda e, k=k: e.matmul(out=ps[bg][:], lhsT=wpw1[:, k, 1024 + j * 128:1024 + (j + 1) * 128], rhs=hblk[:, k, :], start=(k == 0), stop=(k == 7)),
                             reads=[b_w1[2 + j // 4], b_h], writes=[bps[bg]])

                def glu_conv(j):
                    ba, bg = (0, 1) if j % 2 == 0 else (2, 3)
                    s = j % 2
                    u_ = ub[s]
                    dg = diag[s]
                    P.op("act", lambda e: e.activation(out=sgt[s], in_=ps[bg][:], func=AF.Sigmoid, bias=pt[:, PT["b_pw1"] + 8 + j:PT["b_pw1"] + 9 + j], scale=1.0),
                         reads=[bps[bg], b_init], writes=[b_sg[s]])
                    P.op("pool", lambda e: e.tensor_copy(out=u_[:, 0:30], in_=halo[:, j, :]), reads=[b_halo], writes=[b_u[s]])
                    P.op("dve", lambda e: e.scalar_tensor_tensor(out=u_[:, 30:542], in0=ps[ba][:], scalar=pt[:, PT["b_pw1"] + j:PT["b_pw1"] + j + 1], in1=sgt[s], op0=ALU.add, op1=ALU.mult),
                         reads=[bps[ba], b_sg[s], b_init], writes=[b_u[s]])
                    P.op("pool", lambda e: e.tensor_copy(out=halo[:, j, :], in_=u_[:, 512:542]), reads=[b_u[s]], writes=[b_halo])
                    cb = 6 + s
                    for tap in range(31):
                        P.op("pe", lambda e, tap=tap: e.matmul(out=ps[cb][:], lhsT=dg[:, tap, :], rhs=u_[:, tap:tap + 512], start=(tap == 0), stop=(tap == 30)),
                             reads=[b_diag[s], b_u[s]], writes=[bps[cb]])
                    P.op("dve", lambda e: e.tensor_scalar(out=vblk[:, j, :], in0=ps[cb][:], scalar1=pt[:, PT["b_dw"] + j:PT["b_dw"] + j + 1], scalar2=None, op0=ALU.add),
                         reads=[bps[cb], b_init], writes=[b_v])

                pw1(0)
                for j in range(8):
                    if j + 1 < 8:
                        pw1(j + 1)
                    glu_conv(j)
                layer_norm_block(lambda c: vblk[:, c, :], b_v, lambda c: sblk[:, c, :], b_s, PT["cln_g"], PT["cln_b"], AF.Silu, 4, 5)
                for d in range(8):
                    bank = 6 + d % 2
                    for k in range(8):
                        P.op("pe", lambda e, d=d, k=k, bank=bank: e.matmul(out=ps[bank][:], lhsT=wpw2[:, k, d * 128:(d + 1) * 128], rhs=sblk[:, k, :], start=(k == 0), stop=(k == 7)),
                             reads=[b_w2, b_s], writes=[bps[bank]])
                    s = d % 2
                    P.op("act", lambda e, d=d, bank=bank, s=s: e.activation(out=sgt[s], in_=ps[bank][:], func=AF.Identity, scale=mcol(l, 16, d), bias=g1b[:, d:d + 1]),
                         reads=[bps[bank], b_mods], writes=[b_sg[s]])
                    P.op("dve", lambda e, d=d, tb=tb, s=s: e.scalar_tensor_tensor(out=xT[:, d, blk(tb)], in0=xT[:, d, blk(tb)], scalar=ALPHA, in1=sgt[s], op0=ALU.mult, op1=ALU.add),
                         reads=[b_sg[s], bxT[tb]], writes=[bxT[tb]])
                pg = PT["post_g"] + (l * 2 + 0) * 8
                pb = PT["post_b"] + (l * 2 + 0) * 8
                layer_norm_block(lambda c, tb=tb: xT[:, c, blk(tb)], bxT[tb], lambda c, tb=tb: xT[:, c, blk(tb)], bxT[tb], pg, pb, AF.Identity, 4, 5)

        def moe_sublayer(l):
            hT = R_h[:].bitcast(BF16).rearrange("p (c t) -> p c t", c=8)
            b_hT = [Buf(f"hT{b}") for b in range(NBLK)]
            b_GT = Buf("GT")
            b_lg = Buf("lg")
            actT = [R_a[:, 0:1024].bitcast(BF16).rearrange("p (c t) -> p c t", c=4), R_a[:, 1024:2048].bitcast(BF16).rearrange("p (c t) -> p c t", c=4)]
            b_act = [Buf("act0"), Buf("act1")]
            gc = R_s[:, 0:BLK]
            sgm = R_s[:, BLK:2 * BLK]
            lin = R_s[:, 2 * BLK:3 * BLK]
            b_gc, b_sgm, b_lin = b_sq[0], b_sq[1], b_stat
            b_gb = Buf("gb")
            b_gsel = [Buf("gsel0"), Buf("gsel1")]
            claim("Rh", b_hT)
            claim("Rm", [b_GT, b_gb] + b_gsel)
            claim("Rs", [b_sq[0], b_sq[1], b_stat])
            claim("Ra", b_act)
            claim("Rw", [u["buf"] for u in units])
            for tb in range(NBLK):
                for c in range(8):
                    P.op("act", lambda e, c=c, tb=tb: e.activation(out=hT[:, c, blk(tb)], in_=xT[:, c, blk(tb)], func=AF.Identity, scale=mcol(l, 32, c), bias=mcol(l, 24, c)),
                         reads=[bxT[tb], b_mods], writes=[b_hT[tb]])
                for c in range(8):
                    P.op("dve", lambda e, c=c, tb=tb: e.tensor_scalar_mul(out=xT[:, c, blk(tb)], in0=xT[:, c, blk(tb)], scalar1=ALPHA),
                         reads=[bxT[tb]], writes=[bxT[tb]])
            wr = wr_s[:].rearrange("p (l k e) -> p l k e", l=2, k=8)
            for i in range(16):
                tb = i // 4
                tsl = slice(i * 128, (i + 1) * 128)
                for k in range(8):
                    P.op("pe", lambda e, k=k, tsl=tsl: e.matmul(out=ps[7][:, 0:NE], lhsT=hT[:, k, tsl], rhs=wr[:, l, k, :], start=(k == 0), stop=False),
                         reads=[b_hT[tb], b_init], writes=[bps[7]])
                P.op("pe", lambda e: e.matmul(out=ps[7][:, 0:NE], lhsT=onesb[0:1, :], rhs=br_s[0:1, l * NE:(l + 1) * NE], start=False, stop=True),
                     reads=[b_c, b_init], writes=[bps[7]])
                L = lg[:, 0:NE]
                M = lg[:, NE:2 * NE]
                E = lg[:, 2 * NE:3 * NE]
                G = lg[:, 3 * NE:4 * NE]
                P.op("dve", lambda e, L=L: e.tensor_copy(out=L, in_=ps[7][:, 0:NE]), reads=[bps[7]], writes=[b_lg])
                P.op("dve", lambda e, L=L: e.max(out=mx8[:, 0:8], in_=L), reads=[b_lg], writes=[b_lg])
                P.op("dve", lambda e, L=L, M=M: e.tensor_scalar(out=M, in0=L, scalar1=mx8[:, 3:4], scalar2=None, op0=ALU.is_ge), reads=[b_lg], writes=[b_lg])
                P.op("dve", lambda e: e.tensor_scalar_mul(out=mx8[:, 8:9], in0=mx8[:, 0:1], scalar1=-1.0), reads=[b_lg], writes=[b_lg])
                P.op("act", lambda e, L=L, E=E: e.activation(out=E, in_=L, func=AF.Exp, bias=mx8[:, 8:9], scale=1.0), reads=[b_lg], writes=[b_lg])
                P.op("dve", lambda e, M=M, E=E: e.tensor_tensor(out=E, in0=E, in1=M, op=ALU.mult), reads=[b_lg], writes=[b_lg])
                P.op("dve", lambda e, E=E: e.reduce_sum(out=mx8[:, 9:10], in_=E, axis=AX.X), reads=[b_lg], writes=[b_lg])
                P.op("dve", lambda e: e.reciprocal(out=mx8[:, 10:11], in_=mx8[:, 9:10]), reads=[b_lg], writes=[b_lg])
                P.op("dve", lambda e, E=E, G=G: e.tensor_scalar(out=G, in0=E, scalar1=mx8[:, 10:11], scalar2=None, op0=ALU.mult), reads=[b_lg], writes=[b_lg])
                P.op("pe", lambda e, G=G: e.transpose(out=ps[6][0:NE, 0:128], in_=G, identity=ident[:]), reads=[b_lg, b_init], writes=[bps[6]])
                P.op("act", lambda e, tsl=tsl: e.activation(out=GT[:, tsl], in_=ps[6][0:NE, 0:128], func=AF.Copy), reads=[bps[6]], writes=[b_GT])
                P.op("dve", lambda e, tsl=tsl: e.tensor_copy(out=GTb[:, tsl], in_=ps[6][0:NE, 0:128]), reads=[bps[6]], writes=[b_GT])

            def issue_unit(uidx):
                e_, half = uidx // 2, uidx % 2
                u = next_unit()
                g = u["raw"][:, 0:2048].bitcast(BF16).rearrange("p (k n) -> p k n", k=8)
                lw = u["raw"][:, 2048:4096].bitcast(BF16).rearrange("p (k n) -> p k n", k=8)
                dw = u["raw"][:, 4096:6144].bitcast(BF16).rearrange("p (f n) -> p f n", f=4)
                P.dma("pool", g, wgu_d[l, e_, :, half * 512:(half + 1) * 512].rearrange("(k p) n -> p k n", p=128), u["sem"], writes=[u["buf"]])
                P.dma("pool", lw, wgu_d[l, e_, :, 1024 + half * 512:1024 + (half + 1) * 512].rearrange("(k p) n -> p k n", p=128), u["sem"], writes=[u["buf"]])
                P.dma("pool", dw, wdn_d[l, e_, half * 512:(half + 1) * 512, :].rearrange("(f p) n -> p f n", p=128), u["sem"], writes=[u["buf"]])
                return (u, g, lw, dw)

            NU = 2 * NE
            pending = [issue_unit(0)]
            actc = [0]
            for uidx in range(NU):
                if uidx + 1 < NU:
                    pending.append(issue_unit(uidx + 1))
                u, g, lw, dw = pending.pop(0)
                e_, half = uidx // 2, uidx % 2
                bcol = (l * NE + e_) * 16
                if half == 0:
                    for tb in range(NBLK):
                        gs = gsel[:, (tb % 2) * BLK:(tb % 2 + 1) * BLK]
                        P.op("dve", lambda e, e_=e_, tb=tb, gs=gs: e.tensor_scalar(out=gs, in0=GT[:, blk(tb)], scalar1=ident[0:NE, e_:e_ + 1], scalar2=None, op0=ALU.mult), reads=[b_GT, b_init], writes=[b_gsel[tb % 2]])
                        P.op("pe", lambda e, gs=gs: e.matmul(out=ps[6][:], lhsT=ones[0:NE, :], rhs=gs, start=True, stop=True), reads=[b_gsel[tb % 2], b_c], writes=[bps[6]])
                        P.op("act", lambda e, tb=tb: e.activation(out=gbT[:, blk(tb)], in_=ps[6][:], func=AF.Copy), reads=[bps[6]], writes=[b_gb])
                for tb in range(NBLK):
                    a = actc[0] % 2
                    actc[0] += 1
                    for fc in range(4):
                        pg_, pl_ = (0, 1) if fc % 2 == 0 else (2, 3)
                        for k in range(8):
                            P.op("pe", lambda e, g=g, k=k, fc=fc, tb=tb, pg_=pg_: e.matmul(out=ps[pg_][:], lhsT=g[:, k, fc * 128:(fc + 1) * 128], rhs=hT[:, k, blk(tb)], start=(k == 0), stop=(k == 7)),
                                 reads=[u["buf"], b_hT[tb]], writes=[bps[pg_]])
                        for k in range(8):
                            P.op("pe", lambda e, lw=lw, k=k, fc=fc, tb=tb, pl_=pl_: e.matmul(out=ps[pl_][:], lhsT=lw[:, k, fc * 128:(fc + 1) * 128], rhs=hT[:, k, blk(tb)], start=(k == 0), stop=(k == 7)),
                                 reads=[u["buf"], b_hT[tb]], writes=[bps[pl_]])
                        jg = bcol + half * 4 + fc
                        jl = bcol + 8 + half * 4 + fc
                        P.op("dve", lambda e, pg_=pg_, jg=jg: e.tensor_scalar(out=gc, in0=ps[pg_][:], scalar1=pt[:, PT["b_gu"] + jg:PT["b_gu"] + jg + 1], scalar2=7.0, op0=ALU.add, op1=ALU.min),
                             reads=[bps[pg_], b_init], writes=[b_gc])
                        P.op("act", lambda e: e.activation(out=sgm, in_=gc, func=AF.Sigmoid, scale=1.702), reads=[b_gc], writes=[b_sgm])
                        P.op("dve", lambda e, pl_=pl_, jl=jl: e.tensor_scalar(out=lin, in0=ps[pl_][:], scalar1=pt[:, PT["b_gu"] + jl:PT["b_gu"] + jl + 1], scalar2=8.0, op0=ALU.add, op1=ALU.min),
                             reads=[bps[pl_], b_init], writes=[b_lin])
                        P.op("dve", lambda e: e.tensor_tensor(out=gc, in0=gc, in1=sgm, op=ALU.mult), reads=[b_gc, b_sgm], writes=[b_gc])
                        P.op("dve", lambda e: e.scalar_tensor_tensor(out=lin, in0=lin, scalar=-6.0, in1=gc, op0=ALU.max, op1=ALU.mult), reads=[b_lin, b_gc], writes=[b_lin])
                        P.op("dve", lambda e, a=a, fc=fc, tb=tb: e.tensor_tensor(out=actT[a][:, fc, :], in0=lin, in1=gbT[:, blk(tb)], op=ALU.mult), reads=[b_lin, b_gb], writes=[b_act[a]])
                    for d in range(8):
                        bank = 4 + d % 2
                        for fc in range(4):
                            last = (fc == 3) and not (uidx == 0)
                            P.op("pe", lambda e, dw=dw, fc=fc, d=d, a=a, bank=bank, last=last: e.matmul(out=ps[bank][:], lhsT=dw[:, fc, d * 128:(d + 1) * 128], rhs=actT[a][:, fc, :], start=(fc == 0), stop=last),
                                 reads=[u["buf"], b_act[a]], writes=[bps[bank]])
                        if uidx == 0:
                            P.op("pe", lambda e, d=d, tb=tb, bank=bank: e.matmul(out=ps[bank][:], lhsT=bdn_s[:, l * D + d * 128:l * D + (d + 1) * 128], rhs=GTb[:, blk(tb)], start=False, stop=True),
                                 reads=[b_init, b_GT], writes=[bps[bank]])
                        P.op("dve", lambda e, d=d, tb=tb, bank=bank: e.scalar_tensor_tensor(out=xT[:, d, blk(tb)], in0=ps[bank][:], scalar=mcol(l, 40, d), in1=xT[:, d, blk(tb)], op0=ALU.mult, op1=ALU.add),
                             reads=[bps[bank], b_mods, bxT[tb]], writes=[bxT[tb]])
            pg = PT["post_g"] + (l * 2 + 1) * 8
            pb = PT["post_b"] + (l * 2 + 1) * 8
            for tb in range(NBLK):
                layer_norm_block(lambda c, tb=tb: xT[:, c, blk(tb)], bxT[tb], lambda c, tb=tb: xT[:, c, blk(tb)], bxT[tb], pg, pb, AF.Identity, 0, 1)

        def attn_sublayer():
            l = 1
            xb = R_h[:].bitcast(BF16).rearrange("p (c t) -> p c t", c=8)
            h1b = R_w[:, 0:8192].bitcast(BF16).rearrange("p (c t) -> p c t", c=8)
            KT = R_w[:, 8192:9216].bitcast(BF16)
            QT = R_w[:, 9216:10240].bitcast(BF16)
            Vh = R_w[:, 10240:11264].bitcast(BF16).rearrange("p (i e) -> p i e", i=16)
            AT = R_w[:, 11264:12288].bitcast(BF16)
            Zs = R_m[:, 4096:5120]
            masks = R_a[:].rearrange("p (o q) -> p o q", o=4)
            PTt = [R_s[:, 0:256].bitcast(BF16), R_s[:, 256:512].bitcast(BF16)]
            tmp = [R_s[:, 512:1024], R_s[:, 1024:1536]]
            o0 = R_s[:, 1536:2048]
            rsc = R_s[:, 2048:2560]
            b_xb = [Buf(f"xb{i}") for i in range(NBLK)]
            b_h1 = [Buf(f"h1b{i}") for i in range(NBLK)]
            b_KT, b_QT, b_V, b_AT = Buf("KT"), Buf("QT"), Buf("Vh"), Buf("AT")
            b_Zs, b_mask = Buf("Zs"), Buf("masks")
            b_PT = [Buf("PT0"), Buf("PT1")]
            b_tmp = [Buf("tmp0"), Buf("tmp1")]
            b_o0, b_rsc = Buf("o0"), Buf("rsc")
            b_wt = [Buf("awt0"), Buf("awt1")]
            b_gs = [Buf(f"gs{h}") for h in range(8)]
            b_lam = Buf("lam")
            claim("Rh", b_xb)
            claim("Rw", b_h1 + [b_KT, b_QT, b_V, b_AT])
            claim("Rm", b_wt + [b_Zs])
            claim("Ra", [b_mask])
            P.dma("sp", masks, masks_d.rearrange("o p q -> p o q"), "amask", writes=[b_mask])
            P.op("dve", lambda e: e.tensor_tensor(out=lpT[:, 0:1], in0=lpT[:, 0:1], in1=lpT[:, 1:2], op=ALU.mult), reads=[b_init], writes=[b_lam])
            P.op("dve", lambda e: e.tensor_tensor(out=lpT[:, 1:2], in0=lpT[:, 2:3], in1=lpT[:, 3:4], op=ALU.mult), reads=[b_init, b_lam], writes=[b_lam])
            P.op("pe", lambda e: e.matmul(out=ps[7][:, 0:2], lhsT=ones[0:64, :], rhs=lpT[:, 0:2], start=True, stop=True), reads=[b_lam, b_c], writes=[bps[7]])
            P.op("act", lambda e: e.activation(out=nlam[:, 0:2], in_=ps[7][:, 0:2], func=AF.Exp), reads=[bps[7]], writes=[b_lam])
            P.op("dve", lambda e: e.tensor_tensor(out=nlam[:, 2:3], in0=nlam[:, 1:2], in1=nlam[:, 0:1], op=ALU.subtract), reads=[b_lam], writes=[b_lam])
            P.op("dve", lambda e: e.tensor_scalar_add(out=nlam[:, 3:4], in0=nlam[:, 2:3], scalar1=-LAMI), reads=[b_lam], writes=[b_lam])
            P.op("dve", lambda e: e.tensor_scalar_mul(out=gsub[:], in0=pt[:, PT["subln"]:PT["subln"] + 1], scalar1=1.0 - LAMI), reads=[b_init], writes=[b_lam])
            gst = R_s[:, 0:RG]
            rep = R_s[0:32, RG:RG + 128]
            ohg = R_s[0:32, RG + 128:RG + 128 + RG]
            b_gst, b_rep = Buf("gst"), Buf("rep")
            claim("Rs", [b_gst, b_rep])
            P.dma("sp", ohg, ohg_d[:, :], "ohg", writes=[b_rep])
            for h in range(8):
                P.op("act", lambda e, h=h: e.activation(out=rep, in_=ones[0:32, :], func=AF.Identity, scale=tabs[:, h:h + 1]), reads=[b_init, b_c], writes=[b_rep])
                for (n0, nn) in ((0, 512), (512, 512), (1024, 128)):
                    P.op("pe", lambda e, n0=n0, nn=nn: e.matmul(out=ps[6][:, 0:nn], lhsT=rep, rhs=ohg[:, n0:n0 + nn], start=True, stop=True), reads=[b_rep, b_init], writes=[bps[6]])
                    P.op("dve", lambda e, n0=n0, nn=nn: e.tensor_copy(out=gst[:, n0:n0 + nn], in_=ps[6][:, 0:nn]), reads=[bps[6]], writes=[b_gst])
                P.dma("sp", gs_d[h, :, :], gst, "gsw", reads=[b_gst], writes=[b_gs[h]])
            for b in b_PT + b_tmp + [b_o0, b_rsc]:
                pass
            claim("Rs", b_PT + b_tmp + [b_o0, b_rsc])
            for tb in range(NBLK):
                for c in range(8):
                    P.op("act", lambda e, c=c, tb=tb: e.activation(out=xb[:, c, blk(tb)], in_=xT[:, c, blk(tb)], func=AF.Copy), reads=[bxT[tb]], writes=[b_xb[tb]])
                    P.op("act", lambda e, c=c, tb=tb: e.activation(out=h1b[:, c, blk(tb)], in_=xT[:, c, blk(tb)], func=AF.Identity, scale=mcol(l, 8, c), bias=mcol(l, 0, c)),
                         reads=[bxT[tb], b_mods], writes=[b_h1[tb]])
                for c in range(8):
                    P.op("dve", lambda e, c=c, tb=tb: e.tensor_scalar_mul(out=xT[:, c, blk(tb)], in0=xT[:, c, blk(tb)], scalar1=ALPHA), reads=[bxT[tb]], writes=[bxT[tb]])
            for h in range(8):
                wsl = h % 2
                wbase = wsl * 2048
                wk = R_m[:, wbase:wbase + 512].bitcast(BF16).rearrange("p (k n) -> p k n", k=8)
                wv = R_m[:, wbase + 512:wbase + 1024].bitcast(BF16).rearrange("p (k n) -> p k n", k=8)
                wq = R_m[:, wbase + 1024:wbase + 1536].bitcast(BF16).rearrange("p (k n) -> p k n", k=8)
                wo = R_m[:, wbase + 1536:wbase + 2048].bitcast(BF16)
                hs = slice(h * 128, (h + 1) * 128)
                P.dma("pool", wk, wkv_d[:, hs].rearrange("(k p) n -> p k n", p=128), f"awt{wsl}", writes=[b_wt[wsl]])
                P.dma("pool", wv, wkv_d[:, D + h * 128:D + (h + 1) * 128].rearrange("(k p) n -> p k n", p=128), f"awt{wsl}", writes=[b_wt[wsl]])
                P.dma("pool", wq, wq_d[:, hs].rearrange("(k p) n -> p k n", p=128), f"awt{wsl}", writes=[b_wt[wsl]])
                P.dma("pool", wo, wo_d[hs, :], f"awt{wsl}", writes=[b_wt[wsl]])
                zsrc = bass.AP(gs_t, h * 128 * RG + 127, [[RG - 1, 128], [1, 1024]])
                P.dma("sp", Zs, zsrc, "zs", reads=[b_gs[h]], writes=[b_Zs])
                for tb in range(NBLK):
                    for k in range(8):
                        P.op("pe", lambda e, k=k, tb=tb, wk=wk: e.matmul(out=ps[6][:], lhsT=wk[:, k, :], rhs=xb[:, k, blk(tb)], start=(k == 0), stop=(k == 7)), reads=[b_wt[wsl], b_xb[tb]], writes=[bps[6]])
                    P.op("act", lambda e, tb=tb: e.activation(out=KT[:, blk(tb)], in_=ps[6][:], func=AF.Copy), reads=[bps[6]], writes=[b_KT])
                    for k in range(8):
                        P.op("pe", lambda e, k=k, tb=tb, wq=wq: e.matmul(out=ps[7][:], lhsT=wq[:, k, :], rhs=h1b[:, k, blk(tb)], start=(k == 0), stop=(k == 7)), reads=[b_wt[wsl], b_h1[tb]], writes=[bps[7]])
                    P.op("dve", lambda e, tb=tb: e.tensor_copy(out=QT[:, blk(tb)], in_=ps[7][:]), reads=[bps[7]], writes=[b_QT])
                for i4 in range(4):
                    bank = 6 + i4 % 2
                    for ii in range(4):
                        i = i4 * 4 + ii
                        for k in range(8):
                            P.op("pe", lambda e, k=k, i=i, ii=ii, wv=wv, bank=bank: e.matmul(out=ps[bank][:, ii * 128:(ii + 1) * 128], lhsT=xb[:, k, i * 128:(i + 1) * 128], rhs=wv[:, k, :], start=(k == 0), stop=(k == 7)),
                                 reads=[b_wt[wsl], b_xb[i // 4]], writes=[bps[bank]])
                    if i4 % 2 == 0:
                        P.op("act", lambda e, i4=i4, bank=bank: e.activation(out=Vh[:, i4 * 4:(i4 + 1) * 4, :], in_=ps[bank][:].rearrange("p (i e) -> p i e", i=4), func=AF.Copy), reads=[bps[bank]], writes=[b_V])
                    else:
                        P.op("dve", lambda e, i4=i4, bank=bank: e.tensor_copy(out=Vh[:, i4 * 4:(i4 + 1) * 4, :], in_=ps[bank][:].rearrange("p (i e) -> p i e", i=4)), reads=[bps[bank]], writes=[b_V])
                tiles = [(qb, m, kt) for qb in range(NBLK) for m in range(2) for kt in range(4 * qb + 4)]
                NTL = len(tiles)
                pending = []

                def stageA(idx):
                    qb, m, kt = tiles[idx]
                    msl = slice(m * 64, (m + 1) * 64)
                    off = kt * 128 - qb * 512
                    sb = idx % 2
                    P.op("pe", lambda e: e.matmul(out=ps[sb][:], lhsT=KT[msl, kt * 128:(kt + 1) * 128], rhs=QT[msl, blk(qb)], start=True, stop=True),
                         reads=[b_KT, b_QT], writes=[bps[sb]])
                    if off <= -256:
                        P.op("act", lambda e: e.activation(out=PTt[sb], in_=ps[sb][:], func=AF.Exp, scale=0.125, bias=negc[:, 0:1]), reads=[bps[sb], b_c], writes=[b_PT[sb]])
                    else:
                        z0 = 384 - off
                        P.op("dve", lambda e: e.scalar_tensor_tensor(out=tmp[sb], in0=ps[sb][:], scalar=0.125, in1=Zs[:, z0:z0 + 512], op0=ALU.mult, op1=ALU.add),
                             reads=[bps[sb], b_Zs], writes=[b_tmp[sb]])
                        if off >= 0:
                            P.op("dve", lambda e: e.tensor_tensor(out=tmp[sb], in0=tmp[sb], in1=masks[:, off // 128, :], op=ALU.add), reads=[b_tmp[sb], b_mask], writes=[b_tmp[sb]])
                        P.op("act", lambda e: e.activation(out=PTt[sb], in_=tmp[sb], func=AF.Exp, scale=1.0, bias=negc[:, 0:1]), reads=[b_tmp[sb], b_c], writes=[b_PT[sb]])

                def seg1(qb, m):
                    po, psm = (2, 3) if m == 0 else (4, 5)
                    P.op("dve", lambda e: e.reciprocal(out=rsc, in_=ps[psm][:]), reads=[bps[psm]], writes=[b_rsc])
                    if m == 0:
                        P.op("dve", lambda e: e.tensor_tensor(out=o0, in0=ps[po][:], in1=rsc, op=ALU.mult), reads=[bps[po], b_rsc], writes=[b_o0])
                    else:
                        P.op("dve", lambda e: e.tensor_tensor(out=rsc, in0=ps[po][:], in1=rsc, op=ALU.mult), reads=[bps[po], b_rsc], writes=[b_rsc])
                        P.op("dve", lambda e: e.scalar_tensor_tensor(out=o0, in0=rsc, scalar=nlam[:, 3:4], in1=o0, op0=ALU.mult, op1=ALU.add), reads=[b_rsc, b_o0, b_lam], writes=[b_o0])
                        P.op("act", lambda e: e.activation(out=rsc, in_=o0, func=AF.Square), reads=[b_o0], writes=[b_rsc])

                def seg2(qb):
                    P.op("pe", lambda e: e.matmul(out=ps[6][:], lhsT=ones[:], rhs=rsc, start=True, stop=True), reads=[b_rsc, b_c], writes=[bps[6]])
                    P.op("act", lambda e: e.activation(out=rsc, in_=ps[6][:], func=AF.Sqrt, scale=1.0 / 128.0, bias=epsT[:, 0:1]), reads=[bps[6], b_c], writes=[b_rsc])
                    P.op("dve", lambda e: e.reciprocal(out=rsc, in_=rsc), reads=[b_rsc], writes=[b_rsc])
                    P.op("dve", lambda e: e.tensor_tensor(out=o0, in0=o0, in1=rsc, op=ALU.mult), reads=[b_o0, b_rsc], writes=[b_o0])
                    P.op("act", lambda e: e.activation(out=AT[:, blk(qb)], in_=o0, func=AF.Identity, scale=gsub[:, 0:1]), reads=[b_o0, b_lam], writes=[b_AT])

                def seg3(qb):
                    for d in range(8):
                        bank = 6 + d % 2
                        P.op("pe", lambda e, d=d, bank=bank, wo=wo: e.matmul(out=ps[bank][:], lhsT=wo[:, d * 128:(d + 1) * 128], rhs=AT[:, blk(qb)], start=True, stop=True), reads=[b_wt[wsl], b_AT], writes=[bps[bank]])
                        P.op("dve", lambda e, d=d, bank=bank: e.scalar_tensor_tensor(out=xT[:, d, blk(qb)], in0=ps[bank][:], scalar=mcol(l, 16, d), in1=xT[:, d, blk(qb)], op0=ALU.mult, op1=ALU.add),
                             reads=[bps[bank], b_mods, bxT[qb]], writes=[bxT[qb]])

                def stageB(idx):
                    qb, m, kt = tiles[idx]
                    ntile = 4 * qb + 4
                    po, psm = (2, 3) if m == 0 else (4, 5)
                    sb = idx % 2
                    P.op("pe", lambda e: e.matmul(out=ps[po][:], lhsT=Vh[:, kt, :], rhs=PTt[sb], start=(kt == 0), stop=(kt == ntile - 1)), reads=[b_V, b_PT[sb]], writes=[bps[po]])
                    P.op("pe", lambda e: e.matmul(out=ps[psm][:], lhsT=onesb[:], rhs=PTt[sb], start=(kt == 0), stop=(kt == ntile - 1)), reads=[b_c, b_PT[sb]], writes=[bps[psm]])
                    if kt == ntile - 1:
                        seg1(qb, m)
                        if m == 1:
                            pending.append((idx + 3, lambda qb=qb: seg2(qb)))
                            pending.append((idx + 5, lambda qb=qb: seg3(qb)))

                for idx in range(NTL + 1):
                    if idx < NTL:
                        stageA(idx)
                    if idx >= 1:
                        stageB(idx - 1)
                    while pending and pending[0][0] <= idx:
                        pending.pop(0)[1]()
                while pending:
                    pending.pop(0)[1]()
            pg = PT["post_g"] + (l * 2 + 0) * 8
            pb = PT["post_b"] + (l * 2 + 0) * 8
            claim("Rs", [b_sq[0], b_sq[1], b_stat])
            for tb in range(NBLK):
                layer_norm_block(lambda c, tb=tb: xT[:, c, blk(tb)], bxT[tb], lambda c, tb=tb: xT[:, c, blk(tb)], bxT[tb], pg, pb, AF.Identity, 0, 1)

        b_XS = [Buf(f"XS{i}") for i in range(8)]
        b_YS = [Buf("YS0"), Buf("YS1")]

        def moe_sparse(l):
            hT = R_h[:].bitcast(BF16).rearrange("p (c t) -> p c t", c=8)
            b_hT = [Buf(f"hT{b}") for b in range(NBLK)]
            b_GT, b_lg, b_rt = Buf("GT"), Buf("lg"), Buf("rt")
            b_M, b_cnt = Buf("Mall"), Buf("cnt")
            b_posl = [Buf(f"posI{i}") for i in range(16)]
            b_gatesl = [Buf(f"gates{i}") for i in range(16)]
            claim("Rh", b_hT)
            claim("Rm", [b_GT])
            claim("Rs", [b_sq[0], b_sq[1], b_stat])
            for tb in range(NBLK):
                for c in range(8):
                    P.op("act", lambda e, c=c, tb=tb: e.activation(out=hT[:, c, blk(tb)], in_=xT[:, c, blk(tb)], func=AF.Identity, scale=mcol(l, 32, c), bias=mcol(l, 24, c)),
                         reads=[bxT[tb], b_mods], writes=[b_hT[tb]])
                for c in range(8):
                    P.op("dve", lambda e, c=c, tb=tb: e.tensor_scalar_mul(out=xT[:, c, blk(tb)], in0=xT[:, c, blk(tb)], scalar1=ALPHA), reads=[bxT[tb]], writes=[bxT[tb]])
            wr = wr_s[:].rearrange("p (l k e) -> p l k e", l=2, k=8)
            Mv = Mall[:].rearrange("p (i e) -> p i e", i=16)
            L = lg[:, 0:NE]
            M = lg[:, NE:2 * NE]
            E = lg[:, 2 * NE:3 * NE]
            G = lg[:, 3 * NE:4 * NE]
            OH = rt[:, 0:128].rearrange("p (k e) -> p k e", k=4)
            TA = rt[:, 128:256].rearrange("p (k e) -> p k e", k=4)
            TB = rt[:, 256:384].rearrange("p (k e) -> p k e", k=4)
            eid4 = rt[:, 384:388]
            rank4 = rt[:, 388:392]
            pos4 = rt[:, 392:396]
            e4 = rt[:, 396:400]
            for i in range(16):
                tb = i // 4
                tsl = slice(i * 128, (i + 1) * 128)
                for k in range(8):
                    P.op("pe", lambda e, k=k, tsl=tsl: e.matmul(out=ps[7][:, 0:NE], lhsT=hT[:, k, tsl], rhs=wr[:, l, k, :], start=(k == 0), stop=False), reads=[b_hT[tb], b_init], writes=[bps[7]])
                P.op("pe", lambda e: e.matmul(out=ps[7][:, 0:NE], lhsT=onesb[0:1, :], rhs=br_s[0:1, l * NE:(l + 1) * NE], start=False, stop=True), reads=[b_c, b_init], writes=[bps[7]])
                P.op("dve", lambda e: e.tensor_copy(out=L, in_=ps[7][:, 0:NE]), reads=[bps[7]], writes=[b_lg])
                P.op("dve", lambda e: e.max(out=mx8[:, 0:8], in_=L), reads=[b_lg], writes=[b_lg])
                P.op("dve", lambda e: e.tensor_copy(out=TA[:, 0, :], in_=L), reads=[b_lg], writes=[b_rt])
                Lw = TA[:, 0, :]
                EQ = TA[:, 1, :]
                TT = TA[:, 2, :]
                for k in range(4):
                    P.op("dve", lambda e, k=k: e.tensor_scalar(out=EQ, in0=Lw, scalar1=mx8[:, k:k + 1], scalar2=None, op0=ALU.is_equal), reads=[b_rt, b_lg], writes=[b_rt])
                    P.op("dve", lambda e: e.scalar_tensor_tensor(out=TT, in0=EQ, scalar=-1000.0, in1=iotaP[:], op0=ALU.mult, op1=ALU.add), reads=[b_rt, b_c], writes=[b_rt])
                    P.op("dve", lambda e, k=k: e.tensor_reduce(out=eid4[:, k:k + 1], in_=TT, axis=AX.X, op=ALU.min), reads=[b_rt], writes=[b_rt])
                    P.op("dve", lambda e, k=k: e.tensor_scalar(out=OH[:, k, :], in0=iotaT[:], scalar1=eid4[:, k:k + 1], scalar2=None, op0=ALU.is_equal), reads=[b_rt, b_init], writes=[b_rt])
                    if k < 3:
                        P.op("dve", lambda e, k=k: e.scalar_tensor_tensor(out=Lw, in0=OH[:, k, :], scalar=-1.0e30, in1=Lw, op0=ALU.mult, op1=ALU.add), reads=[b_rt], writes=[b_rt])
                P.op("dve", lambda e: e.reduce_sum(out=M, in_=OH.rearrange("p k e -> p e k"), axis=AX.X), reads=[b_rt], writes=[b_lg])
                P.op("act", lambda e, i=i: e.activation(out=Mv[:, i, :], in_=M, func=AF.Copy), reads=[b_lg], writes=[b_M])
                P.op("dve", lambda e: e.tensor_scalar_mul(out=mx8[:, 8:9], in0=mx8[:, 0:1], scalar1=-1.0), reads=[b_lg], writes=[b_lg])
                P.op("act", lambda e: e.activation(out=E, in_=L, func=AF.Exp, bias=mx8[:, 8:9], scale=1.0), reads=[b_lg], writes=[b_lg])
                P.op("act", lambda e: e.activation(out=e4, in_=mx8[:, 0:4], func=AF.Exp, bias=mx8[:, 8:9], scale=1.0), reads=[b_lg], writes=[b_rt])
                P.op("dve", lambda e: e.tensor_tensor(out=E, in0=E, in1=M, op=ALU.mult), reads=[b_lg], writes=[b_lg])
                P.op("dve", lambda e: e.reduce_sum(out=mx8[:, 9:10], in_=E, axis=AX.X), reads=[b_lg], writes=[b_lg])
                P.op("dve", lambda e: e.reciprocal(out=mx8[:, 10:11], in_=mx8[:, 9:10]), reads=[b_lg], writes=[b_lg])
                P.op("dve", lambda e: e.tensor_scalar(out=G, in0=E, scalar1=mx8[:, 10:11], scalar2=None, op0=ALU.mult), reads=[b_lg], writes=[b_lg])
                P.op("dve", lambda e, i=i: e.tensor_scalar(out=gates4[:, 4 * i:4 * i + 4], in0=e4, scalar1=mx8[:, 10:11], scalar2=None, op0=ALU.mult), reads=[b_lg, b_rt], writes=[b_gatesl[i]])
                P.op("pe", lambda e: e.transpose(out=ps[6][0:NE, 0:128], in_=G, identity=ident[:]), reads=[b_lg, b_init], writes=[bps[6]])
                P.op("act", lambda e, tsl=tsl: e.activation(out=GTb[:, tsl], in_=ps[6][0:NE, 0:128], func=AF.Copy), reads=[bps[6]], writes=[b_GT])
                for i2 in range(i + 1):
                    lhs = trib if i2 == i else onesb
                    P.op("pe", lambda e, i2=i2, i=i, lhs=lhs: e.matmul(out=ps[5][:, 0:NE], lhsT=lhs[:], rhs=Mv[:, i2, :], start=(i2 == 0), stop=(i2 == i)), reads=[b_M, b_c, b_init], writes=[bps[5]])
                P.op("pe", lambda e, i=i: e.matmul(out=ps[4][0:1, 0:NE], lhsT=onesb[:, 0:1], rhs=Mv[:, i, :], start=(i == 0), stop=(i == 15)), reads=[b_M, b_c], writes=[bps[4]])
                P.op("dve", lambda e: e.tensor_tensor(out=TB, in0=OH, in1=ps[5][:, 0:NE].unsqueeze(1).to_broadcast([128, 4, NE]), op=ALU.mult), reads=[b_rt, bps[5]], writes=[b_rt])
                P.op("dve", lambda e: e.reduce_sum(out=rank4, in_=TB, axis=AX.X), reads=[b_rt], writes=[b_rt])
                P.op("dve", lambda e: e.scalar_tensor_tensor(out=pos4, in0=eid4, scalar=float(S), in1=rank4, op0=ALU.mult, op1=ALU.add), reads=[b_rt], writes=[b_rt])
                P.op("dve", lambda e, i=i: e.tensor_copy(out=posI[:, 4 * i:4 * i + 4], in_=pos4), reads=[b_rt], writes=[b_posl[i]])
            P.op("dve", lambda e: e.tensor_copy(out=cntF[:], in_=ps[4][0:1, 0:NE]), reads=[bps[4]], writes=[b_cnt])
            P.op("dve", lambda e: e.tensor_copy(out=cntI[:], in_=cntF[:]), reads=[b_cnt], writes=[b_cnt])
            for tb in range(NBLK):
                for d in range(8):
                    bank = 2 + d % 2
                    P.op("pe", lambda e, d=d, tb=tb, bank=bank: e.matmul(out=ps[bank][:], lhsT=bdn_s[:, l * D + d * 128:l * D + (d + 1) * 128], rhs=GTb[:, blk(tb)], start=True, stop=True), reads=[b_init, b_GT], writes=[bps[bank]])
                    P.op("dve", lambda e, d=d, tb=tb, bank=bank: e.scalar_tensor_tensor(out=xT[:, d, blk(tb)], in0=ps[bank][:], scalar=mcol(l, 40, d), in1=xT[:, d, blk(tb)], op0=ALU.mult, op1=ALU.add),
                         reads=[bps[bank], b_mods, bxT[tb]], writes=[bxT[tb]])
            htok = [R_a[:, 0:512].bitcast(BF16), R_a[:, 512:1024].bitcast(BF16)]
            b_htok = [Buf("htok0"), Buf("htok1")]
            actT = [R_a[:, 1024:1536].bitcast(BF16).rearrange("p (f s) -> p f s", f=8), R_a[:, 1536:2048].bitcast(BF16).rearrange("p (f s) -> p f s", f=8)]
            b_actT = [Buf("actT0"), Buf("actT1")]
            claim("Ra", b_htok + b_actT)
            for i in range(16):
                sl = i % 2
                bank = sl
                pv = ps[bank][:].bitcast(BF16)
                for c in range(8):
                    P.op("pe", lambda e, c=c, i=i, pv=pv: e.transpose(out=pv[:, c * 128:(c + 1) * 128], in_=hT[:, c, i * 128:(i + 1) * 128], identity=identb[:]), reads=[b_hT[i // 4], b_c], writes=[bps[bank]])
                P.op("act", lambda e, sl=sl, pv=pv: e.activation(out=htok[sl], in_=pv, func=AF.Copy), reads=[bps[bank]], writes=[b_htok[sl]])
                for k in range(4):
                    col = 4 * i + k
                    P.dma("pool", None, None, f"xsc{sl}", reads=[b_htok[sl], b_posl[i]], writes=[b_XS[sl * 4 + k]],
                          fn=lambda e, sl=sl, col=col: e.indirect_dma_start(out=xs_d[:, :], out_offset=bass.IndirectOffsetOnAxis(ap=posI[:, col:col + 1], axis=0), in_=htok[sl], in_offset=None))
            ring = [R_w[:, 0:4096], R_w[:, 4096:8192], R_w[:, 8192:12288], R_h[:, 0:4096], R_h[:, 4096:8192]]
            b_ring = [Buf(f"ring{q}") for q in range(5)]
            claim("Rw", b_ring[0:3])
            xsb = R_s[:, 0:512].bitcast(BF16)
            xsT = [R_s[:, 512:1024].bitcast(BF16).rearrange("p (k s) -> p k s", k=8), R_s[:, 1024:1536].bitcast(BF16).rearrange("p (k s) -> p k s", k=8)]
            act_tt = [R_s[:, 1536:2048].bitcast(BF16), R_s[:, 2048:2560].bitcast(BF16)]
            b_xsb, b_xsT, b_acttt = Buf("xsb"), [Buf("xsT0"), Buf("xsT1")], [Buf("act_t0"), Buf("act_t1")]
            ys = [R_m[:, 0:1024], R_m[:, 1024:2048]]
            b_ys = [Buf("ys0"), Buf("ys1")]
            bgu_row = [R_m[0:1, 2048:3072].bitcast(BF16), R_m[0:1, 3072:4096].bitcast(BF16)]
            b_bgu = [Buf("bgu0"), Buf("bgu1")]
            gc, sg_, ln_, t1 = R_m[:, 4096:4608], R_m[:, 4608:5120], R_m[:, 5120:5632], R_m[:, 5632:6144]
            b_gc, b_sg2, b_ln, b_t1 = Buf("gc"), Buf("sg2"), Buf("ln"), Buf("t1")
            ring_claimed_h = [False]

            def issue_piece(e_, p):
                q = (3 * e_ + p) % 5
                if q >= 3 and not ring_claimed_h[0]:
                    claim("Rh", b_ring[3:5])
                    ring_claimed_h[0] = True
                wv = ring[q].bitcast(BF16).rearrange("p (k n) -> p k n", k=8)
                if p == 0:
                    src = wgu_d[l, e_, :, 0:D]
                elif p == 1:
                    src = wgu_d[l, e_, :, D:2 * D]
                else:
                    src = wdn_d[l, e_, :, :]
                P.dma("pool", wv, src.rearrange("(k p) n -> p k n", p=128), f"rg{q}", writes=[b_ring[q]])
                return q, wv

            def issue_bias(e_):
                P.dma("pool", bgu_row[e_ % 2], bgu_d[l, e_:e_ + 1, :], f"bgu{e_ % 2}", writes=[b_bgu[e_ % 2]])

            claim("Rs", [b_xsb, b_xsT[0], b_xsT[1]] + b_acttt)
            claim("Rm", b_ys + b_bgu + [b_gc, b_sg2, b_ln, b_t1])
            pieces = {}
            pieces[(0, 0)] = issue_piece(0, 0)
            pieces[(0, 1)] = issue_piece(0, 1)
            pieces[(0, 2)] = issue_piece(0, 2)
            issue_bias(0)
            bcnt = [0]
            for e_ in range(NE):
                if e_ + 1 < NE:
                    pieces[(e_ + 1, 0)] = issue_piece(e_ + 1, 0)
                    pieces[(e_ + 1, 1)] = issue_piece(e_ + 1, 1)
                    issue_bias(e_ + 1)
                (qg, wg), (ql, wl), (qd, wd) = pieces[(e_, 0)], pieces[(e_, 1)], pieces[(e_, 2)]
                brow = bgu_row[e_ % 2]
                for eng in ("pe", "act", "dve", "sp"):
                    P.regload(eng, cntI[0:1, e_:e_ + 1], reads=[b_cnt])
                def stage_a(j, e_=e_, wg=wg, wl=wl, brow=brow, qg=qg, ql=ql):
                    pr = ((l, e_), j * 128)
                    s_ = j % 2
                    act_t, b_actt = act_tt[s_], b_acttt[s_]
                    r0 = e_ * S + j * 128
                    P.dma("sp", xsb, xs_d[r0:r0 + 128, :], "xsl", reads=b_XS, writes=[b_xsb], pred=pr)
                    pv = ps[0][:].bitcast(BF16)
                    for k in range(8):
                        P.op("pe", lambda e, k=k, pv=pv: e.transpose(out=pv[:, k * 128:(k + 1) * 128], in_=xsb[:, k * 128:(k + 1) * 128], identity=identb[:]), reads=[b_xsb, b_c], writes=[bps[0]], pred=pr)
                    P.op("dve", lambda e, s_=s_, pv=pv: e.tensor_copy(out=xsT[s_], in_=pv.rearrange("p (k s) -> p k s", k=8)), reads=[bps[0]], writes=[b_xsT[s_]], pred=pr)
                    for hf in range(2):
                        bg_, bl_ = 2 + 2 * hf, 3 + 2 * hf
                        fs = slice(hf * 512, (hf + 1) * 512)
                        for k in range(8):
                            P.op("pe", lambda e, k=k, s_=s_, bg_=bg_, fs=fs: e.matmul(out=ps[bg_][:], lhsT=xsT[s_][:, k, :], rhs=wg[:, k, fs], start=(k == 0), stop=False), reads=[b_ring[qg], b_xsT[s_]], writes=[bps[bg_]], pred=pr)
                        P.op("pe", lambda e, bg_=bg_, hf=hf: e.matmul(out=ps[bg_][:], lhsT=onesb[0:1, :], rhs=brow[0:1, hf * 512:(hf + 1) * 512], start=False, stop=True), reads=[b_bgu[e_ % 2], b_c], writes=[bps[bg_]], pred=pr)
                        for k in range(8):
                            P.op("pe", lambda e, k=k, s_=s_, bl_=bl_, fs=fs: e.matmul(out=ps[bl_][:], lhsT=xsT[s_][:, k, :], rhs=wl[:, k, fs], start=(k == 0), stop=False), reads=[b_ring[ql], b_xsT[s_]], writes=[bps[bl_]], pred=pr)
                        P.op("pe", lambda e, bl_=bl_, hf=hf: e.matmul(out=ps[bl_][:], lhsT=onesb[0:1, :], rhs=brow[0:1, D + hf * 512:D + (hf + 1) * 512], start=False, stop=True), reads=[b_bgu[e_ % 2], b_c], writes=[bps[bl_]], pred=pr)
                        P.op("dve", lambda e, bg_=bg_: e.tensor_scalar_min(out=gc, in0=ps[bg_][:], scalar1=7.0), reads=[bps[bg_]], writes=[b_gc], pred=pr)
                        P.op("act", lambda e: e.activation(out=sg_, in_=gc, func=AF.Sigmoid, scale=1.702), reads=[b_gc], writes=[b_sg2], pred=pr)
                        P.op("dve", lambda e, bl_=bl_: e.tensor_scalar(out=ln_, in0=ps[bl_][:], scalar1=1.0, scalar2=8.0, op0=ALU.add, op1=ALU.min), reads=[bps[bl_]], writes=[b_ln], pred=pr)
                        P.op("dve", lambda e: e.tensor_tensor(out=t1, in0=gc, in1=sg_, op=ALU.mult), reads=[b_gc, b_sg2], writes=[b_t1], pred=pr)
                        P.op("dve", lambda e, fs=fs, act_t=act_t: e.scalar_tensor_tensor(out=act_t[:, fs], in0=ln_, scalar=-6.0, in1=t1, op0=ALU.max, op1=ALU.mult), reads=[b_ln, b_t1], writes=[b_actt], pred=pr)

                def stage_b(j, e_=e_, wd=wd, qd=qd):
                    pr = ((l, e_), j * 128)
                    s_ = j % 2
                    act_t, b_actt = act_tt[s_], b_acttt[s_]
                    r0 = e_ * S + j * 128
                    pv1 = ps[1][:].bitcast(BF16)
                    for f in range(8):
                        P.op("pe", lambda e, f=f, pv1=pv1, act_t=act_t: e.transpose(out=pv1[:, f * 128:(f + 1) * 128], in_=act_t[:, f * 128:(f + 1) * 128], identity=identb[:]), reads=[b_actt, b_c], writes=[bps[1]], pred=pr)
                    P.op("dve", lambda e, s_=s_, pv1=pv1: e.tensor_copy(out=actT[s_], in_=pv1.rearrange("p (f s) -> p f s", f=8)), reads=[bps[1]], writes=[b_actT[s_]], pred=pr)
                    for n in range(2):
                        for f in range(8):
                            P.op("pe", lambda e, f=f, n=n, s_=s_: e.matmul(out=ps[6 + n][:], lhsT=actT[s_][:, f, :], rhs=wd[:, f, n * 512:(n + 1) * 512], start=(f == 0), stop=(f == 7)), reads=[b_ring[qd], b_actT[s_]], writes=[bps[6 + n]], pred=pr)
                        P.op("dve", lambda e, n=n, s_=s_: e.tensor_copy(out=ys[s_][:, n * 512:(n + 1) * 512], in_=ps[6 + n][:]), reads=[bps[6 + n]], writes=[b_ys[s_]], pred=pr)
                    P.dma("act", ys_d[r0:r0 + 128, :], ys[s_], "yst", reads=[b_ys[s_]], writes=[b_YS[s_]], pred=pr)

                stage_a(0)
                for j in range(16):
                    if j + 1 < 16:
                        stage_a(j + 1)
                    stage_b(j)
                if e_ + 1 < NE:
                    pieces[(e_ + 1, 2)] = issue_piece(e_ + 1, 2)
            yb = [[R_w[:, (s2 * 4 + k) * 1024:(s2 * 4 + k + 1) * 1024] for k in range(4)] for s2 in range(2)]
            acc = [R_w[:, 8192:9216], R_w[:, 9216:10240]]
            b_yb = [[Buf(f"yb{s2}{k}") for k in range(4)] for s2 in range(2)]
            b_acc = [Buf("acc0"), Buf("acc1")]
            claim("Rw", b_yb[0] + b_yb[1] + b_acc)
            for i in range(16):
                s2 = i % 2
                for k in range(4):
                    col = 4 * i + k
                    P.dma("pool", None, None, f"yg{s2}", reads=b_YS + [b_posl[i]], writes=[b_yb[s2][k]],
                          fn=lambda e, s2=s2, k=k, col=col: e.indirect_dma_start(out=yb[s2][k], out_offset=None, in_=ys_d[:, :], in_offset=bass.IndirectOffsetOnAxis(ap=posI[:, col:col + 1], axis=0)))
                P.op("dve", lambda e, s2=s2, i=i: e.tensor_scalar(out=acc[s2], in0=yb[s2][0], scalar1=gates4[:, 4 * i:4 * i + 1], scalar2=None, op0=ALU.mult), reads=[b_yb[s2][0], b_gatesl[i]], writes=[b_acc[s2]])
                for k in range(1, 4):
                    P.op("dve", lambda e, s2=s2, i=i, k=k: e.scalar_tensor_tensor(out=acc[s2], in0=yb[s2][k], scalar=gates4[:, 4 * i + k:4 * i + k + 1], in1=acc[s2], op0=ALU.mult, op1=ALU.add), reads=[b_yb[s2][k], b_gatesl[i], b_acc[s2]], writes=[b_acc[s2]])
                for g in range(2):
                    bank = 2 * s2 + g
                    for cc in range(4):
                        d = g * 4 + cc
                        P.op("pe", lambda e, d=d, cc=cc, s2=s2, bank=bank: e.transpose(out=ps[bank][:, cc * 128:(cc + 1) * 128], in_=acc[s2][:, d * 128:(d + 1) * 128], identity=ident[:]), reads=[b_acc[s2], b_init], writes=[bps[bank]])
                    for cc in range(4):
                        d = g * 4 + cc
                        P.op("dve", lambda e, d=d, cc=cc, i=i, bank=bank: e.scalar_tensor_tensor(out=xT[:, d, i * 128:(i + 1) * 128], in0=ps[bank][:, cc * 128:(cc + 1) * 128], scalar=mcol(l, 40, d), in1=xT[:, d, i * 128:(i + 1) * 128], op0=ALU.mult, op1=ALU.add),
                             reads=[bps[bank], b_mods, bxT[i // 4]], writes=[bxT[i // 4]])
            pg = PT["post_g"] + (l * 2 + 1) * 8
            pb = PT["post_b"] + (l * 2 + 1) * 8
            claim("Rs", [b_sq[0], b_sq[1], b_stat])
            for tb in range(NBLK):
                layer_norm_block(lambda c, tb=tb: xT[:, c, blk(tb)], bxT[tb], lambda c, tb=tb: xT[:, c, blk(tb)], bxT[tb], pg, pb, AF.Identity, 4, 5)

        moe = moe_sparse if SPARSE else moe_sublayer
        phases = [("conv", conv_sublayer), ("moe0", lambda: moe(0)), ("attn", attn_sublayer), ("moe1", lambda: moe(1))]
        for name, fn in phases:
            fn()
            if stop_after == name:
                break

        b_stage = [Buf("ostage0"), Buf("ostage1")]
        claim("Ra", b_stage)
        for i in range(16):
            sl = i % 2
            sv = stage[:, sl * D:(sl + 1) * D]
            for g in range(2):
                bank = 2 + g
                for cc in range(4):
                    c = g * 4 + cc
                    P.op("pe", lambda e, c=c, cc=cc, i=i, bank=bank: e.transpose(out=ps[bank][:, cc * 128:(cc + 1) * 128], in_=xT[:, c, i * 128:(i + 1) * 128], identity=ident[:]),
                         reads=[bxT[i // 4], b_init], writes=[bps[bank]])
                if g == 0:
                    P.op("act", lambda e, sv=sv, bank=bank: e.activation(out=sv[:, 0:512], in_=ps[bank][:], func=AF.Copy), reads=[bps[bank]], writes=[b_stage[sl]])
                else:
                    P.op("dve", lambda e, sv=sv, bank=bank: e.tensor_copy(out=sv[:, 512:1024], in_=ps[bank][:]), reads=[bps[bank]], writes=[b_stage[sl]])
            P.dma("sp", out_d[i * 128:(i + 1) * 128, :], sv, "out", reads=[b_stage[sl]])
        P.final_wait("sp", ["out"])
        P.emit()
    return nc


_CACHE = {}


def _t5_bucket(rel):
    nb = 16
    ret = np.where(rel > 0, nb, 0)
    n = np.abs(rel)
    max_exact = 8
    nf = np.maximum(n, 1).astype(np.float32) / np.float32(max_exact)
    large = max_exact + (np.log(nf) / np.float32(math.log(128 / max_exact)) * np.float32(nb - max_exact)).astype(np.int32)
    large = np.minimum(large, nb - 1)
    return ret + np.where(n < max_exact, n, large)


def _attn_consts():
    rel = 511 - np.arange(RG)
    bk = _t5_bucket(rel)
    ohg = np.zeros((32, RG), np.float32)
    ohg[bk, np.arange(RG)] = 1.0
    ohg[15, :] -= 1.0
    masks = np.zeros((4, 128, BLK), np.float32)
    kl = np.arange(128)[:, None] // 64
    ql = np.arange(BLK)[None, :] // 64
    for o in range(4):
        masks[o] = np.where(kl - ql <= -(o * 128) // 64, 0.0, NEG)
    return ohg, masks


def make_in_maps(inp):
    f = lambda a: np.ascontiguousarray(np.asarray(a, np.float32))
    pt = build_pt(inp)
    shared = {
        "pt": pt,
        "ident": np.eye(128, dtype=np.float32),
        "ada_w": f(inp["ada_w"]),
        "w_pw1": f(inp["conv_w_pw1"][0]),
        "w_pw2": f(inp["conv_w_pw2"][0]),
        "w_r": f(inp["router_w"]),
        "b_r": f(inp["router_b"]).reshape(2, 1, NE),
        "w_gu": f(inp["expert_w_gate_up"]),
        "w_dn": f(inp["expert_w_down"]),
        "b_dn": f(inp["expert_b_down"]),
        "w_kv": f(inp["w_kv"]),
        "w_q": f(inp["attn_w_q"][0]),
        "w_o": f(inp["attn_w_o"][0]),
        "lpt": np.ascontiguousarray(f(inp["attn_lambda"][0]).T),
        "tabs": f(inp["rel_bias_table"]),
        "b_gu": f(inp["expert_b_gate_up"]),
        "tri": np.triu(np.ones((128, 128), np.float32), 1),
        "iota": np.tile(np.arange(NE, dtype=np.float32)[None, :], (128, 1)),
        "ohg": _attn_consts()[0],
        "masks": _attn_consts()[1],
    }
    maps = []
    for b in range(8):
        m = dict(shared)
        m["x"] = f(inp["x"][b])
        m["ct"] = np.ascontiguousarray(f(inp["c"][b]).reshape(8, 128).T)
        maps.append(m)
    return maps


def kernel(**inputs):
    if "nc" not in _CACHE:
        _CACHE["nc"] = build()
    nc = _CACHE["nc"]
    maps = make_in_maps(inputs)
    res = run_bass_kernel_spmd(nc, maps, core_ids=list(range(8)))
    return np.stack([np.asarray(r["out"], np.float32) for r in res.results], axis=0)
```

```python
import math
import numpy as np
import concourse.bass as bass
import concourse.mybir as mybir
from concourse.bass_utils import run_bass_kernel_spmd
from contextlib import ExitStack

F32 = mybir.dt.float32
BF16 = mybir.dt.bfloat16
I32 = mybir.dt.int32
SPARSE = True
AF = mybir.ActivationFunctionType
ALU = mybir.AluOpType
AX = mybir.AxisListType

SAME_ENG_SYNC = True

D = 1024
S = 2048
NE = 32
ALPHA = 4.0 ** 0.25
LN_EPS = 1e-5
BLK = 512
RG = 1152
LAMI = 0.8 - 0.6 * math.exp(-0.3 * 1)
SM_SHIFT = 20.0
NEG = -30000.0
NBLK = S // BLK


class Buf:
    __slots__ = ("name", "w", "r", "rd")

    def __init__(self, name):
        self.name = name
        self.w = None
        self.r = {}
        self.rd = []


class Ins:
    __slots__ = ("eng", "fn", "kind", "cdeps", "dwaits", "sig", "sigidx", "pos", "dsem", "pred", "dord")

    def __init__(self, eng, fn, kind, dsem=None):
        self.pred = None
        self.dord = 0
        self.eng = eng
        self.fn = fn
        self.kind = kind
        self.cdeps = {}
        self.dwaits = {}
        self.sig = False
        self.sigidx = None
        self.pos = None
        self.dsem = dsem


class Prog:
    ENGS = ("pe", "act", "dve", "pool", "sp")

    def __init__(self, nc):
        self.nc = nc
        self.streams = {e: [] for e in self.ENGS}
        self.dma_count = {}
        self.regs = {}

    def _dep(self, ins, p):
        if p is None or p is ins:
            return
        if p.kind == "d":
            s = p.dsem
            ins.dwaits[s] = max(ins.dwaits.get(s, 0), self.dma_count[s])
            return
        if ins.kind == "c" and p.eng == ins.eng:
            if ins.eng == "pe" or not SAME_ENG_SYNC:
                return
        cur = ins.cdeps.get(p.eng)
        if cur is None or cur.pos < p.pos:
            ins.cdeps[p.eng] = p

    def _add(self, ins, reads, writes):
        for b in reads:
            self._dep(ins, b.w)
        for b in writes:
            self._dep(ins, b.w)
            for r in b.r.values():
                self._dep(ins, r)
            for r in b.rd:
                self._dep(ins, r)
        ins.pos = len(self.streams[ins.eng])
        self.streams[ins.eng].append(ins)
        for b in reads:
            if ins.kind == "c":
                b.r[ins.eng] = ins
            else:
                b.rd.append(ins)
        for b in writes:
            b.w = ins
            b.r = {}
            b.rd = []
        return ins

    def op(self, eng, fn, reads=(), writes=(), pred=None):
        ins = Ins(eng, fn, "c")
        ins.pred = pred
        return self._add(ins, reads, writes)

    def dma(self, eng, out, in_, sem, reads=(), writes=(), pred=None, fn=None, **kw):
        self.dma_count.setdefault(sem, 0)
        if fn is None:
            fn = lambda e, out=out, in_=in_, kw=kw: e.dma_start(out=out, in_=in_, **kw)
        ins = Ins(eng, fn, "d", dsem=sem)
        ins.pred = pred
        ins.dord = self.dma_count[sem]
        self._add(ins, reads, writes)
        self.dma_count[sem] += 1
        return ins

    def regload(self, eng, ap, reads=()):
        return self.op(eng, lambda e, ap=ap, eng=eng: e.reg_load(self.regs[eng], ap), reads=reads)

    def final_wait(self, eng, sems):
        ins = Ins(eng, None, "c")
        for s in sems:
            ins.dwaits[s] = self.dma_count[s]
        ins.pos = len(self.streams[eng])
        self.streams[eng].append(ins)

    def emit(self):
        nc = self.nc
        for e in self.ENGS:
            for ins in self.streams[e]:
                for p in ins.cdeps.values():
                    p.sig = True
        for e in self.ENGS:
            n = 0
            for ins in self.streams[e]:
                if ins.sig:
                    n += 1
                    ins.sigidx = n
        with ExitStack() as st:
            csem = {e: st.enter_context(nc.semaphore("c_" + e)) for e in self.ENGS}
            dsem = {s: st.enter_context(nc.semaphore("d_" + s)) for s in self.dma_count}
            block = st.enter_context(nc.Block())

            eobj = {"pe": nc.tensor, "act": nc.scalar, "dve": nc.vector, "pool": nc.gpsimd, "sp": nc.sync}
            for e in self.ENGS:
                self.regs[e] = st.enter_context(eobj[e].register("pr_" + e))

            def run(engname):
                def body(eng):
                    waited = {}

                    def emit_one(ins):
                        for pe_, p in ins.cdeps.items():
                            key = ("c", pe_)
                            if waited.get(key, 0) < p.sigidx:
                                eng.wait_ge(csem[pe_], p.sigidx)
                                waited[key] = p.sigidx
                        for s_, cnt in ins.dwaits.items():
                            key = ("d", s_)
                            if waited.get(key, 0) < cnt:
                                eng.wait_ge(dsem[s_], 16 * cnt)
                                waited[key] = cnt
                        if ins.fn is None:
                            return
                        bi = ins.fn(eng)
                        if ins.kind == "d":
                            bi.then_inc(dsem[ins.dsem], 16)
                        elif ins.sig:
                            bi.then_inc(csem[engname], 1)

                    def body_of(g):
                        for x in g:
                            emit_one(x)

                    def balance(groups):
                        nsig = 0
                        dincs = {}
                        for g in groups:
                            for x in g:
                                if x.kind == "c":
                                    if x.sig:
                                        nsig += 1
                                else:
                                    first, n = dincs.get(x.dsem, (x.dord, 0))
                                    dincs[x.dsem] = (min(first, x.dord), n + 1)
                        if nsig:
                            eng.drain().then_inc(csem[engname], nsig)
                        for s_, (first, n) in dincs.items():
                            if first > 0:
                                eng.wait_ge(dsem[s_], 16 * first)
                            eng.sem_inc(dsem[s_], 16 * n)

                    def emit_seq(groups, lo):
                        i = 0
                        while i < len(groups):
                            g = groups[i]
                            thr = g[0].pred[1]
                            if thr <= lo:
                                body_of(g)
                                i += 1
                                continue
                            k = i
                            while k < len(groups) and groups[k][0].pred[1] >= thr:
                                k += 1
                            run_ = groups[i:k]
                            snap = dict(waited)
                            with eng.If_lt(self.regs[engname], thr + 1):
                                balance(run_)
                            with eng.Else():
                                emit_seq(run_, thr)
                            waited.clear()
                            waited.update(snap)
                            i = k

                    region = []
                    for ins in self.streams[engname]:
                        if ins.pred is None:
                            if region:
                                emit_seq(region, -1)
                                region = []
                            emit_one(ins)
                        else:
                            if region and region[0][0].pred[0] != ins.pred[0]:
                                emit_seq(region, -1)
                                region = []
                            if region and region[-1][0].pred == ins.pred:
                                region[-1].append(ins)
                            else:
                                region.append([ins])
                    if region:
                        emit_seq(region, -1)
                return body

            block.tensor(run("pe"))
            block.scalar(run("act"))
            block.vector(run("dve"))
            block.gpsimd(run("pool"))
            block.sync(run("sp"))


PT = {}
_off = 0


def _pt(name, n):
    global _off
    PT[name] = _off
    _off += n


_pt("ada_b", 96)
_pt("post_g", 32)
_pt("post_b", 32)
_pt("b_pw1", 16)
_pt("w_dw", 248)
_pt("b_dw", 8)
_pt("cln_g", 8)
_pt("cln_b", 8)
_pt("b_pw2", 8)
_pt("b_gu", 1024)
_pt("subln", 8)
NPT = _off


def _cols(v):
    v = np.asarray(v, np.float32).reshape(-1, 128)
    return np.ascontiguousarray(v.T)


def build_pt(inp):
    pt = np.zeros((128, NPT), np.float32)

    def put(name, arr):
        pt[:, PT[name]:PT[name] + arr.shape[1]] = arr

    put("ada_b", np.concatenate([_cols(inp["ada_b"][l]) for l in range(2)], axis=1))
    put("post_g", np.concatenate([_cols(inp["post_ln_g"][l, s]) for l in range(2) for s in range(2)], axis=1))
    put("post_b", np.concatenate([_cols(inp["post_ln_b"][l, s]) for l in range(2) for s in range(2)], axis=1))
    put("b_pw1", _cols(inp["conv_b_pw1"][0]))
    wdw = np.asarray(inp["conv_w_dw"][0], np.float32)
    put("w_dw", np.ascontiguousarray(wdw.reshape(31, 8, 128).transpose(2, 1, 0).reshape(128, 248)))
    put("b_dw", _cols(inp["conv_b_dw"][0]))
    put("cln_g", _cols(inp["conv_ln_g"][0]))
    put("cln_b", _cols(inp["conv_ln_b"][0]))
    put("b_pw2", _cols(inp["conv_b_pw2"][0]))
    put("b_gu", _cols(np.asarray(inp["expert_b_gate_up"], np.float32).reshape(-1)))
    put("subln", np.tile(np.asarray(inp["attn_subln_g"][0], np.float32).reshape(128, 1), (1, 8)))
    return pt


def build(stop_after=None):
    nc = bass.Bass("TRN2", target_bir_lowering=False)

    def din(name, shape, dt=F32):
        return nc.dram_tensor(name, shape, dt, kind="ExternalInput").ap()

    x_d = din("x", [S, D])
    ct_d = din("ct", [128, 8])
    pt_d = din("pt", [128, NPT])
    ident_d = din("ident", [128, 128])
    ada_w_d = din("ada_w", [2, D, 6 * D])
    wpw1_d = din("w_pw1", [D, 2 * D])
    wpw2_d = din("w_pw2", [D, D])
    wr_d = din("w_r", [2, D, NE])
    br_d = din("b_r", [2, 1, NE])
    wgu_d = din("w_gu", [2, NE, D, 2 * D])
    wdn_d = din("w_dn", [2, NE, D, D])
    bdn_d = din("b_dn", [2, NE, D])
    wkv_d = din("w_kv", [D, 2 * D])
    wq_d = din("w_q", [D, D])
    wo_d = din("w_o", [D, D])
    lpt_d = din("lpt", [64, 4])
    tabs_d = din("tabs", [32, 8])
    ohg_d = din("ohg", [32, RG])
    masks_d = din("masks", [4, 128, BLK])
    bgu_d = din("b_gu", [2, NE, 2 * D])
    tri_d = din("tri", [128, 128])
    iota_d = din("iota", [128, NE])
    xs_d = nc.dram_tensor("xs_scratch", [NE * S, D], BF16).ap()
    ys_d = nc.dram_tensor("ys_scratch", [NE * S, D], F32).ap()
    gs_t = nc.dram_tensor("gs_scratch", [8, 128, RG], F32)
    gs_d = gs_t.ap()
    out_d = nc.dram_tensor("out", [S, D], F32, kind="ExternalOutput").ap()

    P = Prog(nc)
    with ExitStack() as st:
        def T(name, shape, dt=F32):
            return st.enter_context(nc.sbuf_tensor("s_" + name, shape, dt))

        region = {}

        def claim(name, new_bufs):
            old = [b for b in region.get(name, []) if b not in new_bufs]
            for nb in new_bufs:
                for ob in old:
                    cands = list(ob.r.values()) + ([ob.w] if ob.w is not None else [])
                    for p in cands:
                        if p.kind == "d":
                            nb.rd.append(p)
                        else:
                            cur = nb.r.get(p.eng)
                            if cur is None or cur.pos < p.pos:
                                nb.r[p.eng] = p
                    nb.rd.extend(ob.rd)
            region[name] = list(new_bufs)

        R_x = T("R_x", [128, 8 * S])
        R_h = T("R_h", [128, 8192])
        R_w = T("R_w", [128, 12288])
        R_a = T("R_a", [128, 2048])
        R_s = T("R_s", [128, 5 * BLK])
        R_m = T("R_m", [128, 6144])
        GT = R_m[0:32, 0:2048]
        gbT = R_m[:, 2048:4096]
        GTb = R_m[0:32, 4096:5120].bitcast(BF16)
        gsel = R_m[0:32, 5120:6144]
        lpT = T("lpT", [64, 4])
        tabs = T("tabs", [32, 8])
        Mall = T("Mall", [128, 16 * NE], BF16)
        posI = T("posI", [128, 64], I32)
        gates4 = T("gates4", [128, 64])
        cntF = T("cntF", [1, NE])
        cntI = T("cntI", [1, NE], I32)
        trib = T("trib", [128, 128], BF16)
        identb = T("identb", [128, 128], BF16)
        iotaT = T("iotaT", [128, NE])
        iotaP = T("iotaP", [128, NE])
        rt = T("rt", [128, 2 * 400])
        nlam = T("nlam", [128, 4])
        negc = T("negc", [128, 1])
        gsub = T("gsub", [128, 1])
        pt = T("pt", [128, NPT])
        ident = T("ident", [128, 128])
        ones = T("ones", [128, 128])
        onesb = T("onesb", [128, 128], BF16)
        cT = T("cT", [128, 8])
        condb = T("condb", [128, 8], BF16)
        mods = T("mods", [128, 96])
        g1b = T("g1b", [128, 8])
        epsT = T("epsT", [128, 1])
        stage = R_a
        wr_s = T("wr_s", [128, 2 * 8 * NE], BF16)
        br_s = T("br_s", [1, 2 * NE], BF16)
        bdn_s = T("bdn_s", [32, 2 * D], BF16)
        lg = T("lg", [128, 2 * 8 * NE])
        mx8 = T("mx8", [128, 32])

        ps = [st.enter_context(nc.psum_tensor(f"ps{i}", [128, 512], F32)) for i in range(8)]
        bps = [Buf(f"ps{i}") for i in range(8)]

        xT = R_x[:].rearrange("p (c t) -> p c t", c=8)
        bxT = [Buf(f"xT{b}") for b in range(NBLK)]
        b_init = Buf("init")
        b_mods = Buf("mods")

        def blk(tb):
            return slice(tb * BLK, (tb + 1) * BLK)

        P.dma("sp", pt[:], pt_d[:, :], "init", writes=[b_init])
        P.dma("sp", ident[:], ident_d[:, :], "init", writes=[b_init])
        P.dma("sp", cT[:], ct_d[:, :], "init", writes=[b_init])
        P.dma("pool", wr_s[:].rearrange("p (l k e) -> p l k e", l=2, k=8), wr_d.rearrange("l (k p) e -> p l k e", p=128), "initp", writes=[b_init])
        P.dma("pool", br_s[:], br_d.rearrange("l o e -> o (l e)"), "initp", writes=[b_init])
        P.dma("pool", bdn_s[:].rearrange("e (l d) -> e l d", l=2), bdn_d.rearrange("l e d -> e l d"), "initp", writes=[b_init])
        b_c = Buf("consts")
        P.op("dve", lambda e: e.memset(ones[:], 1.0), writes=[b_c])
        P.op("dve", lambda e: e.memset(onesb[:], 1.0), writes=[b_c])
        P.op("dve", lambda e: e.memset(epsT[:], LN_EPS), writes=[b_c])
        P.op("dve", lambda e: e.memset(negc[:], -SM_SHIFT), writes=[b_c])
        P.dma("sp", lpT[:], lpt_d[:, :], "init", writes=[b_init])
        P.dma("sp", tabs[:], tabs_d[:, :], "init", writes=[b_init])
        P.dma("pool", trib[:], tri_d[:, :], "initp", writes=[b_init])
        P.dma("sp", iotaT[:], iota_d[:, :], "init", writes=[b_init])
        blin = pt[:, PT["b_gu"]:PT["b_gu"] + 1024].rearrange("p (g j) -> p g j", j=16)[:, :, 8:16]
        P.op("dve", lambda e: e.tensor_scalar_add(out=blin, in0=blin, scalar1=1.0), reads=[b_init], writes=[b_init])
        P.op("act", lambda e: e.activation(out=condb[:], in_=cT[:], func=AF.Silu), reads=[b_init], writes=[b_c])
        P.op("act", lambda e: e.activation(out=identb[:], in_=ident[:], func=AF.Copy), reads=[b_init], writes=[b_c])
        P.op("act", lambda e: e.activation(out=iotaP[:], in_=iotaT[:], func=AF.Identity, bias=1000.0, scale=1.0), reads=[b_init], writes=[b_c])

        units = []
        for u in range(2):
            base = u * 6144
            units.append(dict(
                raw=R_w[:, base:base + 6144],
                buf=Buf(f"unit{u}"), sem=f"wu{u}"))
        ucount = [0]
        claim("Rw", [u["buf"] for u in units])

        def next_unit():
            u = units[ucount[0] % 2]
            ucount[0] += 1
            return u

        for l in range(2):
            for nb in range(6):
                u = next_unit()
                wv = u["raw"][:, 0:4096].bitcast(BF16).rearrange("p (k n) -> p k n", k=8)
                P.dma("pool", wv, ada_w_d[l, :, nb * 1024:(nb + 1) * 1024].rearrange("(k p) n -> p k n", p=128), u["sem"], writes=[u["buf"]])
                for j in range(8):
                    col = nb * 8 + j
                    for k in range(8):
                        P.op("pe", lambda e, wv=wv, j=j, k=k, col=col, l=l: e.matmul(out=ps[l][:, col:col + 1], lhsT=wv[:, k, j * 128:(j + 1) * 128], rhs=condb[:, k:k + 1], start=(k == 0), stop=(k == 7)),
                             reads=[u["buf"], b_c], writes=[bps[l]])
            P.op("dve", lambda e, l=l: e.tensor_tensor(out=mods[:, l * 48:(l + 1) * 48], in0=ps[l][:, 0:48], in1=pt[:, PT["ada_b"] + l * 48:PT["ada_b"] + (l + 1) * 48], op=ALU.add),
                 reads=[bps[l], b_init], writes=[b_mods])
            for o in (8, 32):
                P.op("dve", lambda e, l=l, o=o: e.tensor_scalar_add(out=mods[:, l * 48 + o:l * 48 + o + 8], in0=mods[:, l * 48 + o:l * 48 + o + 8], scalar1=1.0),
                     reads=[b_mods], writes=[b_mods])
        P.op("dve", lambda e: e.tensor_tensor(out=g1b[:], in0=mods[:, 16:24], in1=pt[:, PT["b_pw2"]:PT["b_pw2"] + 8], op=ALU.mult), reads=[b_mods, b_init], writes=[b_mods])

        def mcol(l, o, c):
            return mods[:, l * 48 + o + c:l * 48 + o + c + 1]

        b_stage = [Buf("stage0"), Buf("stage1")]
        claim("Ra", b_stage)
        for i in range(16):
            sl = i % 2
            sv = stage[:, sl * D:(sl + 1) * D]
            P.dma("sp", sv, x_d[i * 128:(i + 1) * 128, :], f"xin{sl}", writes=[b_stage[sl]])
            for g in range(2):
                bank = 2 + g
                for cc in range(4):
                    c = g * 4 + cc
                    P.op("pe", lambda e, sv=sv, c=c, cc=cc, bank=bank: e.transpose(out=ps[bank][:, cc * 128:(cc + 1) * 128], in_=sv[:, c * 128:(c + 1) * 128], identity=ident[:]),
                         reads=[b_stage[sl], b_init], writes=[bps[bank]])
                eng = "act" if g == 0 else "dve"
                if eng == "act":
                    P.op("act", lambda e, g=g, i=i, bank=bank: e.activation(out=xT[:, g * 4:(g + 1) * 4, i * 128:(i + 1) * 128], in_=ps[bank][:].rearrange("p (c t) -> p c t", c=4), func=AF.Copy),
                         reads=[bps[bank]], writes=[bxT[i // 4]])
                else:
                    P.op("dve", lambda e, g=g, i=i, bank=bank: e.tensor_copy(out=xT[:, g * 4:(g + 1) * 4, i * 128:(i + 1) * 128], in_=ps[bank][:].rearrange("p (c t) -> p c t", c=4)),
                         reads=[bps[bank]], writes=[bxT[i // 4]])

        sq = [R_s[:, 0:BLK], R_s[:, BLK:2 * BLK]]
        b_sq = [Buf("sq0"), Buf("sq1")]
        mean_t = R_s[:, 2 * BLK:3 * BLK]
        rstd_t = R_s[:, 3 * BLK:4 * BLK]
        nmr_t = R_s[:, 4 * BLK:5 * BLK]
        b_stat = Buf("stat")
        sqc = [0]

        def _clone(b):
            nb = Buf(b.name + "_c")
            nb.w = b.w
            nb.r = dict(b.r)
            nb.rd = list(b.rd)
            return nb

        def layer_norm_block(src, b_src, dst, b_dst, gcol, bcol, func, bank_s, bank_q):
            inplace = b_src is b_dst
            cbuf = [_clone(b_src) for _ in range(8)]
            dbuf = cbuf if inplace else [_clone(b_dst) for _ in range(8)]
            for c in range(8):
                s = sqc[0] % 2
                sqc[0] += 1
                P.op("act", lambda e, c=c, s=s: e.activation(out=sq[s], in_=src(c), func=AF.Square), reads=[cbuf[c]], writes=[b_sq[s]])
                P.op("pe", lambda e, c=c: e.matmul(out=ps[bank_s][:], lhsT=ones[:], rhs=src(c), start=(c == 0), stop=(c == 7)), reads=[cbuf[c], b_c], writes=[bps[bank_s]])
                P.op("pe", lambda e, c=c, s=s: e.matmul(out=ps[bank_q][:], lhsT=ones[:], rhs=sq[s], start=(c == 0), stop=(c == 7)), reads=[b_sq[s], b_c], writes=[bps[bank_q]])
            P.op("dve", lambda e: e.tensor_scalar_mul(out=mean_t, in0=ps[bank_s][:], scalar1=1.0 / D), reads=[bps[bank_s]], writes=[b_stat])
            P.op("dve", lambda e: e.tensor_tensor(out=nmr_t, in0=mean_t, in1=mean_t, op=ALU.mult), reads=[b_stat], writes=[b_stat])
            P.op("dve", lambda e: e.scalar_tensor_tensor(out=rstd_t, in0=ps[bank_q][:], scalar=1.0 / D, in1=nmr_t, op0=ALU.mult, op1=ALU.subtract), reads=[bps[bank_q], b_stat], writes=[b_stat])
            P.op("act", lambda e: e.activation(out=rstd_t, in_=rstd_t, func=AF.Sqrt, bias=epsT[:, 0:1], scale=1.0), reads=[b_stat, b_c], writes=[b_stat])
            P.op("dve", lambda e: e.reciprocal(out=rstd_t, in_=rstd_t), reads=[b_stat], writes=[b_stat])
            P.op("dve", lambda e: e.scalar_tensor_tensor(out=nmr_t, in0=mean_t, scalar=-1.0, in1=rstd_t, op0=ALU.mult, op1=ALU.mult), reads=[b_stat], writes=[b_stat])
            last = {}
            for c in range(8):
                last["d1"] = P.op("dve", lambda e, c=c: e.tensor_tensor(out=src(c), in0=src(c), in1=rstd_t, op=ALU.mult), reads=[cbuf[c], b_stat], writes=[cbuf[c]])
                last["d2"] = P.op("dve", lambda e, c=c: e.tensor_tensor(out=src(c), in0=src(c), in1=nmr_t, op=ALU.add), reads=[cbuf[c], b_stat], writes=[cbuf[c]])
                last["a"] = P.op("act", lambda e, c=c: e.activation(out=dst(c), in_=src(c), func=func, scale=pt[:, gcol + c:gcol + c + 1], bias=pt[:, bcol + c:bcol + c + 1]),
                                 reads=[cbuf[c], b_init], writes=[dbuf[c]])
            if inplace:
                b_src.w = last["a"]
                b_src.r = {}
                b_src.rd = []
            else:
                b_src.w = last["d2"]
                b_src.r = {"act": last["a"]}
                b_src.rd = []
                b_dst.w = last["a"]
                b_dst.r = {}
                b_dst.rd = []

        def conv_sublayer():
            l = 0
            wpw1 = R_w[:, 0:8192].bitcast(BF16).rearrange("p (k n) -> p k n", k=8)
            wpw2 = R_w[:, 8192:12288].bitcast(BF16).rearrange("p (k n) -> p k n", k=8)
            b_w1 = [Buf(f"wpw1_{i}") for i in range(4)]
            b_w2 = Buf("wpw2")
            claim("Rw", b_w1 + [b_w2])
            for i in range(4):
                P.dma("pool", wpw1[:, :, i * 512:(i + 1) * 512], wpw1_d[:, i * 512:(i + 1) * 512].rearrange("(k p) n -> p k n", p=128), "wconv",
                      writes=[b_w1[i]])
            P.dma("pool", wpw2, wpw2_d.rearrange("(k p) n -> p k n", p=128), "wconv", writes=[b_w2])
            hblk = R_h[:, 0:2048].bitcast(BF16).rearrange("p (c t) -> p c t", c=8)
            sblk = R_h[:, 2048:4096].bitcast(BF16).rearrange("p (c t) -> p c t", c=8)
            vblk = R_h[:, 4096:8192].rearrange("p (c t) -> p c t", c=8)
            ub = [R_a[:, 0:271].bitcast(BF16), R_a[:, 272:543].bitcast(BF16)]
            halo = R_a[:, 1084:1084 + 120].bitcast(BF16).rearrange("p (c t) -> p c t", c=8)
            diag = [R_m[:, 0:1984].bitcast(BF16).rearrange("p (t n) -> p t n", t=31), R_m[:, 2048:2048 + 1984].bitcast(BF16).rearrange("p (t n) -> p t n", t=31)]
            b_diag = [Buf("diag0"), Buf("diag1")]
            claim("Rm", b_diag)
            sgt = [R_a[:, 1324:1324 + 512], R_s[:, 0:BLK]]
            b_h, b_s, b_v = Buf("hblk"), Buf("sblk"), Buf("vblk")
            b_u = [Buf("u0"), Buf("u1")]
            b_halo = Buf("halo")
            b_sg = [Buf("sg0"), b_sq[0]]
            claim("Rs", [b_sq[0], b_sq[1], b_stat])
            claim("Rh", [b_h, b_s, b_v])
            claim("Ra", [b_u[0], b_u[1], b_halo, b_sg[0]])
            P.op("pool", lambda e: e.memset(halo, 0.0), writes=[b_halo])
            wdw0 = PT["w_dw"]
            for tb in range(NBLK):
                for c in range(8):
                    P.op("act", lambda e, c=c, tb=tb: e.activation(out=hblk[:, c, :], in_=xT[:, c, blk(tb)], func=AF.Identity, scale=mcol(l, 8, c), bias=mcol(l, 0, c)),
                         reads=[bxT[tb], b_mods], writes=[b_h])
                def pw1(j):
                    ba, bg = (0, 1) if j % 2 == 0 else (2, 3)
                    s = j % 2
                    dg = diag[s]
                    P.op("dve", lambda e: e.tensor_tensor(out=dg, in0=identb[:].unsqueeze(1).to_broadcast([128, 31, 128]),
                                                          in1=pt[:, wdw0 + j * 31:wdw0 + (j + 1) * 31].unsqueeze(2).to_broadcast([128, 31, 128]), op=ALU.mult),
                         reads=[b_c, b_init], writes=[b_diag[s]])
                    for k in range(8):
                        P.op("pe", lambda e, k=k: e.matmul(out=ps[ba][:], lhsT=wpw1[:, k, j * 128:(j + 1) * 128], rhs=hblk[:, k, :], start=(k == 0), stop=(k == 7)),
                             reads=[b_w1[j // 4], b_h], writes=[bps[ba]])
                    for k in range(8):
                        P.op("pe", lambda e, k=k: e.matmul(out=ps[bg][:], lhsT=wpw1[:, k, 1024 + j * 128:1024 + (j + 1) * 128], rhs=hblk[:, k, :], start=(k == 0), stop=(k == 7)),
                             reads=[b_w1[2 + j // 4], b_h], writes=[bps[bg]])

                def glu_conv(j):
                    ba, bg = (0, 1) if j % 2 == 0 else (2, 3)
                    s = j % 2
                    u_ = ub[s]
                    dg = diag[s]
                    P.op("act", lambda e: e.activation(out=sgt[s], in_=ps[bg][:], func=AF.Sigmoid, bias=pt[:, PT["b_pw1"] + 8 + j:PT["b_pw1"] + 9 + j], scale=1.0),
                         reads=[bps[bg], b_init], writes=[b_sg[s]])
                    P.op("pool", lambda e: e.tensor_copy(out=u_[:, 0:30], in_=halo[:, j, :]), reads=[b_halo], writes=[b_u[s]])
                    P.op("dve", lambda e: e.scalar_tensor_tensor(out=u_[:, 30:542], in0=ps[ba][:], scalar=pt[:, PT["b_pw1"] + j:PT["b_pw1"] + j + 1], in1=sgt[s], op0=ALU.add, op1=ALU.mult),
                         reads=[bps[ba], b_sg[s], b_init], writes=[b_u[s]])
                    P.op("pool", lambda e: e.tensor_copy(out=halo[:, j, :], in_=u_[:, 512:542]), reads=[b_u[s]], writes=[b_halo])
                    cb = 6 + s
                    for tap in range(31):
                        P.op("pe", lambda e, tap=tap: e.matmul(out=ps[cb][:], lhsT=dg[:, tap, :], rhs=u_[:, tap:tap + 512], start=(tap == 0), stop=(tap == 30)),
                             reads=[b_diag[s], b_u[s]], writes=[bps[cb]])
                    P.op("dve", lambda e: e.tensor_scalar(out=vblk[:, j, :], in0=ps[cb][:], scalar1=pt[:, PT["b_dw"] + j:PT["b_dw"] + j + 1], scalar2=None, op0=ALU.add),
                         reads=[bps[cb], b_init], writes=[b_v])

                pw1(0)
                for j in range(8):
                    if j + 1 < 8:
                        pw1(j + 1)
                    glu_conv(j)
                layer_norm_block(lambda c: vblk[:, c, :], b_v, lambda c: sblk[:, c, :], b_s, PT["cln_g"], PT["cln_b"], AF.Silu, 4, 5)
                for d in range(8):
                    bank = 6 + d % 2
                    for k in range(8):
                        P.op("pe", lambda e, d=d, k=k, bank=bank: e.matmul(out=ps[bank][:], lhsT=wpw2[:, k, d * 128:(d + 1) * 128], rhs=sblk[:, k, :], start=(k == 0), stop=(k == 7)),
                             reads=[b_w2, b_s], writes=[bps[bank]])
                    s = d % 2
                    P.op("act", lambda e, d=d, bank=bank, s=s: e.activation(out=sgt[s], in_=ps[bank][:], func=AF.Identity, scale=mcol(l, 16, d), bias=g1b[:, d:d + 1]),
                         reads=[bps[bank], b_mods], writes=[b_sg[s]])
                    P.op("dve", lambda e, d=d, tb=tb, s=s: e.scalar_tensor_tensor(out=xT[:, d, blk(tb)], in0=xT[:, d, blk(tb)], scalar=ALPHA, in1=sgt[s], op0=ALU.mult, op1=ALU.add),
                         reads=[b_sg[s], bxT[tb]], writes=[bxT[tb]])
                pg = PT["post_g"] + (l * 2 + 0) * 8
                pb = PT["post_b"] + (l * 2 + 0) * 8
                layer_norm_block(lambda c, tb=tb: xT[:, c, blk(tb)], bxT[tb], lambda c, tb=tb: xT[:, c, blk(tb)], bxT[tb], pg, pb, AF.Identity, 4, 5)

        def moe_sublayer(l):
            hT = R_h[:].bitcast(BF16).rearrange("p (c t) -> p c t", c=8)
            b_hT = [Buf(f"hT{b}") for b in range(NBLK)]
            b_GT = Buf("GT")
            b_lg = Buf("lg")
            actT = [R_a[:, 0:1024].bitcast(BF16).rearrange("p (c t) -> p c t", c=4), R_a[:, 1024:2048].bitcast(BF16).rearrange("p (c t) -> p c t", c=4)]
            b_act = [Buf("act0"), Buf("act1")]
            gc = R_s[:, 0:BLK]
            sgm = R_s[:, BLK:2 * BLK]
            lin = R_s[:, 2 * BLK:3 * BLK]
            b_gc, b_sgm, b_lin = b_sq[0], b_sq[1], b_stat
            b_gb = Buf("gb")
            b_gsel = [Buf("gsel0"), Buf("gsel1")]
            claim("Rh", b_hT)
            claim("Rm", [b_GT, b_gb] + b_gsel)
            claim("Rs", [b_sq[0], b_sq[1], b_stat])
            claim("Ra", b_act)
            claim("Rw", [u["buf"] for u in units])
            for tb in range(NBLK):
                for c in range(8):
                    P.op("act", lambda e, c=c, tb=tb: e.activation(out=hT[:, c, blk(tb)], in_=xT[:, c, blk(tb)], func=AF.Identity, scale=mcol(l, 32, c), bias=mcol(l, 24, c)),
                         reads=[bxT[tb], b_mods], writes=[b_hT[tb]])
                for c in range(8):
                    P.op("dve", lambda e, c=c, tb=tb: e.tensor_scalar_mul(out=xT[:, c, blk(tb)], in0=xT[:, c, blk(tb)], scalar1=ALPHA),
                         reads=[bxT[tb]], writes=[bxT[tb]])
            wr = wr_s[:].rearrange("p (l k e) -> p l k e", l=2, k=8)
            for i in range(16):
                tb = i // 4
                tsl = slice(i * 128, (i + 1) * 128)
                for k in range(8):
                    P.op("pe", lambda e, k=k, tsl=tsl: e.matmul(out=ps[7][:, 0:NE], lhsT=hT[:, k, tsl], rhs=wr[:, l, k, :], start=(k == 0), stop=False),
                         reads=[b_hT[tb], b_init], writes=[bps[7]])
                P.op("pe", lambda e: e.matmul(out=ps[7][:, 0:NE], lhsT=onesb[0:1, :], rhs=br_s[0:1, l * NE:(l + 1) * NE], start=False, stop=True),
                     reads=[b_c, b_init], writes=[bps[7]])
                L = lg[:, 0:NE]
                M = lg[:, NE:2 * NE]
                E = lg[:, 2 * NE:3 * NE]
                G = lg[:, 3 * NE:4 * NE]
                P.op("dve", lambda e, L=L: e.tensor_copy(out=L, in_=ps[7][:, 0:NE]), reads=[bps[7]], writes=[b_lg])
                P.op("dve", lambda e, L=L: e.max(out=mx8[:, 0:8], in_=L), reads=[b_lg], writes=[b_lg])
                P.op("dve", lambda e, L=L, M=M: e.tensor_scalar(out=M, in0=L, scalar1=mx8[:, 3:4], scalar2=None, op0=ALU.is_ge), reads=[b_lg], writes=[b_lg])
                P.op("dve", lambda e: e.tensor_scalar_mul(out=mx8[:, 8:9], in0=mx8[:, 0:1], scalar1=-1.0), reads=[b_lg], writes=[b_lg])
                P.op("act", lambda e, L=L, E=E: e.activation(out=E, in_=L, func=AF.Exp, bias=mx8[:, 8:9], scale=1.0), reads=[b_lg], writes=[b_lg])
                P.op("dve", lambda e, M=M, E=E: e.tensor_tensor(out=E, in0=E, in1=M, op=ALU.mult), reads=[b_lg], writes=[b_lg])
                P.op("dve", lambda e, E=E: e.reduce_sum(out=mx8[:, 9:10], in_=E, axis=AX.X), reads=[b_lg], writes=[b_lg])
                P.op("dve", lambda e: e.reciprocal(out=mx8[:, 10:11], in_=mx8[:, 9:10]), reads=[b_lg], writes=[b_lg])
                P.op("dve", lambda e, E=E, G=G: e.tensor_scalar(out=G, in0=E, scalar1=mx8[:, 10:11], scalar2=None, op0=ALU.mult), reads=[b_lg], writes=[b_lg])
                P.op("pe", lambda e, G=G: e.transpose(out=ps[6][0:NE, 0:128], in_=G, identity=ident[:]), reads=[b_lg, b_init], writes=[bps[6]])
                P.op("act", lambda e, tsl=tsl: e.activation(out=GT[:, tsl], in_=ps[6][0:NE, 0:128], func=AF.Copy), reads=[bps[6]], writes=[b_GT])
                P.op("dve", lambda e, tsl=tsl: e.tensor_copy(out=GTb[:, tsl], in_=ps[6][0:NE, 0:128]), reads=[bps[6]], writes=[b_GT])

            def issue_unit(uidx):
                e_, half = uidx // 2, uidx % 2
                u = next_unit()
                g = u["raw"][:, 0:2048].bitcast(BF16).rearrange("p (k n) -> p k n", k=8)
                lw = u["raw"][:, 2048:4096].bitcast(BF16).rearrange("p (k n) -> p k n", k=8)
                dw = u["raw"][:, 4096:6144].bitcast(BF16).rearrange("p (f n) -> p f n", f=4)
                P.dma("pool", g, wgu_d[l, e_, :, half * 512:(half + 1) * 512].rearrange("(k p) n -> p k n", p=128), u["sem"], writes=[u["buf"]])
                P.dma("pool", lw, wgu_d[l, e_, :, 1024 + half * 512:1024 + (half + 1) * 512].rearrange("(k p) n -> p k n", p=128), u["sem"], writes=[u["buf"]])
                P.dma("pool", dw, wdn_d[l, e_, half * 512:(half + 1) * 512, :].rearrange("(f p) n -> p f n", p=128), u["sem"], writes=[u["buf"]])
                return (u, g, lw, dw)

            NU = 2 * NE
            pending = [issue_unit(0)]
            actc = [0]
            for uidx in range(NU):
                if uidx + 1 < NU:
                    pending.append(issue_unit(uidx + 1))
                u, g, lw, dw = pending.pop(0)
                e_, half = uidx // 2, uidx % 2
                bcol = (l * NE + e_) * 16
                if half == 0:
                    for tb in range(NBLK):
                        gs = gsel[:, (tb % 2) * BLK:(tb % 2 + 1) * BLK]
                        P.op("dve", lambda e, e_=e_, tb=tb, gs=gs: e.tensor_scalar(out=gs, in0=GT[:, blk(tb)], scalar1=ident[0:NE, e_:e_ + 1], scalar2=None, op0=ALU.mult), reads=[b_GT, b_init], writes=[b_gsel[tb % 2]])
                        P.op("pe", lambda e, gs=gs: e.matmul(out=ps[6][:], lhsT=ones[0:NE, :], rhs=gs, start=True, stop=True), reads=[b_gsel[tb % 2], b_c], writes=[bps[6]])
                        P.op("act", lambda e, tb=tb: e.activation(out=gbT[:, blk(tb)], in_=ps[6][:], func=AF.Copy), reads=[bps[6]], writes=[b_gb])
                for tb in range(NBLK):
                    a = actc[0] % 2
                    actc[0] += 1
                    for fc in range(4):
                        pg_, pl_ = (0, 1) if fc % 2 == 0 else (2, 3)
                        for k in range(8):
                            P.op("pe", lambda e, g=g, k=k, fc=fc, tb=tb, pg_=pg_: e.matmul(out=ps[pg_][:], lhsT=g[:, k, fc * 128:(fc + 1) * 128], rhs=hT[:, k, blk(tb)], start=(k == 0), stop=(k == 7)),
                                 reads=[u["buf"], b_hT[tb]], writes=[bps[pg_]])
                        for k in range(8):
                            P.op("pe", lambda e, lw=lw, k=k, fc=fc, tb=tb, pl_=pl_: e.matmul(out=ps[pl_][:], lhsT=lw[:, k, fc * 128:(fc + 1) * 128], rhs=hT[:, k, blk(tb)], start=(k == 0), stop=(k == 7)),
                                 reads=[u["buf"], b_hT[tb]], writes=[bps[pl_]])
                        jg = bcol + half * 4 + fc
                        jl = bcol + 8 + half * 4 + fc
                        P.op("dve", lambda e, pg_=pg_, jg=jg: e.tensor_scalar(out=gc, in0=ps[pg_][:], scalar1=pt[:, PT["b_gu"] + jg:PT["b_gu"] + jg + 1], scalar2=7.0, op0=ALU.add, op1=ALU.min),
                             reads=[bps[pg_], b_init], writes=[b_gc])
                        P.op("act", lambda e: e.activation(out=sgm, in_=gc, func=AF.Sigmoid, scale=1.702), reads=[b_gc], writes=[b_sgm])
                        P.op("dve", lambda e, pl_=pl_, jl=jl: e.tensor_scalar(out=lin, in0=ps[pl_][:], scalar1=pt[:, PT["b_gu"] + jl:PT["b_gu"] + jl + 1], scalar2=8.0, op0=ALU.add, op1=ALU.min),
                             reads=[bps[pl_], b_init], writes=[b_lin])
                        P.op("dve", lambda e: e.tensor_tensor(out=gc, in0=gc, in1=sgm, op=ALU.mult), reads=[b_gc, b_sgm], writes=[b_gc])
                        P.op("dve", lambda e: e.scalar_tensor_tensor(out=lin, in0=lin, scalar=-6.0, in1=gc, op0=ALU.max, op1=ALU.mult), reads=[b_lin, b_gc], writes=[b_lin])
                        P.op("dve", lambda e, a=a, fc=fc, tb=tb: e.tensor_tensor(out=actT[a][:, fc, :], in0=lin, in1=gbT[:, blk(tb)], op=ALU.mult), reads=[b_lin, b_gb], writes=[b_act[a]])
                    for d in range(8):
                        bank = 4 + d % 2
                        for fc in range(4):
                            last = (fc == 3) and not (uidx == 0)
                            P.op("pe", lambda e, dw=dw, fc=fc, d=d, a=a, bank=bank, last=last: e.matmul(out=ps[bank][:], lhsT=dw[:, fc, d * 128:(d + 1) * 128], rhs=actT[a][:, fc, :], start=(fc == 0), stop=last),
                                 reads=[u["buf"], b_act[a]], writes=[bps[bank]])
                        if uidx == 0:
                            P.op("pe", lambda e, d=d, tb=tb, bank=bank: e.matmul(out=ps[bank][:], lhsT=bdn_s[:, l * D + d * 128:l * D + (d + 1) * 128], rhs=GTb[:, blk(tb)], start=False, stop=True),
                                 reads=[b_init, b_GT], writes=[bps[bank]])
                        P.op("dve", lambda e, d=d, tb=tb, bank=bank: e.scalar_tensor_tensor(out=xT[:, d, blk(tb)], in0=ps[bank][:], scalar=mcol(l, 40, d), in1=xT[:, d, blk(tb)], op0=ALU.mult, op1=ALU.add),
                             reads=[bps[bank], b_mods, bxT[tb]], writes=[bxT[tb]])
            pg = PT["post_g"] + (l * 2 + 1) * 8
            pb = PT["post_b"] + (l * 2 + 1) * 8
            for tb in range(NBLK):
                layer_norm_block(lambda c, tb=tb: xT[:, c, blk(tb)], bxT[tb], lambda c, tb=tb: xT[:, c, blk(tb)], bxT[tb], pg, pb, AF.Identity, 0, 1)

        def attn_sublayer():
            l = 1
            xb = R_h[:].bitcast(BF16).rearrange("p (c t) -> p c t", c=8)
            h1b = R_w[:, 0:8192].bitcast(BF16).rearrange("p (c t) -> p c t", c=8)
            KT = R_w[:, 8192:9216].bitcast(BF16)
            QT = R_w[:, 9216:10240].bitcast(BF16)
            Vh = R_w[:, 10240:11264].bitcast(BF16).rearrange("p (i e) -> p i e", i=16)
            AT = R_w[:, 11264:12288].bitcast(BF16)
            Zs = R_m[:, 4096:5120]
            masks = R_a[:].rearrange("p (o q) -> p o q", o=4)
            PTt = [R_s[:, 0:256].bitcast(BF16), R_s[:, 256:512].bitcast(BF16)]
            tmp = [R_s[:, 512:1024], R_s[:, 1024:1536]]
            o0 = R_s[:, 1536:2048]
            rsc = R_s[:, 2048:2560]
            b_xb = [Buf(f"xb{i}") for i in range(NBLK)]
            b_h1 = [Buf(f"h1b{i}") for i in range(NBLK)]
            b_KT, b_QT, b_V, b_AT = Buf("KT"), Buf("QT"), Buf("Vh"), Buf("AT")
            b_Zs, b_mask = Buf("Zs"), Buf("masks")
            b_PT = [Buf("PT0"), Buf("PT1")]
            b_tmp = [Buf("tmp0"), Buf("tmp1")]
            b_o0, b_rsc = Buf("o0"), Buf("rsc")
            b_wt = [Buf("awt0"), Buf("awt1")]
            b_gs = [Buf(f"gs{h}") for h in range(8)]
            b_lam = Buf("lam")
            claim("Rh", b_xb)
            claim("Rw", b_h1 + [b_KT, b_QT, b_V, b_AT])
            claim("Rm", b_wt + [b_Zs])
            claim("Ra", [b_mask])
            P.dma("sp", masks, masks_d.rearrange("o p q -> p o q"), "amask", writes=[b_mask])
            P.op("dve", lambda e: e.tensor_tensor(out=lpT[:, 0:1], in0=lpT[:, 0:1], in1=lpT[:, 1:2], op=ALU.mult), reads=[b_init], writes=[b_lam])
            P.op("dve", lambda e: e.tensor_tensor(out=lpT[:, 1:2], in0=lpT[:, 2:3], in1=lpT[:, 3:4], op=ALU.mult), reads=[b_init, b_lam], writes=[b_lam])
            P.op("pe", lambda e: e.matmul(out=ps[7][:, 0:2], lhsT=ones[0:64, :], rhs=lpT[:, 0:2], start=True, stop=True), reads=[b_lam, b_c], writes=[bps[7]])
            P.op("act", lambda e: e.activation(out=nlam[:, 0:2], in_=ps[7][:, 0:2], func=AF.Exp), reads=[bps[7]], writes=[b_lam])
            P.op("dve", lambda e: e.tensor_tensor(out=nlam[:, 2:3], in0=nlam[:, 1:2], in1=nlam[:, 0:1], op=ALU.subtract), reads=[b_lam], writes=[b_lam])
            P.op("dve", lambda e: e.tensor_scalar_add(out=nlam[:, 3:4], in0=nlam[:, 2:3], scalar1=-LAMI), reads=[b_lam], writes=[b_lam])
            P.op("dve", lambda e: e.tensor_scalar_mul(out=gsub[:], in0=pt[:, PT["subln"]:PT["subln"] + 1], scalar1=1.0 - LAMI), reads=[b_init], writes=[b_lam])
            gst = R_s[:, 0:RG]
            rep = R_s[0:32, RG:RG + 128]
            ohg = R_s[0:32, RG + 128:RG + 128 + RG]
            b_gst, b_rep = Buf("gst"), Buf("rep")
            claim("Rs", [b_gst, b_rep])
            P.dma("sp", ohg, ohg_d[:, :], "ohg", writes=[b_rep])
            for h in range(8):
                P.op("act", lambda e, h=h: e.activation(out=rep, in_=ones[0:32, :], func=AF.Identity, scale=tabs[:, h:h + 1]), reads=[b_init, b_c], writes=[b_rep])
                for (n0, nn) in ((0, 512), (512, 512), (1024, 128)):
                    P.op("pe", lambda e, n0=n0, nn=nn: e.matmul(out=ps[6][:, 0:nn], lhsT=rep, rhs=ohg[:, n0:n0 + nn], start=True, stop=True), reads=[b_rep, b_init], writes=[bps[6]])
                    P.op("dve", lambda e, n0=n0, nn=nn: e.tensor_copy(out=gst[:, n0:n0 + nn], in_=ps[6][:, 0:nn]), reads=[bps[6]], writes=[b_gst])
                P.dma("sp", gs_d[h, :, :], gst, "gsw", reads=[b_gst], writes=[b_gs[h]])
            for b in b_PT + b_tmp + [b_o0, b_rsc]:
                pass
            claim("Rs", b_PT + b_tmp + [b_o0, b_rsc])
            for tb in range(NBLK):
                for c in range(8):
                    P.op("act", lambda e, c=c, tb=tb: e.activation(out=xb[:, c, blk(tb)], in_=xT[:, c, blk(tb)], func=AF.Copy), reads=[bxT[tb]], writes=[b_xb[tb]])
                    P.op("act", lambda e, c=c, tb=tb: e.activation(out=h1b[:, c, blk(tb)], in_=xT[:, c, blk(tb)], func=AF.Identity, scale=mcol(l, 8, c), bias=mcol(l, 0, c)),
                         reads=[bxT[tb], b_mods], writes=[b_h1[tb]])
                for c in range(8):
                    P.op("dve", lambda e, c=c, tb=tb: e.tensor_scalar_mul(out=xT[:, c, blk(tb)], in0=xT[:, c, blk(tb)], scalar1=ALPHA), reads=[bxT[tb]], writes=[bxT[tb]])
            for h in range(8):
                wsl = h % 2
                wbase = wsl * 2048
                wk = R_m[:, wbase:wbase + 512].bitcast(BF16).rearrange("p (k n) -> p k n", k=8)
                wv = R_m[:, wbase + 512:wbase + 1024].bitcast(BF16).rearrange("p (k n) -> p k n", k=8)
                wq = R_m[:, wbase + 1024:wbase + 1536].bitcast(BF16).rearrange("p (k n) -> p k n", k=8)
                wo = R_m[:, wbase + 1536:wbase + 2048].bitcast(BF16)
                hs = slice(h * 128, (h + 1) * 128)
                P.dma("pool", wk, wkv_d[:, hs].rearrange("(k p) n -> p k n", p=128), f"awt{wsl}", writes=[b_wt[wsl]])
                P.dma("pool", wv, wkv_d[:, D + h * 128:D + (h + 1) * 128].rearrange("(k p) n -> p k n", p=128), f"awt{wsl}", writes=[b_wt[wsl]])
                P.dma("pool", wq, wq_d[:, hs].rearrange("(k p) n -> p k n", p=128), f"awt{wsl}", writes=[b_wt[wsl]])
                P.dma("pool", wo, wo_d[hs, :], f"awt{wsl}", writes=[b_wt[wsl]])
                zsrc = bass.AP(gs_t, h * 128 * RG + 127, [[RG - 1, 128], [1, 1024]])
                P.dma("sp", Zs, zsrc, "zs", reads=[b_gs[h]], writes=[b_Zs])
                for tb in range(NBLK):
                    for k in range(8):
                        P.op("pe", lambda e, k=k, tb=tb, wk=wk: e.matmul(out=ps[6][:], lhsT=wk[:, k, :], rhs=xb[:, k, blk(tb)], start=(k == 0), stop=(k == 7)), reads=[b_wt[wsl], b_xb[tb]], writes=[bps[6]])
                    P.op("act", lambda e, tb=tb: e.activation(out=KT[:, blk(tb)], in_=ps[6][:], func=AF.Copy), reads=[bps[6]], writes=[b_KT])
                    for k in range(8):
                        P.op("pe", lambda e, k=k, tb=tb, wq=wq: e.matmul(out=ps[7][:], lhsT=wq[:, k, :], rhs=h1b[:, k, blk(tb)], start=(k == 0), stop=(k == 7)), reads=[b_wt[wsl], b_h1[tb]], writes=[bps[7]])
                    P.op("dve", lambda e, tb=tb: e.tensor_copy(out=QT[:, blk(tb)], in_=ps[7][:]), reads=[bps[7]], writes=[b_QT])
                for i4 in range(4):
                    bank = 6 + i4 % 2
                    for ii in range(4):
                        i = i4 * 4 + ii
                        for k in range(8):
                            P.op("pe", lambda e, k=k, i=i, ii=ii, wv=wv, bank=bank: e.matmul(out=ps[bank][:, ii * 128:(ii + 1) * 128], lhsT=xb[:, k, i * 128:(i + 1) * 128], rhs=wv[:, k, :], start=(k == 0), stop=(k == 7)),
                                 reads=[b_wt[wsl], b_xb[i // 4]], writes=[bps[bank]])
                    if i4 % 2 == 0:
                        P.op("act", lambda e, i4=i4, bank=bank: e.activation(out=Vh[:, i4 * 4:(i4 + 1) * 4, :], in_=ps[bank][:].rearrange("p (i e) -> p i e", i=4), func=AF.Copy), reads=[bps[bank]], writes=[b_V])
                    else:
                        P.op("dve", lambda e, i4=i4, bank=bank: e.tensor_copy(out=Vh[:, i4 * 4:(i4 + 1) * 4, :], in_=ps[bank][:].rearrange("p (i e) -> p i e", i=4)), reads=[bps[bank]], writes=[b_V])
                tiles = [(qb, m, kt) for qb in range(NBLK) for m in range(2) for kt in range(4 * qb + 4)]
                NTL = len(tiles)
                pending = []

                def stageA(idx):
                    qb, m, kt = tiles[idx]
                    msl = slice(m * 64, (m + 1) * 64)
                    off = kt * 128 - qb * 512
                    sb = idx % 2
                    P.op("pe", lambda e: e.matmul(out=ps[sb][:], lhsT=KT[msl, kt * 128:(kt + 1) * 128], rhs=QT[msl, blk(qb)], start=True, stop=True),
                         reads=[b_KT, b_QT], writes=[bps[sb]])
                    if off <= -256:
                        P.op("act", lambda e: e.activation(out=PTt[sb], in_=ps[sb][:], func=AF.Exp, scale=0.125, bias=negc[:, 0:1]), reads=[bps[sb], b_c], writes=[b_PT[sb]])
                    else:
                        z0 = 384 - off
                        P.op("dve", lambda e: e.scalar_tensor_tensor(out=tmp[sb], in0=ps[sb][:], scalar=0.125, in1=Zs[:, z0:z0 + 512], op0=ALU.mult, op1=ALU.add),
                             reads=[bps[sb], b_Zs], writes=[b_tmp[sb]])
                        if off >= 0:
                            P.op("dve", lambda e: e.tensor_tensor(out=tmp[sb], in0=tmp[sb], in1=masks[:, off // 128, :], op=ALU.add), reads=[b_tmp[sb], b_mask], writes=[b_tmp[sb]])
                        P.op("act", lambda e: e.activation(out=PTt[sb], in_=tmp[sb], func=AF.Exp, scale=1.0, bias=negc[:, 0:1]), reads=[b_tmp[sb], b_c], writes=[b_PT[sb]])

                def seg1(qb, m):
                    po, psm = (2, 3) if m == 0 else (4, 5)
                    P.op("dve", lambda e: e.reciprocal(out=rsc, in_=ps[psm][:]), reads=[bps[psm]], writes=[b_rsc])
                    if m == 0:
                        P.op("dve", lambda e: e.tensor_tensor(out=o0, in0=ps[po][:], in1=rsc, op=ALU.mult), reads=[bps[po], b_rsc], writes=[b_o0])
                    else:
                        P.op("dve", lambda e: e.tensor_tensor(out=rsc, in0=ps[po][:], in1=rsc, op=ALU.mult), reads=[bps[po], b_rsc], writes=[b_rsc])
                        P.op("dve", lambda e: e.scalar_tensor_tensor(out=o0, in0=rsc, scalar=nlam[:, 3:4], in1=o0, op0=ALU.mult, op1=ALU.add), reads=[b_rsc, b_o0, b_lam], writes=[b_o0])
                        P.op("act", lambda e: e.activation(out=rsc, in_=o0, func=AF.Square), reads=[b_o0], writes=[b_rsc])

                def seg2(qb):
                    P.op("pe", lambda e: e.matmul(out=ps[6][:], lhsT=ones[:], rhs=rsc, start=True, stop=True), reads=[b_rsc, b_c], writes=[bps[6]])
                    P.op("act", lambda e: e.activation(out=rsc, in_=ps[6][:], func=AF.Sqrt, scale=1.0 / 128.0, bias=epsT[:, 0:1]), reads=[bps[6], b_c], writes=[b_rsc])
                    P.op("dve", lambda e: e.reciprocal(out=rsc, in_=rsc), reads=[b_rsc], writes=[b_rsc])
                    P.op("dve", lambda e: e.tensor_tensor(out=o0, in0=o0, in1=rsc, op=ALU.mult), reads=[b_o0, b_rsc], writes=[b_o0])
                    P.op("act", lambda e: e.activation(out=AT[:, blk(qb)], in_=o0, func=AF.Identity, scale=gsub[:, 0:1]), reads=[b_o0, b_lam], writes=[b_AT])

                def seg3(qb):
                    for d in range(8):
                        bank = 6 + d % 2
                        P.op("pe", lambda e, d=d, bank=bank, wo=wo: e.matmul(out=ps[bank][:], lhsT=wo[:, d * 128:(d + 1) * 128], rhs=AT[:, blk(qb)], start=True, stop=True), reads=[b_wt[wsl], b_AT], writes=[bps[bank]])
                        P.op("dve", lambda e, d=d, bank=bank: e.scalar_tensor_tensor(out=xT[:, d, blk(qb)], in0=ps[bank][:], scalar=mcol(l, 16, d), in1=xT[:, d, blk(qb)], op0=ALU.mult, op1=ALU.add),
                             reads=[bps[bank], b_mods, bxT[qb]], writes=[bxT[qb]])

                def stageB(idx):
                    qb, m, kt = tiles[idx]
                    ntile = 4 * qb + 4
                    po, psm = (2, 3) if m == 0 else (4, 5)
                    sb = idx % 2
                    P.op("pe", lambda e: e.matmul(out=ps[po][:], lhsT=Vh[:, kt, :], rhs=PTt[sb], start=(kt == 0), stop=(kt == ntile - 1)), reads=[b_V, b_PT[sb]], writes=[bps[po]])
                    P.op("pe", lambda e: e.matmul(out=ps[psm][:], lhsT=onesb[:], rhs=PTt[sb], start=(kt == 0), stop=(kt == ntile - 1)), reads=[b_c, b_PT[sb]], writes=[bps[psm]])
                    if kt == ntile - 1:
                        seg1(qb, m)
                        if m == 1:
                            pending.append((idx + 3, lambda qb=qb: seg2(qb)))
                            pending.append((idx + 5, lambda qb=qb: seg3(qb)))

                for idx in range(NTL + 1):
                    if idx < NTL:
                        stageA(idx)
                    if idx >= 1:
                        stageB(idx - 1)
                    while pending and pending[0][0] <= idx:
                        pending.pop(0)[1]()
                while pending:
                    pending.pop(0)[1]()
            pg = PT["post_g"] + (l * 2 + 0) * 8
            pb = PT["post_b"] + (l * 2 + 0) * 8
            claim("Rs", [b_sq[0], b_sq[1], b_stat])
            for tb in range(NBLK):
                layer_norm_block(lambda c, tb=tb: xT[:, c, blk(tb)], bxT[tb], lambda c, tb=tb: xT[:, c, blk(tb)], bxT[tb], pg, pb, AF.Identity, 0, 1)

        b_XS = [Buf(f"XS{i}") for i in range(8)]
        b_YS = [Buf("YS0"), Buf("YS1")]

        def moe_sparse(l):
            hT = R_h[:].bitcast(BF16).rearrange("p (c t) -> p c t", c=8)
            b_hT = [Buf(f"hT{b}") for b in range(NBLK)]
            b_GT = Buf("GT")
            b_M, b_cnt = Buf("Mall"), Buf("cnt")
            b_posl = [Buf(f"posI{i}") for i in range(16)]
            b_gatesl = [Buf(f"gates{i}") for i in range(16)]
            claim("Rh", b_hT)
            claim("Rm", [b_GT])
            claim("Rs", [b_sq[0], b_sq[1], b_stat])
            for tb in range(NBLK):
                for c in range(8):
                    P.op("act", lambda e, c=c, tb=tb: e.activation(out=hT[:, c, blk(tb)], in_=xT[:, c, blk(tb)], func=AF.Identity, scale=mcol(l, 32, c), bias=mcol(l, 24, c)),
                         reads=[bxT[tb], b_mods], writes=[b_hT[tb]])
                for c in range(8):
                    P.op("dve", lambda e, c=c, tb=tb: e.tensor_scalar_mul(out=xT[:, c, blk(tb)], in0=xT[:, c, blk(tb)], scalar1=ALPHA), reads=[bxT[tb]], writes=[bxT[tb]])
            wr = wr_s[:].rearrange("p (l k e) -> p l k e", l=2, k=8)
            Mv = Mall[:].rearrange("p (i e) -> p i e", i=16)
            b_lg2 = [Buf("lg0"), Buf("lg1")]
            b_rt2 = [Buf("rt0"), Buf("rt1")]

            def tile_gen(i):
                st_ = i % 2
                b_lg, b_rt = b_lg2[st_], b_rt2[st_]
                lgv = lg[:, st_ * 256:(st_ + 1) * 256]
                rtv = rt[:, st_ * 400:(st_ + 1) * 400]
                mxv = mx8[:, st_ * 16:(st_ + 1) * 16]
                bl, bg6, br5 = (7, 6, 5) if st_ == 0 else (3, 2, 1)
                L = lgv[:, 0:NE]
                M = lgv[:, NE:2 * NE]
                E = lgv[:, 2 * NE:3 * NE]
                G = lgv[:, 3 * NE:4 * NE]
                OH = rtv[:, 0:128].rearrange("p (k e) -> p k e", k=4)
                TA = rtv[:, 128:256].rearrange("p (k e) -> p k e", k=4)
                TB = rtv[:, 256:384].rearrange("p (k e) -> p k e", k=4)
                eid4 = rtv[:, 384:388]
                rank4 = rtv[:, 388:392]
                pos4 = rtv[:, 392:396]
                e4 = rtv[:, 396:400]
                tb = i // 4
                tsl = slice(i * 128, (i + 1) * 128)
                for k in range(8):
                    P.op("pe", lambda e, k=k: e.matmul(out=ps[bl][:, 0:NE], lhsT=hT[:, k, tsl], rhs=wr[:, l, k, :], start=(k == 0), stop=False), reads=[b_hT[tb], b_init], writes=[bps[bl]])
                P.op("pe", lambda e: e.matmul(out=ps[bl][:, 0:NE], lhsT=onesb[0:1, :], rhs=br_s[0:1, l * NE:(l + 1) * NE], start=False, stop=True), reads=[b_c, b_init], writes=[bps[bl]])
                yield
                P.op("dve", lambda e: e.tensor_copy(out=L, in_=ps[bl][:, 0:NE]), reads=[bps[bl]], writes=[b_lg])
                yield
                P.op("dve", lambda e: e.max(out=mxv[:, 0:8], in_=L), reads=[b_lg], writes=[b_lg])
                yield
                P.op("dve", lambda e: e.tensor_copy(out=TA[:, 0, :], in_=L), reads=[b_lg], writes=[b_rt])
                yield
                Lw = TA[:, 0, :]
                EQ = TA[:, 1, :]
                TT = TA[:, 2, :]
                for k in range(4):
                    P.op("dve", lambda e, k=k: e.tensor_scalar(out=EQ, in0=Lw, scalar1=mxv[:, k:k + 1], scalar2=None, op0=ALU.is_equal), reads=[b_rt, b_lg], writes=[b_rt])
                    yield
                    P.op("dve", lambda e: e.scalar_tensor_tensor(out=TT, in0=EQ, scalar=-1000.0, in1=iotaP[:], op0=ALU.mult, op1=ALU.add), reads=[b_rt, b_c], writes=[b_rt])
                    yield
                    P.op("dve", lambda e, k=k: e.tensor_reduce(out=eid4[:, k:k + 1], in_=TT, axis=AX.X, op=ALU.min), reads=[b_rt], writes=[b_rt])
                    yield
                    P.op("dve", lambda e, k=k: e.tensor_scalar(out=OH[:, k, :], in0=iotaT[:], scalar1=eid4[:, k:k + 1], scalar2=None, op0=ALU.is_equal), reads=[b_rt, b_init], writes=[b_rt])
                    yield
                    if k < 3:
                        P.op("dve", lambda e, k=k: e.scalar_tensor_tensor(out=Lw, in0=OH[:, k, :], scalar=-1.0e30, in1=Lw, op0=ALU.mult, op1=ALU.add), reads=[b_rt], writes=[b_rt])
                        yield
                P.op("dve", lambda e: e.reduce_sum(out=M, in_=OH.rearrange("p k e -> p e k"), axis=AX.X), reads=[b_rt], writes=[b_lg])
                yield
                P.op("act", lambda e: e.activation(out=Mv[:, i, :], in_=M, func=AF.Copy), reads=[b_lg], writes=[b_M])
                P.op("dve", lambda e: e.tensor_scalar_mul(out=mxv[:, 8:9], in0=mxv[:, 0:1], scalar1=-1.0), reads=[b_lg], writes=[b_lg])
                yield
                P.op("act", lambda e: e.activation(out=E, in_=L, func=AF.Exp, bias=mxv[:, 8:9], scale=1.0), reads=[b_lg], writes=[b_lg])
                P.op("act", lambda e: e.activation(out=e4, in_=mxv[:, 0:4], func=AF.Exp, bias=mxv[:, 8:9], scale=1.0), reads=[b_lg], writes=[b_rt])
                yield
                P.op("dve", lambda e: e.tensor_tensor(out=E, in0=E, in1=M, op=ALU.mult), reads=[b_lg], writes=[b_lg])
                yield
                P.op("dve", lambda e: e.reduce_sum(out=mxv[:, 9:10], in_=E, axis=AX.X), reads=[b_lg], writes=[b_lg])
                yield
                P.op("dve", lambda e: e.reciprocal(out=mxv[:, 10:11], in_=mxv[:, 9:10]), reads=[b_lg], writes=[b_lg])
                yield
                P.op("dve", lambda e: e.tensor_scalar(out=G, in0=E, scalar1=mxv[:, 10:11], scalar2=None, op0=ALU.mult), reads=[b_lg], writes=[b_lg])
                yield
                P.op("dve", lambda e: e.tensor_scalar(out=gates4[:, 4 * i:4 * i + 4], in0=e4, scalar1=mxv[:, 10:11], scalar2=None, op0=ALU.mult), reads=[b_lg, b_rt], writes=[b_gatesl[i]])
                P.op("pe", lambda e: e.transpose(out=ps[bg6][0:NE, 0:128], in_=G, identity=ident[:]), reads=[b_lg, b_init], writes=[bps[bg6]])
                yield
                P.op("act", lambda e: e.activation(out=GTb[:, tsl], in_=ps[bg6][0:NE, 0:128], func=AF.Copy), reads=[bps[bg6]], writes=[b_GT])
                for i2 in range(i + 1):
                    lhs = trib if i2 == i else onesb
                    P.op("pe", lambda e, i2=i2, lhs=lhs: e.matmul(out=ps[br5][:, 0:NE], lhsT=lhs[:], rhs=Mv[:, i2, :], start=(i2 == 0), stop=(i2 == i)), reads=[b_M, b_c, b_init], writes=[bps[br5]])
                yield
                P.op("dve", lambda e: e.tensor_tensor(out=TB, in0=OH, in1=ps[br5][:, 0:NE].unsqueeze(1).to_broadcast([128, 4, NE]), op=ALU.mult), reads=[b_rt, bps[br5]], writes=[b_rt])
                yield
                P.op("dve", lambda e: e.reduce_sum(out=rank4, in_=TB, axis=AX.X), reads=[b_rt], writes=[b_rt])
                yield
                P.op("dve", lambda e: e.scalar_tensor_tensor(out=pos4, in0=eid4, scalar=float(S), in1=rank4, op0=ALU.mult, op1=ALU.add), reads=[b_rt], writes=[b_rt])
                yield
                P.op("dve", lambda e: e.tensor_copy(out=posI[:, 4 * i:4 * i + 4], in_=pos4), reads=[b_rt], writes=[b_posl[i]])
                yield

            for p_ in range(8):
                g0, g1 = tile_gen(2 * p_), tile_gen(2 * p_ + 1)
                done0 = done1 = False
                while not (done0 and done1):
                    if not done0:
                        try:
                            next(g0)
                        except StopIteration:
                            done0 = True
                    if not done1:
                        try:
                            next(g1)
                        except StopIteration:
                            done1 = True
            for i in range(16):
                P.op("pe", lambda e, i=i: e.matmul(out=ps[4][0:1, 0:NE], lhsT=onesb[:, 0:1], rhs=Mv[:, i, :], start=(i == 0), stop=(i == 15)), reads=[b_M, b_c], writes=[bps[4]])
            P.op("dve", lambda e: e.tensor_copy(out=cntF[:], in_=ps[4][0:1, 0:NE]), reads=[bps[4]], writes=[b_cnt])
            P.op("dve", lambda e: e.tensor_copy(out=cntI[:], in_=cntF[:]), reads=[b_cnt], writes=[b_cnt])
            for tb in range(NBLK):
                for d in range(8):
                    bank = 2 + d % 2
                    P.op("pe", lambda e, d=d, tb=tb, bank=bank: e.matmul(out=ps[bank][:], lhsT=bdn_s[:, l * D + d * 128:l * D + (d + 1) * 128], rhs=GTb[:, blk(tb)], start=True, stop=True), reads=[b_init, b_GT], writes=[bps[bank]])
                    P.op("dve", lambda e, d=d, tb=tb, bank=bank: e.scalar_tensor_tensor(out=xT[:, d, blk(tb)], in0=ps[bank][:], scalar=mcol(l, 40, d), in1=xT[:, d, blk(tb)], op0=ALU.mult, op1=ALU.add),
                         reads=[bps[bank], b_mods, bxT[tb]], writes=[bxT[tb]])
            htok = [R_a[:, 0:512].bitcast(BF16), R_a[:, 512:1024].bitcast(BF16)]
            b_htok = [Buf("htok0"), Buf("htok1")]
            actT = [R_a[:, 1024:1536].bitcast(BF16).rearrange("p (f s) -> p f s", f=8), R_a[:, 1536:2048].bitcast(BF16).rearrange("p (f s) -> p f s", f=8)]
            b_actT = [Buf("actT0"), Buf("actT1")]
            claim("Ra", b_htok + b_actT)
            for i in range(16):
                sl = i % 2
                bank = sl
                pv = ps[bank][:].bitcast(BF16)
                for c in range(8):
                    P.op("pe", lambda e, c=c, i=i, pv=pv: e.transpose(out=pv[:, c * 128:(c + 1) * 128], in_=hT[:, c, i * 128:(i + 1) * 128], identity=identb[:]), reads=[b_hT[i // 4], b_c], writes=[bps[bank]])
                P.op("act", lambda e, sl=sl, pv=pv: e.activation(out=htok[sl], in_=pv, func=AF.Copy), reads=[bps[bank]], writes=[b_htok[sl]])
                for k in range(4):
                    col = 4 * i + k
                    P.dma("pool", None, None, f"xsc{sl}", reads=[b_htok[sl], b_posl[i]], writes=[b_XS[sl * 4 + k]],
                          fn=lambda e, sl=sl, col=col: e.indirect_dma_start(out=xs_d[:, :], out_offset=bass.IndirectOffsetOnAxis(ap=posI[:, col:col + 1], axis=0), in_=htok[sl], in_offset=None))
            ring = [R_w[:, 0:4096], R_w[:, 4096:8192], R_w[:, 8192:12288], R_h[:, 0:4096], R_h[:, 4096:8192]]
            b_ring = [Buf(f"ring{q}") for q in range(5)]
            claim("Rw", b_ring[0:3])
            xsb = R_s[:, 0:512].bitcast(BF16)
            xsT = [R_s[:, 512:1024].bitcast(BF16).rearrange("p (k s) -> p k s", k=8), R_s[:, 1024:1536].bitcast(BF16).rearrange("p (k s) -> p k s", k=8)]
            act_tt = [R_s[:, 1536:2048].bitcast(BF16), R_s[:, 2048:2560].bitcast(BF16)]
            b_xsb, b_xsT, b_acttt = Buf("xsb"), [Buf("xsT0"), Buf("xsT1")], [Buf("act_t0"), Buf("act_t1")]
            ys = [R_m[:, 0:1024], R_m[:, 1024:2048]]
            b_ys = [Buf("ys0"), Buf("ys1")]
            bgu_row = [R_m[0:1, 2048:3072].bitcast(BF16), R_m[0:1, 3072:4096].bitcast(BF16)]
            b_bgu = [Buf("bgu0"), Buf("bgu1")]
            gc, sg_, ln_, t1 = R_m[:, 4096:4608], R_m[:, 4608:5120], R_m[:, 5120:5632], R_m[:, 5632:6144]
            b_gc, b_sg2, b_ln, b_t1 = Buf("gc"), Buf("sg2"), Buf("ln"), Buf("t1")
            ring_claimed_h = [False]

            def issue_piece(e_, p):
                q = (3 * e_ + p) % 5
                if q >= 3 and not ring_claimed_h[0]:
                    claim("Rh", b_ring[3:5])
                    ring_claimed_h[0] = True
                wv = ring[q].bitcast(BF16).rearrange("p (k n) -> p k n", k=8)
                if p == 0:
                    src = wgu_d[l, e_, :, 0:D]
                elif p == 1:
                    src = wgu_d[l, e_, :, D:2 * D]
                else:
                    src = wdn_d[l, e_, :, :]
                P.dma("pool", wv, src.rearrange("(k p) n -> p k n", p=128), f"rg{q}", writes=[b_ring[q]])
                return q, wv

            def issue_bias(e_):
                P.dma("pool", bgu_row[e_ % 2], bgu_d[l, e_:e_ + 1, :], f"bgu{e_ % 2}", writes=[b_bgu[e_ % 2]])

            claim("Rs", [b_xsb, b_xsT[0], b_xsT[1]] + b_acttt)
            claim("Rm", b_ys + b_bgu + [b_gc, b_sg2, b_ln, b_t1])
            pieces = {}
            pieces[(0, 0)] = issue_piece(0, 0)
            pieces[(0, 1)] = issue_piece(0, 1)
            pieces[(0, 2)] = issue_piece(0, 2)
            issue_bias(0)
            bcnt = [0]
            for e_ in range(NE):
                if e_ + 1 < NE:
                    pieces[(e_ + 1, 0)] = issue_piece(e_ + 1, 0)
                    pieces[(e_ + 1, 1)] = issue_piece(e_ + 1, 1)
                    issue_bias(e_ + 1)
                (qg, wg), (ql, wl), (qd, wd) = pieces[(e_, 0)], pieces[(e_, 1)], pieces[(e_, 2)]
                brow = bgu_row[e_ % 2]
                for eng in ("pe", "act", "dve", "sp"):
                    P.regload(eng, cntI[0:1, e_:e_ + 1], reads=[b_cnt])
                def stage_a(j, e_=e_, wg=wg, wl=wl, brow=brow, qg=qg, ql=ql):
                    pr = ((l, e_), j * 128)
                    s_ = j % 2
                    act_t, b_actt = act_tt[s_], b_acttt[s_]
                    r0 = e_ * S + j * 128
                    P.dma("sp", xsb, xs_d[r0:r0 + 128, :], "xsl", reads=b_XS, writes=[b_xsb], pred=pr)
                    pv = ps[0][:].bitcast(BF16)
                    for k in range(8):
                        P.op("pe", lambda e, k=k, pv=pv: e.transpose(out=pv[:, k * 128:(k + 1) * 128], in_=xsb[:, k * 128:(k + 1) * 128], identity=identb[:]), reads=[b_xsb, b_c], writes=[bps[0]], pred=pr)
                    P.op("dve", lambda e, s_=s_, pv=pv: e.tensor_copy(out=xsT[s_], in_=pv.rearrange("p (k s) -> p k s", k=8)), reads=[bps[0]], writes=[b_xsT[s_]], pred=pr)
                    for hf in range(2):
                        bg_, bl_ = 2 + 2 * hf, 3 + 2 * hf
                        fs = slice(hf * 512, (hf + 1) * 512)
                        for k in range(8):
                            P.op("pe", lambda e, k=k, s_=s_, bg_=bg_, fs=fs: e.matmul(out=ps[bg_][:], lhsT=xsT[s_][:, k, :], rhs=wg[:, k, fs], start=(k == 0), stop=False), reads=[b_ring[qg], b_xsT[s_]], writes=[bps[bg_]], pred=pr)
                        P.op("pe", lambda e, bg_=bg_, hf=hf: e.matmul(out=ps[bg_][:], lhsT=onesb[0:1, :], rhs=brow[0:1, hf * 512:(hf + 1) * 512], start=False, stop=True), reads=[b_bgu[e_ % 2], b_c], writes=[bps[bg_]], pred=pr)
                        for k in range(8):
                            P.op("pe", lambda e, k=k, s_=s_, bl_=bl_, fs=fs: e.matmul(out=ps[bl_][:], lhsT=xsT[s_][:, k, :], rhs=wl[:, k, fs], start=(k == 0), stop=False), reads=[b_ring[ql], b_xsT[s_]], writes=[bps[bl_]], pred=pr)
                        P.op("pe", lambda e, bl_=bl_, hf=hf: e.matmul(out=ps[bl_][:], lhsT=onesb[0:1, :], rhs=brow[0:1, D + hf * 512:D + (hf + 1) * 512], start=False, stop=True), reads=[b_bgu[e_ % 2], b_c], writes=[bps[bl_]], pred=pr)
                        P.op("dve", lambda e, bg_=bg_: e.tensor_scalar_min(out=gc, in0=ps[bg_][:], scalar1=7.0), reads=[bps[bg_]], writes=[b_gc], pred=pr)
                        P.op("act", lambda e: e.activation(out=sg_, in_=gc, func=AF.Sigmoid, scale=1.702), reads=[b_gc], writes=[b_sg2], pred=pr)
                        P.op("dve", lambda e, bl_=bl_: e.tensor_scalar(out=ln_, in0=ps[bl_][:], scalar1=1.0, scalar2=8.0, op0=ALU.add, op1=ALU.min), reads=[bps[bl_]], writes=[b_ln], pred=pr)
                        P.op("dve", lambda e: e.tensor_tensor(out=t1, in0=gc, in1=sg_, op=ALU.mult), reads=[b_gc, b_sg2], writes=[b_t1], pred=pr)
                        P.op("dve", lambda e, fs=fs, act_t=act_t: e.scalar_tensor_tensor(out=act_t[:, fs], in0=ln_, scalar=-6.0, in1=t1, op0=ALU.max, op1=ALU.mult), reads=[b_ln, b_t1], writes=[b_actt], pred=pr)

                def stage_b(j, e_=e_, wd=wd, qd=qd):
                    pr = ((l, e_), j * 128)
                    s_ = j % 2
                    act_t, b_actt = act_tt[s_], b_acttt[s_]
                    r0 = e_ * S + j * 128
                    pv1 = ps[1][:].bitcast(BF16)
                    for f in range(8):
                        P.op("pe", lambda e, f=f, pv1=pv1, act_t=act_t: e.transpose(out=pv1[:, f * 128:(f + 1) * 128], in_=act_t[:, f * 128:(f + 1) * 128], identity=identb[:]), reads=[b_actt, b_c], writes=[bps[1]], pred=pr)
                    P.op("dve", lambda e, s_=s_, pv1=pv1: e.tensor_copy(out=actT[s_], in_=pv1.rearrange("p (f s) -> p f s", f=8)), reads=[bps[1]], writes=[b_actT[s_]], pred=pr)
                    for n in range(2):
                        for f in range(8):
                            P.op("pe", lambda e, f=f, n=n, s_=s_: e.matmul(out=ps[6 + n][:], lhsT=actT[s_][:, f, :], rhs=wd[:, f, n * 512:(n + 1) * 512], start=(f == 0), stop=(f == 7)), reads=[b_ring[qd], b_actT[s_]], writes=[bps[6 + n]], pred=pr)
                        P.op("dve", lambda e, n=n, s_=s_: e.tensor_copy(out=ys[s_][:, n * 512:(n + 1) * 512], in_=ps[6 + n][:]), reads=[bps[6 + n]], writes=[b_ys[s_]], pred=pr)
                    P.dma("act", ys_d[r0:r0 + 128, :], ys[s_], "yst", reads=[b_ys[s_]], writes=[b_YS[s_]], pred=pr)

                stage_a(0)
                for j in range(16):
                    if j + 1 < 16:
                        stage_a(j + 1)
                    stage_b(j)
                if e_ + 1 < NE:
                    pieces[(e_ + 1, 2)] = issue_piece(e_ + 1, 2)
            yb = [[R_w[:, (s2 * 4 + k) * 1024:(s2 * 4 + k + 1) * 1024] for k in range(4)] for s2 in range(2)]
            acc = [R_w[:, 8192:9216], R_w[:, 9216:10240]]
            b_yb = [[Buf(f"yb{s2}{k}") for k in range(4)] for s2 in range(2)]
            b_acc = [Buf("acc0"), Buf("acc1")]
            claim("Rw", b_yb[0] + b_yb[1] + b_acc)
            for i in range(16):
                s2 = i % 2
                for k in range(4):
                    col = 4 * i + k
                    P.dma("pool", None, None, f"yg{s2}", reads=b_YS + [b_posl[i]], writes=[b_yb[s2][k]],
                          fn=lambda e, s2=s2, k=k, col=col: e.indirect_dma_start(out=yb[s2][k], out_offset=None, in_=ys_d[:, :], in_offset=bass.IndirectOffsetOnAxis(ap=posI[:, col:col + 1], axis=0)))
                P.op("dve", lambda e, s2=s2, i=i: e.tensor_scalar(out=acc[s2], in0=yb[s2][0], scalar1=gates4[:, 4 * i:4 * i + 1], scalar2=None, op0=ALU.mult), reads=[b_yb[s2][0], b_gatesl[i]], writes=[b_acc[s2]])
                for k in range(1, 4):
                    P.op("dve", lambda e, s2=s2, i=i, k=k: e.scalar_tensor_tensor(out=acc[s2], in0=yb[s2][k], scalar=gates4[:, 4 * i + k:4 * i + k + 1], in1=acc[s2], op0=ALU.mult, op1=ALU.add), reads=[b_yb[s2][k], b_gatesl[i], b_acc[s2]], writes=[b_acc[s2]])
                for g in range(2):
                    bank = 2 * s2 + g
                    for cc in range(4):
                        d = g * 4 + cc
                        P.op("pe", lambda e, d=d, cc=cc, s2=s2, bank=bank: e.transpose(out=ps[bank][:, cc * 128:(cc + 1) * 128], in_=acc[s2][:, d * 128:(d + 1) * 128], identity=ident[:]), reads=[b_acc[s2], b_init], writes=[bps[bank]])
                    for cc in range(4):
                        d = g * 4 + cc
                        P.op("dve", lambda e, d=d, cc=cc, i=i, bank=bank: e.scalar_tensor_tensor(out=xT[:, d, i * 128:(i + 1) * 128], in0=ps[bank][:, cc * 128:(cc + 1) * 128], scalar=mcol(l, 40, d), in1=xT[:, d, i * 128:(i + 1) * 128], op0=ALU.mult, op1=ALU.add),
                             reads=[bps[bank], b_mods, bxT[i // 4]], writes=[bxT[i // 4]])
            pg = PT["post_g"] + (l * 2 + 1) * 8
            pb = PT["post_b"] + (l * 2 + 1) * 8
            claim("Rs", [b_sq[0], b_sq[1], b_stat])
            for tb in range(NBLK):
                layer_norm_block(lambda c, tb=tb: xT[:, c, blk(tb)], bxT[tb], lambda c, tb=tb: xT[:, c, blk(tb)], bxT[tb], pg, pb, AF.Identity, 4, 5)

        moe = moe_sparse if SPARSE else moe_sublayer
        phases = [("conv", conv_sublayer), ("moe0", lambda: moe(0)), ("attn", attn_sublayer), ("moe1", lambda: moe(1))]
        for name, fn in phases:
            fn()
            if stop_after == name:
                break

        b_stage = [Buf("ostage0"), Buf("ostage1")]
        claim("Ra", b_stage)
        for i in range(16):
            sl = i % 2
            sv = stage[:, sl * D:(sl + 1) * D]
            for g in range(2):
                bank = 2 + g
                for cc in range(4):
                    c = g * 4 + cc
                    P.op("pe", lambda e, c=c, cc=cc, i=i, bank=bank: e.transpose(out=ps[bank][:, cc * 128:(cc + 1) * 128], in_=xT[:, c, i * 128:(i + 1) * 128], identity=ident[:]),
                         reads=[bxT[i // 4], b_init], writes=[bps[bank]])
                if g == 0:
                    P.op("act", lambda e, sv=sv, bank=bank: e.activation(out=sv[:, 0:512], in_=ps[bank][:], func=AF.Copy), reads=[bps[bank]], writes=[b_stage[sl]])
                else:
                    P.op("dve", lambda e, sv=sv, bank=bank: e.tensor_copy(out=sv[:, 512:1024], in_=ps[bank][:]), reads=[bps[bank]], writes=[b_stage[sl]])
            P.dma("sp", out_d[i * 128:(i + 1) * 128, :], sv, "out", reads=[b_stage[sl]])
        P.final_wait("sp", ["out"])
        P.emit()
    return nc


_CACHE = {}


def _t5_bucket(rel):
    nb = 16
    ret = np.where(rel > 0, nb, 0)
    n = np.abs(rel)
    max_exact = 8
    nf = np.maximum(n, 1).astype(np.float32) / np.float32(max_exact)
    large = max_exact + (np.log(nf) / np.float32(math.log(128 / max_exact)) * np.float32(nb - max_exact)).astype(np.int32)
    large = np.minimum(large, nb - 1)
    return ret + np.where(n < max_exact, n, large)


def _attn_consts():
    rel = 511 - np.arange(RG)
    bk = _t5_bucket(rel)
    ohg = np.zeros((32, RG), np.float32)
    ohg[bk, np.arange(RG)] = 1.0
    ohg[15, :] -= 1.0
    masks = np.zeros((4, 128, BLK), np.float32)
    kl = np.arange(128)[:, None] // 64
    ql = np.arange(BLK)[None, :] // 64
    for o in range(4):
        masks[o] = np.where(kl - ql <= -(o * 128) // 64, 0.0, NEG)
    return ohg, masks


def make_in_maps(inp):
    f = lambda a: np.ascontiguousarray(np.asarray(a, np.float32))
    pt = build_pt(inp)
    shared = {
        "pt": pt,
        "ident": np.eye(128, dtype=np.float32),
        "ada_w": f(inp["ada_w"]),
        "w_pw1": f(inp["conv_w_pw1"][0]),
        "w_pw2": f(inp["conv_w_pw2"][0]),
        "w_r": f(inp["router_w"]),
        "b_r": f(inp["router_b"]).reshape(2, 1, NE),
        "w_gu": f(inp["expert_w_gate_up"]),
        "w_dn": f(inp["expert_w_down"]),
        "b_dn": f(inp["expert_b_down"]),
        "w_kv": f(inp["w_kv"]),
        "w_q": f(inp["attn_w_q"][0]),
        "w_o": f(inp["attn_w_o"][0]),
        "lpt": np.ascontiguousarray(f(inp["attn_lambda"][0]).T),
        "tabs": f(inp["rel_bias_table"]),
        "b_gu": f(inp["expert_b_gate_up"]),
        "tri": np.triu(np.ones((128, 128), np.float32), 1),
        "iota": np.tile(np.arange(NE, dtype=np.float32)[None, :], (128, 1)),
        "ohg": _attn_consts()[0],
        "masks": _attn_consts()[1],
    }
    maps = []
    for b in range(8):
        m = dict(shared)
        m["x"] = f(inp["x"][b])
        m["ct"] = np.ascontiguousarray(f(inp["c"][b]).reshape(8, 128).T)
        maps.append(m)
    return maps


def kernel(**inputs):
    if "nc" not in _CACHE:
        _CACHE["nc"] = build()
    nc = _CACHE["nc"]
    maps = make_in_maps(inputs)
    res = run_bass_kernel_spmd(nc, maps, core_ids=list(range(8)))
    return np.stack([np.asarray(r["out"], np.float32) for r in res.results], axis=0)
```

```python
import math
import numpy as np
import concourse.bass as bass
import concourse.mybir as mybir
from concourse.bass_utils import run_bass_kernel_spmd
from contextlib import ExitStack

F32 = mybir.dt.float32
BF16 = mybir.dt.bfloat16
I32 = mybir.dt.int32
SPARSE = True
AF = mybir.ActivationFunctionType
ALU = mybir.AluOpType
AX = mybir.AxisListType

SAME_ENG_SYNC = True

D = 1024
S = 2048
NE = 32
ALPHA = 4.0 ** 0.25
LN_EPS = 1e-5
BLK = 512
RG = 1152
LAMI = 0.8 - 0.6 * math.exp(-0.3 * 1)
SM_SHIFT = 20.0
NEG = -30000.0
NBLK = S // BLK


class Buf:
    __slots__ = ("name", "w", "r", "rd")

    def __init__(self, name):
        self.name = name
        self.w = None
        self.r = {}
        self.rd = []


class Ins:
    __slots__ = ("eng", "fn", "kind", "cdeps", "dwaits", "sig", "sigidx", "pos", "dsem", "pred", "dord")

    def __init__(self, eng, fn, kind, dsem=None):
        self.pred = None
        self.dord = 0
        self.eng = eng
        self.fn = fn
        self.kind = kind
        self.cdeps = {}
        self.dwaits = {}
        self.sig = False
        self.sigidx = None
        self.pos = None
        self.dsem = dsem


class Prog:
    ENGS = ("pe", "act", "dve", "pool", "sp")

    def __init__(self, nc):
        self.nc = nc
        self.streams = {e: [] for e in self.ENGS}
        self.dma_count = {}
        self.regs = {}

    def _dep(self, ins, p):
        if p is None or p is ins:
            return
        if p.kind == "d":
            s = p.dsem
            ins.dwaits[s] = max(ins.dwaits.get(s, 0), self.dma_count[s])
            return
        if ins.kind == "c" and p.eng == ins.eng:
            if ins.eng == "pe" or not SAME_ENG_SYNC:
                return
        cur = ins.cdeps.get(p.eng)
        if cur is None or cur.pos < p.pos:
            ins.cdeps[p.eng] = p

    def _add(self, ins, reads, writes):
        for b in reads:
            self._dep(ins, b.w)
        for b in writes:
            self._dep(ins, b.w)
            for r in b.r.values():
                self._dep(ins, r)
            for r in b.rd:
                self._dep(ins, r)
        ins.pos = len(self.streams[ins.eng])
        self.streams[ins.eng].append(ins)
        for b in reads:
            if ins.kind == "c":
                b.r[ins.eng] = ins
            else:
                b.rd.append(ins)
        for b in writes:
            b.w = ins
            b.r = {}
            b.rd = []
        return ins

    def op(self, eng, fn, reads=(), writes=(), pred=None):
        ins = Ins(eng, fn, "c")
        ins.pred = pred
        return self._add(ins, reads, writes)

    def dma(self, eng, out, in_, sem, reads=(), writes=(), pred=None, fn=None, **kw):
        self.dma_count.setdefault(sem, 0)
        if fn is None:
            fn = lambda e, out=out, in_=in_, kw=kw: e.dma_start(out=out, in_=in_, **kw)
        ins = Ins(eng, fn, "d", dsem=sem)
        ins.pred = pred
        ins.dord = self.dma_count[sem]
        self._add(ins, reads, writes)
        self.dma_count[sem] += 1
        return ins

    def regload(self, eng, ap, reads=()):
        return self.op(eng, lambda e, ap=ap, eng=eng: e.reg_load(self.regs[eng], ap), reads=reads)

    def final_wait(self, eng, sems):
        ins = Ins(eng, None, "c")
        for s in sems:
            ins.dwaits[s] = self.dma_count[s]
        ins.pos = len(self.streams[eng])
        self.streams[eng].append(ins)

    def emit(self):
        nc = self.nc
        for e in self.ENGS:
            for ins in self.streams[e]:
                for p in ins.cdeps.values():
                    p.sig = True
        for e in self.ENGS:
            n = 0
            for ins in self.streams[e]:
                if ins.sig:
                    n += 1
                    ins.sigidx = n
        with ExitStack() as st:
            csem = {e: st.enter_context(nc.semaphore("c_" + e)) for e in self.ENGS}
            dsem = {s: st.enter_context(nc.semaphore("d_" + s)) for s in self.dma_count}
            block = st.enter_context(nc.Block())

            eobj = {"pe": nc.tensor, "act": nc.scalar, "dve": nc.vector, "pool": nc.gpsimd, "sp": nc.sync}
            for e in self.ENGS:
                self.regs[e] = st.enter_context(eobj[e].register("pr_" + e))

            def run(engname):
                def body(eng):
                    waited = {}

                    def emit_one(ins):
                        for pe_, p in ins.cdeps.items():
                            key = ("c", pe_)
                            if waited.get(key, 0) < p.sigidx:
                                eng.wait_ge(csem[pe_], p.sigidx)
                                waited[key] = p.sigidx
                        for s_, cnt in ins.dwaits.items():
                            key = ("d", s_)
                            if waited.get(key, 0) < cnt:
                                eng.wait_ge(dsem[s_], 16 * cnt)
                                waited[key] = cnt
                        if ins.fn is None:
                            return
                        bi = ins.fn(eng)
                        if ins.kind == "d":
                            bi.then_inc(dsem[ins.dsem], 16)
                        elif ins.sig:
                            bi.then_inc(csem[engname], 1)

                    def body_of(g):
                        for x in g:
                            emit_one(x)

                    def balance(groups):
                        nsig = 0
                        dincs = {}
                        for g in groups:
                            for x in g:
                                if x.kind == "c":
                                    if x.sig:
                                        nsig += 1
                                else:
                                    first, n = dincs.get(x.dsem, (x.dord, 0))
                                    dincs[x.dsem] = (min(first, x.dord), n + 1)
                        if nsig:
                            eng.drain().then_inc(csem[engname], nsig)
                        for s_, (first, n) in dincs.items():
                            if first > 0:
                                eng.wait_ge(dsem[s_], 16 * first)
                            eng.sem_inc(dsem[s_], 16 * n)

                    def emit_seq(groups, lo):
                        i = 0
                        while i < len(groups):
                            g = groups[i]
                            thr = g[0].pred[1]
                            if thr <= lo:
                                body_of(g)
                                i += 1
                                continue
                            k = i
                            while k < len(groups) and groups[k][0].pred[1] >= thr:
                                k += 1
                            run_ = groups[i:k]
                            snap = dict(waited)
                            with eng.If_lt(self.regs[engname], thr + 1):
                                balance(run_)
                            with eng.Else():
                                emit_seq(run_, thr)
                            waited.clear()
                            waited.update(snap)
                            i = k

                    region = []
                    for ins in self.streams[engname]:
                        if ins.pred is None:
                            if region:
                                emit_seq(region, -1)
                                region = []
                            emit_one(ins)
                        else:
                            if region and region[0][0].pred[0] != ins.pred[0]:
                                emit_seq(region, -1)
                                region = []
                            if region and region[-1][0].pred == ins.pred:
                                region[-1].append(ins)
                            else:
                                region.append([ins])
                    if region:
                        emit_seq(region, -1)
                return body

            block.tensor(run("pe"))
            block.scalar(run("act"))
            block.vector(run("dve"))
            block.gpsimd(run("pool"))
            block.sync(run("sp"))


PT = {}
_off = 0


def _pt(name, n):
    global _off
    PT[name] = _off
    _off += n


_pt("ada_b", 96)
_pt("post_g", 32)
_pt("post_b", 32)
_pt("b_pw1", 16)
_pt("w_dw", 248)
_pt("b_dw", 8)
_pt("cln_g", 8)
_pt("cln_b", 8)
_pt("b_pw2", 8)
_pt("b_gu", 1024)
_pt("subln", 8)
NPT = _off


def _cols(v):
    v = np.asarray(v, np.float32).reshape(-1, 128)
    return np.ascontiguousarray(v.T)


def build_pt(inp):
    pt = np.zeros((128, NPT), np.float32)

    def put(name, arr):
        pt[:, PT[name]:PT[name] + arr.shape[1]] = arr

    put("ada_b", np.concatenate([_cols(inp["ada_b"][l]) for l in range(2)], axis=1))
    put("post_g", np.concatenate([_cols(inp["post_ln_g"][l, s]) for l in range(2) for s in range(2)], axis=1))
    put("post_b", np.concatenate([_cols(inp["post_ln_b"][l, s]) for l in range(2) for s in range(2)], axis=1))
    put("b_pw1", _cols(inp["conv_b_pw1"][0]))
    wdw = np.asarray(inp["conv_w_dw"][0], np.float32)
    put("w_dw", np.ascontiguousarray(wdw.reshape(31, 8, 128).transpose(2, 1, 0).reshape(128, 248)))
    put("b_dw", _cols(inp["conv_b_dw"][0]))
    put("cln_g", _cols(inp["conv_ln_g"][0]))
    put("cln_b", _cols(inp["conv_ln_b"][0]))
    put("b_pw2", _cols(inp["conv_b_pw2"][0]))
    put("b_gu", _cols(np.asarray(inp["expert_b_gate_up"], np.float32).reshape(-1)))
    put("subln", np.tile(np.asarray(inp["attn_subln_g"][0], np.float32).reshape(128, 1), (1, 8)))
    return pt


def build(stop_after=None):
    nc = bass.Bass("TRN2", target_bir_lowering=False)

    def din(name, shape, dt=F32):
        return nc.dram_tensor(name, shape, dt, kind="ExternalInput").ap()

    x_d = din("x", [S, D])
    ct_d = din("ct", [128, 8])
    pt_d = din("pt", [128, NPT])
    ident_d = din("ident", [128, 128])
    ada_w_d = din("ada_w", [2, D, 6 * D])
    wpw1_d = din("w_pw1", [D, 2 * D])
    wpw2_d = din("w_pw2", [D, D])
    wr_d = din("w_r", [2, D, NE])
    br_d = din("b_r", [2, 1, NE])
    wgu_d = din("w_gu", [2, NE, D, 2 * D])
    wdn_d = din("w_dn", [2, NE, D, D])
    bdn_d = din("b_dn", [2, NE, D])
    wkv_d = din("w_kv", [D, 2 * D])
    wq_d = din("w_q", [D, D])
    wo_d = din("w_o", [D, D])
    lpt_d = din("lpt", [64, 4])
    tabs_d = din("tabs", [32, 8])
    ohg_d = din("ohg", [32, RG])
    masks_d = din("masks", [4, 128, BLK])
    bgu_d = din("b_gu", [2, NE, 2 * D])
    tri_d = din("tri", [128, 128])
    iota_d = din("iota", [128, NE])
    xs_d = nc.dram_tensor("xs_scratch", [NE * S, D], BF16).ap()
    ys_d = nc.dram_tensor("ys_scratch", [NE * S, D], F32).ap()
    gs_t = nc.dram_tensor("gs_scratch", [8, 128, RG], F32)
    gs_d = gs_t.ap()
    out_d = nc.dram_tensor("out", [S, D], F32, kind="ExternalOutput").ap()

    P = Prog(nc)
    with ExitStack() as st:
        def T(name, shape, dt=F32):
            return st.enter_context(nc.sbuf_tensor("s_" + name, shape, dt))

        region = {}

        def claim(name, new_bufs):
            old = [b for b in region.get(name, []) if b not in new_bufs]
            for nb in new_bufs:
                for ob in old:
                    cands = list(ob.r.values()) + ([ob.w] if ob.w is not None else [])
                    for p in cands:
                        if p.kind == "d":
                            nb.rd.append(p)
                        else:
                            cur = nb.r.get(p.eng)
                            if cur is None or cur.pos < p.pos:
                                nb.r[p.eng] = p
                    nb.rd.extend(ob.rd)
            region[name] = list(new_bufs)

        R_x = T("R_x", [128, 8 * S])
        R_h = T("R_h", [128, 8192])
        R_w = T("R_w", [128, 12288])
        R_a = T("R_a", [128, 2048])
        R_s = T("R_s", [128, 5 * BLK])
        R_m = T("R_m", [128, 6144])
        GT = R_m[0:32, 0:2048]
        gbT = R_m[:, 2048:4096]
        GTb = R_m[0:32, 4096:5120].bitcast(BF16)
        gsel = R_m[0:32, 5120:6144]
        lpT = T("lpT", [64, 4])
        tabs = T("tabs", [32, 8])
        Mall = T("Mall", [128, 16 * NE], BF16)
        posI = T("posI", [128, 64], I32)
        gates4 = T("gates4", [128, 64])
        cntF = T("cntF", [1, NE])
        cntI = T("cntI", [1, NE], I32)
        trib = T("trib", [128, 128], BF16)
        identb = T("identb", [128, 128], BF16)
        iotaT = T("iotaT", [128, NE])
        iotaP = T("iotaP", [128, NE])
        rt = T("rt", [128, 2 * 400])
        nlam = T("nlam", [128, 4])
        negc = T("negc", [128, 1])
        gsub = T("gsub", [128, 1])
        pt = T("pt", [128, NPT])
        ident = T("ident", [128, 128])
        ones = T("ones", [128, 128])
        onesb = T("onesb", [128, 128], BF16)
        cT = T("cT", [128, 8])
        condb = T("condb", [128, 8], BF16)
        mods = T("mods", [128, 96])
        g1b = T("g1b", [128, 8])
        epsT = T("epsT", [128, 1])
        stage = R_a
        wr_s = T("wr_s", [128, 2 * 8 * NE], BF16)
        br_s = T("br_s", [1, 2 * NE], BF16)
        bdn_s = T("bdn_s", [32, 2 * D], BF16)
        lg = T("lg", [128, 2 * 8 * NE])
        mx8 = T("mx8", [128, 32])

        ps = [st.enter_context(nc.psum_tensor(f"ps{i}", [128, 512], F32)) for i in range(8)]
        bps = [Buf(f"ps{i}") for i in range(8)]

        xT = R_x[:].rearrange("p (c t) -> p c t", c=8)
        bxT = [Buf(f"xT{b}") for b in range(NBLK)]
        b_init = Buf("init")
        b_mods = Buf("mods")

        def blk(tb):
            return slice(tb * BLK, (tb + 1) * BLK)

        P.dma("sp", pt[:], pt_d[:, :], "init", writes=[b_init])
        P.dma("sp", ident[:], ident_d[:, :], "init", writes=[b_init])
        P.dma("sp", cT[:], ct_d[:, :], "init", writes=[b_init])
        P.dma("pool", wr_s[:].rearrange("p (l k e) -> p l k e", l=2, k=8), wr_d.rearrange("l (k p) e -> p l k e", p=128), "initp", writes=[b_init])
        P.dma("pool", br_s[:], br_d.rearrange("l o e -> o (l e)"), "initp", writes=[b_init])
        P.dma("pool", bdn_s[:].rearrange("e (l d) -> e l d", l=2), bdn_d.rearrange("l e d -> e l d"), "initp", writes=[b_init])
        b_c = Buf("consts")
        P.op("dve", lambda e: e.memset(ones[:], 1.0), writes=[b_c])
        P.op("dve", lambda e: e.memset(onesb[:], 1.0), writes=[b_c])
        P.op("dve", lambda e: e.memset(epsT[:], LN_EPS), writes=[b_c])
        P.op("dve", lambda e: e.memset(negc[:], -SM_SHIFT), writes=[b_c])
        P.dma("sp", lpT[:], lpt_d[:, :], "init", writes=[b_init])
        P.dma("sp", tabs[:], tabs_d[:, :], "init", writes=[b_init])
        P.dma("pool", trib[:], tri_d[:, :], "initp", writes=[b_init])
        P.dma("sp", iotaT[:], iota_d[:, :], "init", writes=[b_init])
        blin = pt[:, PT["b_gu"]:PT["b_gu"] + 1024].rearrange("p (g j) -> p g j", j=16)[:, :, 8:16]
        P.op("dve", lambda e: e.tensor_scalar_add(out=blin, in0=blin, scalar1=1.0), reads=[b_init], writes=[b_init])
        P.op("act", lambda e: e.activation(out=condb[:], in_=cT[:], func=AF.Silu), reads=[b_init], writes=[b_c])
        P.op("act", lambda e: e.activation(out=identb[:], in_=ident[:], func=AF.Copy), reads=[b_init], writes=[b_c])
        P.op("act", lambda e: e.activation(out=iotaP[:], in_=iotaT[:], func=AF.Identity, bias=1000.0, scale=1.0), reads=[b_init], writes=[b_c])

        units = []
        for u in range(2):
            base = u * 6144
            units.append(dict(
                raw=R_w[:, base:base + 6144],
                buf=Buf(f"unit{u}"), sem=f"wu{u}"))
        ucount = [0]
        claim("Rw", [u["buf"] for u in units])

        def next_unit():
            u = units[ucount[0] % 2]
            ucount[0] += 1
            return u

        for l in range(2):
            for nb in range(6):
                u = next_unit()
                wv = u["raw"][:, 0:4096].bitcast(BF16).rearrange("p (k n) -> p k n", k=8)
                P.dma("pool", wv, ada_w_d[l, :, nb * 1024:(nb + 1) * 1024].rearrange("(k p) n -> p k n", p=128), u["sem"], writes=[u["buf"]])
                for j in range(8):
                    col = nb * 8 + j
                    for k in range(8):
                        P.op("pe", lambda e, wv=wv, j=j, k=k, col=col, l=l: e.matmul(out=ps[l][:, col:col + 1], lhsT=wv[:, k, j * 128:(j + 1) * 128], rhs=condb[:, k:k + 1], start=(k == 0), stop=(k == 7)),
                             reads=[u["buf"], b_c], writes=[bps[l]])
            P.op("dve", lambda e, l=l: e.tensor_tensor(out=mods[:, l * 48:(l + 1) * 48], in0=ps[l][:, 0:48], in1=pt[:, PT["ada_b"] + l * 48:PT["ada_b"] + (l + 1) * 48], op=ALU.add),
                 reads=[bps[l], b_init], writes=[b_mods])
            for o in (8, 32):
                P.op("dve", lambda e, l=l, o=o: e.tensor_scalar_add(out=mods[:, l * 48 + o:l * 48 + o + 8], in0=mods[:, l * 48 + o:l * 48 + o + 8], scalar1=1.0),
                     reads=[b_mods], writes=[b_mods])
        P.op("dve", lambda e: e.tensor_tensor(out=g1b[:], in0=mods[:, 16:24], in1=pt[:, PT["b_pw2"]:PT["b_pw2"] + 8], op=ALU.mult), reads=[b_mods, b_init], writes=[b_mods])

        def mcol(l, o, c):
            return mods[:, l * 48 + o + c:l * 48 + o + c + 1]

        b_stage = [Buf("stage0"), Buf("stage1")]
        claim("Ra", b_stage)
        for i in range(16):
            sl = i % 2
            sv = stage[:, sl * D:(sl + 1) * D]
            P.dma("sp", sv, x_d[i * 128:(i + 1) * 128, :], f"xin{sl}", writes=[b_stage[sl]])
            for g in range(2):
                bank = 2 + g
                for cc in range(4):
                    c = g * 4 + cc
                    P.op("pe", lambda e, sv=sv, c=c, cc=cc, bank=bank: e.transpose(out=ps[bank][:, cc * 128:(cc + 1) * 128], in_=sv[:, c * 128:(c + 1) * 128], identity=ident[:]),
                         reads=[b_stage[sl], b_init], writes=[bps[bank]])
                eng = "act" if g == 0 else "dve"
                if eng == "act":
                    P.op("act", lambda e, g=g, i=i, bank=bank: e.activation(out=xT[:, g * 4:(g + 1) * 4, i * 128:(i + 1) * 128], in_=ps[bank][:].rearrange("p (c t) -> p c t", c=4), func=AF.Copy),
                         reads=[bps[bank]], writes=[bxT[i // 4]])
                else:
                    P.op("dve", lambda e, g=g, i=i, bank=bank: e.tensor_copy(out=xT[:, g * 4:(g + 1) * 4, i * 128:(i + 1) * 128], in_=ps[bank][:].rearrange("p (c t) -> p c t", c=4)),
                         reads=[bps[bank]], writes=[bxT[i // 4]])

        sq = [R_s[:, 0:BLK], R_s[:, BLK:2 * BLK]]
        b_sq = [Buf("sq0"), Buf("sq1")]
        mean_t = R_s[:, 2 * BLK:3 * BLK]
        rstd_t = R_s[:, 3 * BLK:4 * BLK]
        nmr_t = R_s[:, 4 * BLK:5 * BLK]
        b_stat = Buf("stat")
        sqc = [0]

        def _clone(b):
            nb = Buf(b.name + "_c")
            nb.w = b.w
            nb.r = dict(b.r)
            nb.rd = list(b.rd)
            return nb

        def layer_norm_block(src, b_src, dst, b_dst, gcol, bcol, func, bank_s, bank_q):
            inplace = b_src is b_dst
            cbuf = [_clone(b_src) for _ in range(8)]
            dbuf = cbuf if inplace else [_clone(b_dst) for _ in range(8)]
            for c in range(8):
                s = sqc[0] % 2
                sqc[0] += 1
                P.op("act", lambda e, c=c, s=s: e.activation(out=sq[s], in_=src(c), func=AF.Square), reads=[cbuf[c]], writes=[b_sq[s]])
                P.op("pe", lambda e, c=c: e.matmul(out=ps[bank_s][:], lhsT=ones[:], rhs=src(c), start=(c == 0), stop=(c == 7)), reads=[cbuf[c], b_c], writes=[bps[bank_s]])
                P.op("pe", lambda e, c=c, s=s: e.matmul(out=ps[bank_q][:], lhsT=ones[:], rhs=sq[s], start=(c == 0), stop=(c == 7)), reads=[b_sq[s], b_c], writes=[bps[bank_q]])
            P.op("dve", lambda e: e.tensor_scalar_mul(out=mean_t, in0=ps[bank_s][:], scalar1=1.0 / D), reads=[bps[bank_s]], writes=[b_stat])
            P.op("dve", lambda e: e.tensor_tensor(out=nmr_t, in0=mean_t, in1=mean_t, op=ALU.mult), reads=[b_stat], writes=[b_stat])
            P.op("dve", lambda e: e.scalar_tensor_tensor(out=rstd_t, in0=ps[bank_q][:], scalar=1.0 / D, in1=nmr_t, op0=ALU.mult, op1=ALU.subtract), reads=[bps[bank_q], b_stat], writes=[b_stat])
            P.op("act", lambda e: e.activation(out=rstd_t, in_=rstd_t, func=AF.Sqrt, bias=epsT[:, 0:1], scale=1.0), reads=[b_stat, b_c], writes=[b_stat])
            P.op("dve", lambda e: e.reciprocal(out=rstd_t, in_=rstd_t), reads=[b_stat], writes=[b_stat])
            P.op("dve", lambda e: e.scalar_tensor_tensor(out=nmr_t, in0=mean_t, scalar=-1.0, in1=rstd_t, op0=ALU.mult, op1=ALU.mult), reads=[b_stat], writes=[b_stat])
            last = {}
            for c in range(8):
                last["d1"] = P.op("dve", lambda e, c=c: e.tensor_tensor(out=src(c), in0=src(c), in1=rstd_t, op=ALU.mult), reads=[cbuf[c], b_stat], writes=[cbuf[c]])
                last["d2"] = P.op("dve", lambda e, c=c: e.tensor_tensor(out=src(c), in0=src(c), in1=nmr_t, op=ALU.add), reads=[cbuf[c], b_stat], writes=[cbuf[c]])
                last["a"] = P.op("act", lambda e, c=c: e.activation(out=dst(c), in_=src(c), func=func, scale=pt[:, gcol + c:gcol + c + 1], bias=pt[:, bcol + c:bcol + c + 1]),
                                 reads=[cbuf[c], b_init], writes=[dbuf[c]])
            if inplace:
                b_src.w = last["a"]
                b_src.r = {}
                b_src.rd = []
            else:
                b_src.w = last["d2"]
                b_src.r = {"act": last["a"]}
                b_src.rd = []
                b_dst.w = last["a"]
                b_dst.r = {}
                b_dst.rd = []

        def conv_sublayer():
            l = 0
            wpw1 = R_w[:, 0:8192].bitcast(BF16).rearrange("p (k n) -> p k n", k=8)
            wpw2 = R_w[:, 8192:12288].bitcast(BF16).rearrange("p (k n) -> p k n", k=8)
            b_w1 = [Buf(f"wpw1_{i}") for i in range(4)]
            b_w2 = Buf("wpw2")
            claim("Rw", b_w1 + [b_w2])
            for i in range(4):
                P.dma("pool", wpw1[:, :, i * 512:(i + 1) * 512], wpw1_d[:, i * 512:(i + 1) * 512].rearrange("(k p) n -> p k n", p=128), "wconv",
                      writes=[b_w1[i]])
            P.dma("pool", wpw2, wpw2_d.rearrange("(k p) n -> p k n", p=128), "wconv", writes=[b_w2])
            hblk = R_h[:, 0:2048].bitcast(BF16).rearrange("p (c t) -> p c t", c=8)
            sblk = R_h[:, 2048:4096].bitcast(BF16).rearrange("p (c t) -> p c t", c=8)
            vblk = R_h[:, 4096:8192].rearrange("p (c t) -> p c t", c=8)
            ub = [R_a[:, 0:271].bitcast(BF16), R_a[:, 272:543].bitcast(BF16)]
            halo = R_a[:, 1084:1084 + 120].bitcast(BF16).rearrange("p (c t) -> p c t", c=8)
            diag = [R_m[:, 0:1984].bitcast(BF16).rearrange("p (t n) -> p t n", t=31), R_m[:, 2048:2048 + 1984].bitcast(BF16).rearrange("p (t n) -> p t n", t=31)]
            b_diag = [Buf("diag0"), Buf("diag1")]
            claim("Rm", b_diag)
            sgt = [R_a[:, 1324:1324 + 512], R_s[:, 0:BLK]]
            b_h, b_s, b_v = Buf("hblk"), Buf("sblk"), Buf("vblk")
            b_u = [Buf("u0"), Buf("u1")]
            b_halo = Buf("halo")
            b_sg = [Buf("sg0"), b_sq[0]]
            claim("Rs", [b_sq[0], b_sq[1], b_stat])
            claim("Rh", [b_h, b_s, b_v])
            claim("Ra", [b_u[0], b_u[1], b_halo, b_sg[0]])
            P.op("pool", lambda e: e.memset(halo, 0.0), writes=[b_halo])
            wdw0 = PT["w_dw"]
            for tb in range(NBLK):
                for c in range(8):
                    P.op("act", lambda e, c=c, tb=tb: e.activation(out=hblk[:, c, :], in_=xT[:, c, blk(tb)], func=AF.Identity, scale=mcol(l, 8, c), bias=mcol(l, 0, c)),
                         reads=[bxT[tb], b_mods], writes=[b_h])
                def pw1(j):
                    ba, bg = (0, 1) if j % 2 == 0 else (2, 3)
                    s = j % 2
                    dg = diag[s]
                    P.op("dve", lambda e: e.tensor_tensor(out=dg, in0=identb[:].unsqueeze(1).to_broadcast([128, 31, 128]),
                                                          in1=pt[:, wdw0 + j * 31:wdw0 + (j + 1) * 31].unsqueeze(2).to_broadcast([128, 31, 128]), op=ALU.mult),
                         reads=[b_c, b_init], writes=[b_diag[s]])
                    for k in range(8):
                        P.op("pe", lambda e, k=k: e.matmul(out=ps[ba][:], lhsT=wpw1[:, k, j * 128:(j + 1) * 128], rhs=hblk[:, k, :], start=(k == 0), stop=(k == 7)),
                             reads=[b_w1[j // 4], b_h], writes=[bps[ba]])
                    for k in range(8):
                        P.op("pe", lambda e, k=k: e.matmul(out=ps[bg][:], lhsT=wpw1[:, k, 1024 + j * 128:1024 + (j + 1) * 128], rhs=hblk[:, k, :], start=(k == 0), stop=(k == 7)),
                             reads=[b_w1[2 + j // 4], b_h], writes=[bps[bg]])

                def glu_conv(j):
                    ba, bg = (0, 1) if j % 2 == 0 else (2, 3)
                    s = j % 2
                    u_ = ub[s]
                    dg = diag[s]
                    P.op("act", lambda e: e.activation(out=sgt[s], in_=ps[bg][:], func=AF.Sigmoid, bias=pt[:, PT["b_pw1"] + 8 + j:PT["b_pw1"] + 9 + j], scale=1.0),
                         reads=[bps[bg], b_init], writes=[b_sg[s]])
                    P.op("pool", lambda e: e.tensor_copy(out=u_[:, 0:30], in_=halo[:, j, :]), reads=[b_halo], writes=[b_u[s]])
                    P.op("dve", lambda e: e.scalar_tensor_tensor(out=u_[:, 30:542], in0=ps[ba][:], scalar=pt[:, PT["b_pw1"] + j:PT["b_pw1"] + j + 1], in1=sgt[s], op0=ALU.add, op1=ALU.mult),
                         reads=[bps[ba], b_sg[s], b_init], writes=[b_u[s]])
                    P.op("pool", lambda e: e.tensor_copy(out=halo[:, j, :], in_=u_[:, 512:542]), reads=[b_u[s]], writes=[b_halo])
                    cb = 6 + s
                    for tap in range(31):
                        P.op("pe", lambda e, tap=tap: e.matmul(out=ps[cb][:], lhsT=dg[:, tap, :], rhs=u_[:, tap:tap + 512], start=(tap == 0), stop=(tap == 30)),
                             reads=[b_diag[s], b_u[s]], writes=[bps[cb]])
                    P.op("dve", lambda e: e.tensor_scalar(out=vblk[:, j, :], in0=ps[cb][:], scalar1=pt[:, PT["b_dw"] + j:PT["b_dw"] + j + 1], scalar2=None, op0=ALU.add),
                         reads=[bps[cb], b_init], writes=[b_v])

                pw1(0)
                for j in range(8):
                    if j + 1 < 8:
                        pw1(j + 1)
                    glu_conv(j)
                layer_norm_block(lambda c: vblk[:, c, :], b_v, lambda c: sblk[:, c, :], b_s, PT["cln_g"], PT["cln_b"], AF.Silu, 4, 5)
                for d in range(8):
                    bank = 6 + d % 2
                    for k in range(8):
                        P.op("pe", lambda e, d=d, k=k, bank=bank: e.matmul(out=ps[bank][:], lhsT=wpw2[:, k, d * 128:(d + 1) * 128], rhs=sblk[:, k, :], start=(k == 0), stop=(k == 7)),
                             reads=[b_w2, b_s], writes=[bps[bank]])
                    s = d % 2
                    P.op("act", lambda e, d=d, bank=bank, s=s: e.activation(out=sgt[s], in_=ps[bank][:], func=AF.Identity, scale=mcol(l, 16, d), bias=g1b[:, d:d + 1]),
                         reads=[bps[bank], b_mods], writes=[b_sg[s]])
                    P.op("dve", lambda e, d=d, tb=tb, s=s: e.scalar_tensor_tensor(out=xT[:, d, blk(tb)], in0=xT[:, d, blk(tb)], scalar=ALPHA, in1=sgt[s], op0=ALU.mult, op1=ALU.add),
                         reads=[b_sg[s], bxT[tb]], writes=[bxT[tb]])
                pg = PT["post_g"] + (l * 2 + 0) * 8
                pb = PT["post_b"] + (l * 2 + 0) * 8
                layer_norm_block(lambda c, tb=tb: xT[:, c, blk(tb)], bxT[tb], lambda c, tb=tb: xT[:, c, blk(tb)], bxT[tb], pg, pb, AF.Identity, 4, 5)

        def moe_sublayer(l):
            hT = R_h[:].bitcast(BF16).rearrange("p (c t) -> p c t", c=8)
            b_hT = [Buf(f"hT{b}") for b in range(NBLK)]
            b_GT = Buf("GT")
            b_lg = Buf("lg")
            actT = [R_a[:, 0:1024].bitcast(BF16).rearrange("p (c t) -> p c t", c=4), R_a[:, 1024:2048].bitcast(BF16).rearrange("p (c t) -> p c t", c=4)]
            b_act = [Buf("act0"), Buf("act1")]
            gc = R_s[:, 0:BLK]
            sgm = R_s[:, BLK:2 * BLK]
            lin = R_s[:, 2 * BLK:3 * BLK]
            b_gc, b_sgm, b_lin = b_sq[0], b_sq[1], b_stat
            b_gb = Buf("gb")
            b_gsel = [Buf("gsel0"), Buf("gsel1")]
            claim("Rh", b_hT)
            claim("Rm", [b_GT, b_gb] + b_gsel)
            claim("Rs", [b_sq[0], b_sq[1], b_stat])
            claim("Ra", b_act)
            claim("Rw", [u["buf"] for u in units])
            for tb in range(NBLK):
                for c in range(8):
                    P.op("act", lambda e, c=c, tb=tb: e.activation(out=hT[:, c, blk(tb)], in_=xT[:, c, blk(tb)], func=AF.Identity, scale=mcol(l, 32, c), bias=mcol(l, 24, c)),
                         reads=[bxT[tb], b_mods], writes=[b_hT[tb]])
                for c in range(8):
                    P.op("dve", lambda e, c=c, tb=tb: e.tensor_scalar_mul(out=xT[:, c, blk(tb)], in0=xT[:, c, blk(tb)], scalar1=ALPHA),
                         reads=[bxT[tb]], writes=[bxT[tb]])
            wr = wr_s[:].rearrange("p (l k e) -> p l k e", l=2, k=8)
            for i in range(16):
                tb = i // 4
                tsl = slice(i * 128, (i + 1) * 128)
                for k in range(8):
                    P.op("pe", lambda e, k=k, tsl=tsl: e.matmul(out=ps[7][:, 0:NE], lhsT=hT[:, k, tsl], rhs=wr[:, l, k, :], start=(k == 0), stop=False),
                         reads=[b_hT[tb], b_init], writes=[bps[7]])
                P.op("pe", lambda e: e.matmul(out=ps[7][:, 0:NE], lhsT=onesb[0:1, :], rhs=br_s[0:1, l * NE:(l + 1) * NE], start=False, stop=True),
                     reads=[b_c, b_init], writes=[bps[7]])
                L = lg[:, 0:NE]
                M = lg[:, NE:2 * NE]
                E = lg[:, 2 * NE:3 * NE]
                G = lg[:, 3 * NE:4 * NE]
                P.op("dve", lambda e, L=L: e.tensor_copy(out=L, in_=ps[7][:, 0:NE]), reads=[bps[7]], writes=[b_lg])
                P.op("dve", lambda e, L=L: e.max(out=mx8[:, 0:8], in_=L), reads=[b_lg], writes=[b_lg])
                P.op("dve", lambda e, L=L, M=M: e.tensor_scalar(out=M, in0=L, scalar1=mx8[:, 3:4], scalar2=None, op0=ALU.is_ge), reads=[b_lg], writes=[b_lg])
                P.op("dve", lambda e: e.tensor_scalar_mul(out=mx8[:, 8:9], in0=mx8[:, 0:1], scalar1=-1.0), reads=[b_lg], writes=[b_lg])
                P.op("act", lambda e, L=L, E=E: e.activation(out=E, in_=L, func=AF.Exp, bias=mx8[:, 8:9], scale=1.0), reads=[b_lg], writes=[b_lg])
                P.op("dve", lambda e, M=M, E=E: e.tensor_tensor(out=E, in0=E, in1=M, op=ALU.mult), reads=[b_lg], writes=[b_lg])
                P.op("dve", lambda e, E=E: e.reduce_sum(out=mx8[:, 9:10], in_=E, axis=AX.X), reads=[b_lg], writes=[b_lg])
                P.op("dve", lambda e: e.reciprocal(out=mx8[:, 10:11], in_=mx8[:, 9:10]), reads=[b_lg], writes=[b_lg])
                P.op("dve", lambda e, E=E, G=G: e.tensor_scalar(out=G, in0=E, scalar1=mx8[:, 10:11], scalar2=None, op0=ALU.mult), reads=[b_lg], writes=[b_lg])
                P.op("pe", lambda e, G=G: e.transpose(out=ps[6][0:NE, 0:128], in_=G, identity=ident[:]), reads=[b_lg, b_init], writes=[bps[6]])
                P.op("act", lambda e, tsl=tsl: e.activation(out=GT[:, tsl], in_=ps[6][0:NE, 0:128], func=AF.Copy), reads=[bps[6]], writes=[b_GT])
                P.op("dve", lambda e, tsl=tsl: e.tensor_copy(out=GTb[:, tsl], in_=ps[6][0:NE, 0:128]), reads=[bps[6]], writes=[b_GT])

            def issue_unit(uidx):
                e_, half = uidx // 2, uidx % 2
                u = next_unit()
                g = u["raw"][:, 0:2048].bitcast(BF16).rearrange("p (k n) -> p k n", k=8)
                lw = u["raw"][:, 2048:4096].bitcast(BF16).rearrange("p (k n) -> p k n", k=8)
                dw = u["raw"][:, 4096:6144].bitcast(BF16).rearrange("p (f n) -> p f n", f=4)
                P.dma("pool", g, wgu_d[l, e_, :, half * 512:(half + 1) * 512].rearrange("(k p) n -> p k n", p=128), u["sem"], writes=[u["buf"]])
                P.dma("pool", lw, wgu_d[l, e_, :, 1024 + half * 512:1024 + (half + 1) * 512].rearrange("(k p) n -> p k n", p=128), u["sem"], writes=[u["buf"]])
                P.dma("pool", dw, wdn_d[l, e_, half * 512:(half + 1) * 512, :].rearrange("(f p) n -> p f n", p=128), u["sem"], writes=[u["buf"]])
                return (u, g, lw, dw)

            NU = 2 * NE
            pending = [issue_unit(0)]
            actc = [0]
            for uidx in range(NU):
                if uidx + 1 < NU:
                    pending.append(issue_unit(uidx + 1))
                u, g, lw, dw = pending.pop(0)
                e_, half = uidx // 2, uidx % 2
                bcol = (l * NE + e_) * 16
                if half == 0:
                    for tb in range(NBLK):
                        gs = gsel[:, (tb % 2) * BLK:(tb % 2 + 1) * BLK]
                        P.op("dve", lambda e, e_=e_, tb=tb, gs=gs: e.tensor_scalar(out=gs, in0=GT[:, blk(tb)], scalar1=ident[0:NE, e_:e_ + 1], scalar2=None, op0=ALU.mult), reads=[b_GT, b_init], writes=[b_gsel[tb % 2]])
                        P.op("pe", lambda e, gs=gs: e.matmul(out=ps[6][:], lhsT=ones[0:NE, :], rhs=gs, start=True, stop=True), reads=[b_gsel[tb % 2], b_c], writes=[bps[6]])
                        P.op("act", lambda e, tb=tb: e.activation(out=gbT[:, blk(tb)], in_=ps[6][:], func=AF.Copy), reads=[bps[6]], writes=[b_gb])
                for tb in range(NBLK):
                    a = actc[0] % 2
                    actc[0] += 1
                    for fc in range(4):
                        pg_, pl_ = (0, 1) if fc % 2 == 0 else (2, 3)
                        for k in range(8):
                            P.op("pe", lambda e, g=g, k=k, fc=fc, tb=tb, pg_=pg_: e.matmul(out=ps[pg_][:], lhsT=g[:, k, fc * 128:(fc + 1) * 128], rhs=hT[:, k, blk(tb)], start=(k == 0), stop=(k == 7)),
                                 reads=[u["buf"], b_hT[tb]], writes=[bps[pg_]])
                        for k in range(8):
                            P.op("pe", lambda e, lw=lw, k=k, fc=fc, tb=tb, pl_=pl_: e.matmul(out=ps[pl_][:], lhsT=lw[:, k, fc * 128:(fc + 1) * 128], rhs=hT[:, k, blk(tb)], start=(k == 0), stop=(k == 7)),
                                 reads=[u["buf"], b_hT[tb]], writes=[bps[pl_]])
                        jg = bcol + half * 4 + fc
                        jl = bcol + 8 + half * 4 + fc
                        P.op("dve", lambda e, pg_=pg_, jg=jg: e.tensor_scalar(out=gc, in0=ps[pg_][:], scalar1=pt[:, PT["b_gu"] + jg:PT["b_gu"] + jg + 1], scalar2=7.0, op0=ALU.add, op1=ALU.min),
                             reads=[bps[pg_], b_init], writes=[b_gc])
                        P.op("act", lambda e: e.activation(out=sgm, in_=gc, func=AF.Sigmoid, scale=1.702), reads=[b_gc], writes=[b_sgm])
                        P.op("dve", lambda e, pl_=pl_, jl=jl: e.tensor_scalar(out=lin, in0=ps[pl_][:], scalar1=pt[:, PT["b_gu"] + jl:PT["b_gu"] + jl + 1], scalar2=8.0, op0=ALU.add, op1=ALU.min),
                             reads=[bps[pl_], b_init], writes=[b_lin])
                        P.op("dve", lambda e: e.tensor_tensor(out=gc, in0=gc, in1=sgm, op=ALU.mult), reads=[b_gc, b_sgm], writes=[b_gc])
                        P.op("dve", lambda e: e.scalar_tensor_tensor(out=lin, in0=lin, scalar=-6.0, in1=gc, op0=ALU.max, op1=ALU.mult), reads=[b_lin, b_gc], writes=[b_lin])
                        P.op("dve", lambda e, a=a, fc=fc, tb=tb: e.tensor_tensor(out=actT[a][:, fc, :], in0=lin, in1=gbT[:, blk(tb)], op=ALU.mult), reads=[b_lin, b_gb], writes=[b_act[a]])
                    for d in range(8):
                        bank = 4 + d % 2
                        for fc in range(4):
                            last = (fc == 3) and not (uidx == 0)
                            P.op("pe", lambda e, dw=dw, fc=fc, d=d, a=a, bank=bank, last=last: e.matmul(out=ps[bank][:], lhsT=dw[:, fc, d * 128:(d + 1) * 128], rhs=actT[a][:, fc, :], start=(fc == 0), stop=last),
                                 reads=[u["buf"], b_act[a]], writes=[bps[bank]])
                        if uidx == 0:
                            P.op("pe", lambda e, d=d, tb=tb, bank=bank: e.matmul(out=ps[bank][:], lhsT=bdn_s[:, l * D + d * 128:l * D + (d + 1) * 128], rhs=GTb[:, blk(tb)], start=False, stop=True),
                                 reads=[b_init, b_GT], writes=[bps[bank]])
                        P.op("dve", lambda e, d=d, tb=tb, bank=bank: e.scalar_tensor_tensor(out=xT[:, d, blk(tb)], in0=ps[bank][:], scalar=mcol(l, 40, d), in1=xT[:, d, blk(tb)], op0=ALU.mult, op1=ALU.add),
                             reads=[bps[bank], b_mods, bxT[tb]], writes=[bxT[tb]])
            pg = PT["post_g"] + (l * 2 + 1) * 8
            pb = PT["post_b"] + (l * 2 + 1) * 8
            for tb in range(NBLK):
                layer_norm_block(lambda c, tb=tb: xT[:, c, blk(tb)], bxT[tb], lambda c, tb=tb: xT[:, c, blk(tb)], bxT[tb], pg, pb, AF.Identity, 0, 1)

        def attn_sublayer():
            l = 1
            xb = R_h[:].bitcast(BF16).rearrange("p (c t) -> p c t", c=8)
            h1b = R_w[:, 0:8192].bitcast(BF16).rearrange("p (c t) -> p c t", c=8)
            KT = R_w[:, 8192:9216].bitcast(BF16)
            QT = R_w[:, 9216:10240].bitcast(BF16)
            Vh = R_w[:, 10240:11264].bitcast(BF16).rearrange("p (i e) -> p i e", i=16)
            AT = R_w[:, 11264:12288].bitcast(BF16)
            Zs = R_m[:, 4096:5120]
            masks = R_a[:].rearrange("p (o q) -> p o q", o=4)
            PTt = [R_s[:, 0:256].bitcast(BF16), R_s[:, 256:512].bitcast(BF16)]
            tmp = [R_s[:, 512:1024], R_s[:, 1024:1536]]
            o0 = R_s[:, 1536:2048]
            rsc = R_s[:, 2048:2560]
            b_xb = [Buf(f"xb{i}") for i in range(NBLK)]
            b_h1 = [Buf(f"h1b{i}") for i in range(NBLK)]
            b_KT, b_QT, b_V, b_AT = Buf("KT"), Buf("QT"), Buf("Vh"), Buf("AT")
            b_Zs, b_mask = Buf("Zs"), Buf("masks")
            b_PT = [Buf("PT0"), Buf("PT1")]
            b_tmp = [Buf("tmp0"), Buf("tmp1")]
            b_o0, b_rsc = Buf("o0"), Buf("rsc")
            b_wt = [Buf("awt0"), Buf("awt1")]
            b_gs = [Buf(f"gs{h}") for h in range(8)]
            b_lam = Buf("lam")
            claim("Rh", b_xb)
            claim("Rw", b_h1 + [b_KT, b_QT, b_V, b_AT])
            claim("Rm", b_wt + [b_Zs])
            claim("Ra", [b_mask])
            P.dma("sp", masks, masks_d.rearrange("o p q -> p o q"), "amask", writes=[b_mask])
            P.op("dve", lambda e: e.tensor_tensor(out=lpT[:, 0:1], in0=lpT[:, 0:1], in1=lpT[:, 1:2], op=ALU.mult), reads=[b_init], writes=[b_lam])
            P.op("dve", lambda e: e.tensor_tensor(out=lpT[:, 1:2], in0=lpT[:, 2:3], in1=lpT[:, 3:4], op=ALU.mult), reads=[b_init, b_lam], writes=[b_lam])
            P.op("pe", lambda e: e.matmul(out=ps[7][:, 0:2], lhsT=ones[0:64, :], rhs=lpT[:, 0:2], start=True, stop=True), reads=[b_lam, b_c], writes=[bps[7]])
            P.op("act", lambda e: e.activation(out=nlam[:, 0:2], in_=ps[7][:, 0:2], func=AF.Exp), reads=[bps[7]], writes=[b_lam])
            P.op("dve", lambda e: e.tensor_tensor(out=nlam[:, 2:3], in0=nlam[:, 1:2], in1=nlam[:, 0:1], op=ALU.subtract), reads=[b_lam], writes=[b_lam])
            P.op("dve", lambda e: e.tensor_scalar_add(out=nlam[:, 3:4], in0=nlam[:, 2:3], scalar1=-LAMI), reads=[b_lam], writes=[b_lam])
            P.op("dve", lambda e: e.tensor_scalar_mul(out=gsub[:], in0=pt[:, PT["subln"]:PT["subln"] + 1], scalar1=1.0 - LAMI), reads=[b_init], writes=[b_lam])
            gst = R_s[:, 0:RG]
            rep = R_s[0:32, RG:RG + 128]
            ohg = R_s[0:32, RG + 128:RG + 128 + RG]
            b_gst, b_rep = Buf("gst"), Buf("rep")
            claim("Rs", [b_gst, b_rep])
            P.dma("sp", ohg, ohg_d[:, :], "ohg", writes=[b_rep])
            for h in range(8):
                P.op("act", lambda e, h=h: e.activation(out=rep, in_=ones[0:32, :], func=AF.Identity, scale=tabs[:, h:h + 1]), reads=[b_init, b_c], writes=[b_rep])
                for (n0, nn) in ((0, 512), (512, 512), (1024, 128)):
                    P.op("pe", lambda e, n0=n0, nn=nn: e.matmul(out=ps[6][:, 0:nn], lhsT=rep, rhs=ohg[:, n0:n0 + nn], start=True, stop=True), reads=[b_rep, b_init], writes=[bps[6]])
                    P.op("dve", lambda e, n0=n0, nn=nn: e.tensor_copy(out=gst[:, n0:n0 + nn], in_=ps[6][:, 0:nn]), reads=[bps[6]], writes=[b_gst])
                P.dma("sp", gs_d[h, :, :], gst, "gsw", reads=[b_gst], writes=[b_gs[h]])
            for b in b_PT + b_tmp + [b_o0, b_rsc]:
                pass
            claim("Rs", b_PT + b_tmp + [b_o0, b_rsc])
            for tb in range(NBLK):
                for c in range(8):
                    P.op("act", lambda e, c=c, tb=tb: e.activation(out=xb[:, c, blk(tb)], in_=xT[:, c, blk(tb)], func=AF.Copy), reads=[bxT[tb]], writes=[b_xb[tb]])
                    P.op("act", lambda e, c=c, tb=tb: e.activation(out=h1b[:, c, blk(tb)], in_=xT[:, c, blk(tb)], func=AF.Identity, scale=mcol(l, 8, c), bias=mcol(l, 0, c)),
                         reads=[bxT[tb], b_mods], writes=[b_h1[tb]])
                for c in range(8):
                    P.op("dve", lambda e, c=c, tb=tb: e.tensor_scalar_mul(out=xT[:, c, blk(tb)], in0=xT[:, c, blk(tb)], scalar1=ALPHA), reads=[bxT[tb]], writes=[bxT[tb]])
            for h in range(8):
                wsl = h % 2
                wbase = wsl * 2048
                wk = R_m[:, wbase:wbase + 512].bitcast(BF16).rearrange("p (k n) -> p k n", k=8)
                wv = R_m[:, wbase + 512:wbase + 1024].bitcast(BF16).rearrange("p (k n) -> p k n", k=8)
                wq = R_m[:, wbase + 1024:wbase + 1536].bitcast(BF16).rearrange("p (k n) -> p k n", k=8)
                wo = R_m[:, wbase + 1536:wbase + 2048].bitcast(BF16)
                hs = slice(h * 128, (h + 1) * 128)
                P.dma("pool", wk, wkv_d[:, hs].rearrange("(k p) n -> p k n", p=128), f"awt{wsl}", writes=[b_wt[wsl]])
                P.dma("pool", wv, wkv_d[:, D + h * 128:D + (h + 1) * 128].rearrange("(k p) n -> p k n", p=128), f"awt{wsl}", writes=[b_wt[wsl]])
                P.dma("pool", wq, wq_d[:, hs].rearrange("(k p) n -> p k n", p=128), f"awt{wsl}", writes=[b_wt[wsl]])
                P.dma("pool", wo, wo_d[hs, :], f"awt{wsl}", writes=[b_wt[wsl]])
                zsrc = bass.AP(gs_t, h * 128 * RG + 127, [[RG - 1, 128], [1, 1024]])
                P.dma("sp", Zs, zsrc, "zs", reads=[b_gs[h]], writes=[b_Zs])
                for tb in range(NBLK):
                    for k in range(8):
                        P.op("pe", lambda e, k=k, tb=tb, wk=wk: e.matmul(out=ps[6][:], lhsT=wk[:, k, :], rhs=xb[:, k, blk(tb)], start=(k == 0), stop=(k == 7)), reads=[b_wt[wsl], b_xb[tb]], writes=[bps[6]])
                    P.op("act", lambda e, tb=tb: e.activation(out=KT[:, blk(tb)], in_=ps[6][:], func=AF.Copy), reads=[bps[6]], writes=[b_KT])
                    for k in range(8):
                        P.op("pe", lambda e, k=k, tb=tb, wq=wq: e.matmul(out=ps[7][:], lhsT=wq[:, k, :], rhs=h1b[:, k, blk(tb)], start=(k == 0), stop=(k == 7)), reads=[b_wt[wsl], b_h1[tb]], writes=[bps[7]])
                    P.op("dve", lambda e, tb=tb: e.tensor_copy(out=QT[:, blk(tb)], in_=ps[7][:]), reads=[bps[7]], writes=[b_QT])
                for i4 in range(4):
                    bank = 6 + i4 % 2
                    for ii in range(4):
                        i = i4 * 4 + ii
                        for k in range(8):
                            P.op("pe", lambda e, k=k, i=i, ii=ii, wv=wv, bank=bank: e.matmul(out=ps[bank][:, ii * 128:(ii + 1) * 128], lhsT=xb[:, k, i * 128:(i + 1) * 128], rhs=wv[:, k, :], start=(k == 0), stop=(k == 7)),
                                 reads=[b_wt[wsl], b_xb[i // 4]], writes=[bps[bank]])
                    if i4 % 2 == 0:
                        P.op("act", lambda e, i4=i4, bank=bank: e.activation(out=Vh[:, i4 * 4:(i4 + 1) * 4, :], in_=ps[bank][:].rearrange("p (i e) -> p i e", i=4), func=AF.Copy), reads=[bps[bank]], writes=[b_V])
                    else:
                        P.op("dve", lambda e, i4=i4, bank=bank: e.tensor_copy(out=Vh[:, i4 * 4:(i4 + 1) * 4, :], in_=ps[bank][:].rearrange("p (i e) -> p i e", i=4)), reads=[bps[bank]], writes=[b_V])
                tiles = [(qb, m, kt) for qb in range(NBLK) for m in range(2) for kt in range(4 * qb + 4)]
                NTL = len(tiles)
                pending = []

                def stageA(idx):
                    qb, m, kt = tiles[idx]
                    msl = slice(m * 64, (m + 1) * 64)
                    off = kt * 128 - qb * 512
                    sb = idx % 2
                    P.op("pe", lambda e: e.matmul(out=ps[sb][:], lhsT=KT[msl, kt * 128:(kt + 1) * 128], rhs=QT[msl, blk(qb)], start=True, stop=True),
                         reads=[b_KT, b_QT], writes=[bps[sb]])
                    if off <= -256:
                        P.op("act", lambda e: e.activation(out=PTt[sb], in_=ps[sb][:], func=AF.Exp, scale=0.125, bias=negc[:, 0:1]), reads=[bps[sb], b_c], writes=[b_PT[sb]])
                    else:
                        z0 = 384 - off
                        P.op("dve", lambda e: e.scalar_tensor_tensor(out=tmp[sb], in0=ps[sb][:], scalar=0.125, in1=Zs[:, z0:z0 + 512], op0=ALU.mult, op1=ALU.add),
                             reads=[bps[sb], b_Zs], writes=[b_tmp[sb]])
                        if off >= 0:
                            P.op("dve", lambda e: e.tensor_tensor(out=tmp[sb], in0=tmp[sb], in1=masks[:, off // 128, :], op=ALU.add), reads=[b_tmp[sb], b_mask], writes=[b_tmp[sb]])
                        P.op("act", lambda e: e.activation(out=PTt[sb], in_=tmp[sb], func=AF.Exp, scale=1.0, bias=negc[:, 0:1]), reads=[b_tmp[sb], b_c], writes=[b_PT[sb]])

                def seg1(qb, m):
                    po, psm = (2, 3) if m == 0 else (4, 5)
                    P.op("dve", lambda e: e.reciprocal(out=rsc, in_=ps[psm][:]), reads=[bps[psm]], writes=[b_rsc])
                    if m == 0:
                        P.op("dve", lambda e: e.tensor_tensor(out=o0, in0=ps[po][:], in1=rsc, op=ALU.mult), reads=[bps[po], b_rsc], writes=[b_o0])
                    else:
                        P.op("dve", lambda e: e.tensor_tensor(out=rsc, in0=ps[po][:], in1=rsc, op=ALU.mult), reads=[bps[po], b_rsc], writes=[b_rsc])
                        P.op("dve", lambda e: e.scalar_tensor_tensor(out=o0, in0=rsc, scalar=nlam[:, 3:4], in1=o0, op0=ALU.mult, op1=ALU.add), reads=[b_rsc, b_o0, b_lam], writes=[b_o0])
                        P.op("act", lambda e: e.activation(out=rsc, in_=o0, func=AF.Square), reads=[b_o0], writes=[b_rsc])

                def seg2(qb):
                    P.op("pe", lambda e: e.matmul(out=ps[6][:], lhsT=ones[:], rhs=rsc, start=True, stop=True), reads=[b_rsc, b_c], writes=[bps[6]])
                    P.op("act", lambda e: e.activation(out=rsc, in_=ps[6][:], func=AF.Sqrt, scale=1.0 / 128.0, bias=epsT[:, 0:1]), reads=[bps[6], b_c], writes=[b_rsc])
                    P.op("dve", lambda e: e.reciprocal(out=rsc, in_=rsc), reads=[b_rsc], writes=[b_rsc])
                    P.op("dve", lambda e: e.tensor_tensor(out=o0, in0=o0, in1=rsc, op=ALU.mult), reads=[b_o0, b_rsc], writes=[b_o0])
                    P.op("act", lambda e: e.activation(out=AT[:, blk(qb)], in_=o0, func=AF.Identity, scale=gsub[:, 0:1]), reads=[b_o0, b_lam], writes=[b_AT])

                def seg3(qb):
                    for d in range(8):
                        bank = 6 + d % 2
                        P.op("pe", lambda e, d=d, bank=bank, wo=wo: e.matmul(out=ps[bank][:], lhsT=wo[:, d * 128:(d + 1) * 128], rhs=AT[:, blk(qb)], start=True, stop=True), reads=[b_wt[wsl], b_AT], writes=[bps[bank]])
                        P.op("dve", lambda e, d=d, bank=bank: e.scalar_tensor_tensor(out=xT[:, d, blk(qb)], in0=ps[bank][:], scalar=mcol(l, 16, d), in1=xT[:, d, blk(qb)], op0=ALU.mult, op1=ALU.add),
                             reads=[bps[bank], b_mods, bxT[qb]], writes=[bxT[qb]])

                def stageB(idx):
                    qb, m, kt = tiles[idx]
                    ntile = 4 * qb + 4
                    po, psm = (2, 3) if m == 0 else (4, 5)
                    sb = idx % 2
                    P.op("pe", lambda e: e.matmul(out=ps[po][:], lhsT=Vh[:, kt, :], rhs=PTt[sb], start=(kt == 0), stop=(kt == ntile - 1)), reads=[b_V, b_PT[sb]], writes=[bps[po]])
                    P.op("pe", lambda e: e.matmul(out=ps[psm][:], lhsT=onesb[:], rhs=PTt[sb], start=(kt == 0), stop=(kt == ntile - 1)), reads=[b_c, b_PT[sb]], writes=[bps[psm]])
                    if kt == ntile - 1:
                        seg1(qb, m)
                        if m == 1:
                            pending.append((idx + 3, lambda qb=qb: seg2(qb)))
                            pending.append((idx + 5, lambda qb=qb: seg3(qb)))

                for idx in range(NTL + 1):
                    if idx < NTL:
                        stageA(idx)
                    if idx >= 1:
                        stageB(idx - 1)
                    while pending and pending[0][0] <= idx:
                        pending.pop(0)[1]()
                while pending:
                    pending.pop(0)[1]()
            pg = PT["post_g"] + (l * 2 + 0) * 8
            pb = PT["post_b"] + (l * 2 + 0) * 8
            claim("Rs", [b_sq[0], b_sq[1], b_stat])
            for tb in range(NBLK):
                layer_norm_block(lambda c, tb=tb: xT[:, c, blk(tb)], bxT[tb], lambda c, tb=tb: xT[:, c, blk(tb)], bxT[tb], pg, pb, AF.Identity, 0, 1)

        b_XS = [Buf(f"XS{i}") for i in range(8)]
        b_YS = [Buf("YS0"), Buf("YS1")]

        def moe_sparse(l):
            hT = R_h[:].bitcast(BF16).rearrange("p (c t) -> p c t", c=8)
            b_hT = [Buf(f"hT{b}") for b in range(NBLK)]
            b_GT = Buf("GT")
            b_M, b_cnt = Buf("Mall"), Buf("cnt")
            b_posl = [Buf(f"posI{i}") for i in range(16)]
            b_gatesl = [Buf(f"gates{i}") for i in range(16)]
            claim("Rh", b_hT)
            claim("Rm", [b_GT])
            claim("Rs", [b_sq[0], b_sq[1], b_stat])
            for tb in range(NBLK):
                for c in range(8):
                    P.op("act", lambda e, c=c, tb=tb: e.activation(out=hT[:, c, blk(tb)], in_=xT[:, c, blk(tb)], func=AF.Identity, scale=mcol(l, 32, c), bias=mcol(l, 24, c)),
                         reads=[bxT[tb], b_mods], writes=[b_hT[tb]])
                for c in range(8):
                    P.op("dve", lambda e, c=c, tb=tb: e.tensor_scalar_mul(out=xT[:, c, blk(tb)], in0=xT[:, c, blk(tb)], scalar1=ALPHA), reads=[bxT[tb]], writes=[bxT[tb]])
            wr = wr_s[:].rearrange("p (l k e) -> p l k e", l=2, k=8)
            Mv = Mall[:].rearrange("p (i e) -> p i e", i=16)
            b_lg2 = [Buf("lg0"), Buf("lg1")]
            b_rt2 = [Buf("rt0"), Buf("rt1")]

            def tile_gen(i):
                st_ = i % 2
                b_lg, b_rt = b_lg2[st_], b_rt2[st_]
                lgv = lg[:, st_ * 256:(st_ + 1) * 256]
                rtv = rt[:, st_ * 400:(st_ + 1) * 400]
                mxv = mx8[:, st_ * 16:(st_ + 1) * 16]
                bl, bg6, br5 = (7, 6, 5) if st_ == 0 else (3, 2, 1)
                L = lgv[:, 0:NE]
                M = lgv[:, NE:2 * NE]
                E = lgv[:, 2 * NE:3 * NE]
                G = lgv[:, 3 * NE:4 * NE]
                OH = rtv[:, 0:128].rearrange("p (k e) -> p k e", k=4)
                TA = rtv[:, 128:256].rearrange("p (k e) -> p k e", k=4)
                TB = rtv[:, 256:384].rearrange("p (k e) -> p k e", k=4)
                eid4 = rtv[:, 384:388]
                rank4 = rtv[:, 388:392]
                pos4 = rtv[:, 392:396]
                e4 = rtv[:, 396:400]
                tb = i // 4
                tsl = slice(i * 128, (i + 1) * 128)
                for k in range(8):
                    P.op("pe", lambda e, k=k: e.matmul(out=ps[bl][:, 0:NE], lhsT=hT[:, k, tsl], rhs=wr[:, l, k, :], start=(k == 0), stop=False), reads=[b_hT[tb], b_init], writes=[bps[bl]])
                P.op("pe", lambda e: e.matmul(out=ps[bl][:, 0:NE], lhsT=onesb[0:1, :], rhs=br_s[0:1, l * NE:(l + 1) * NE], start=False, stop=True), reads=[b_c, b_init], writes=[bps[bl]])
                yield
                P.op("dve", lambda e: e.tensor_copy(out=L, in_=ps[bl][:, 0:NE]), reads=[bps[bl]], writes=[b_lg])
                yield
                P.op("dve", lambda e: e.max(out=mxv[:, 0:8], in_=L), reads=[b_lg], writes=[b_lg])
                yield
                P.op("dve", lambda e: e.tensor_copy(out=TA[:, 0, :], in_=L), reads=[b_lg], writes=[b_rt])
                yield
                Lw = TA[:, 0, :]
                EQ = TA[:, 1, :]
                TT = TA[:, 2, :]
                for k in range(4):
                    P.op("dve", lambda e, k=k: e.tensor_scalar(out=EQ, in0=Lw, scalar1=mxv[:, k:k + 1], scalar2=None, op0=ALU.is_equal), reads=[b_rt, b_lg], writes=[b_rt])
                    yield
                    P.op("dve", lambda e: e.scalar_tensor_tensor(out=TT, in0=EQ, scalar=-1000.0, in1=iotaP[:], op0=ALU.mult, op1=ALU.add), reads=[b_rt, b_c], writes=[b_rt])
                    yield
                    P.op("dve", lambda e, k=k: e.tensor_reduce(out=eid4[:, k:k + 1], in_=TT, axis=AX.X, op=ALU.min), reads=[b_rt], writes=[b_rt])
                    yield
                    P.op("dve", lambda e, k=k: e.tensor_scalar(out=OH[:, k, :], in0=iotaT[:], scalar1=eid4[:, k:k + 1], scalar2=None, op0=ALU.is_equal), reads=[b_rt, b_init], writes=[b_rt])
                    yield
                    if k < 3:
                        P.op("dve", lambda e, k=k: e.scalar_tensor_tensor(out=Lw, in0=OH[:, k, :], scalar=-1.0e30, in1=Lw, op0=ALU.mult, op1=ALU.add), reads=[b_rt], writes=[b_rt])
                        yield
                P.op("dve", lambda e: e.reduce_sum(out=M, in_=OH.rearrange("p k e -> p e k"), axis=AX.X), reads=[b_rt], writes=[b_lg])
                yield
                P.op("act", lambda e: e.activation(out=Mv[:, i, :], in_=M, func=AF.Copy), reads=[b_lg], writes=[b_M])
                P.op("dve", lambda e: e.tensor_scalar_mul(out=mxv[:, 8:9], in0=mxv[:, 0:1], scalar1=-1.0), reads=[b_lg], writes=[b_lg])
                yield
                P.op("act", lambda e: e.activation(out=E, in_=L, func=AF.Exp, bias=mxv[:, 8:9], scale=1.0), reads=[b_lg], writes=[b_lg])
                P.op("act", lambda e: e.activation(out=e4, in_=mxv[:, 0:4], func=AF.Exp, bias=mxv[:, 8:9], scale=1.0), reads=[b_lg], writes=[b_rt])
                yield
                P.op("dve", lambda e: e.tensor_tensor(out=E, in0=E, in1=M, op=ALU.mult), reads=[b_lg], writes=[b_lg])
                yield
                P.op("dve", lambda e: e.reduce_sum(out=mxv[:, 9:10], in_=E, axis=AX.X), reads=[b_lg], writes=[b_lg])
                yield
                P.op("dve", lambda e: e.reciprocal(out=mxv[:, 10:11], in_=mxv[:, 9:10]), reads=[b_lg], writes=[b_lg])
                yield
                P.op("dve", lambda e: e.tensor_scalar(out=G, in0=E, scalar1=mxv[:, 10:11], scalar2=None, op0=ALU.mult), reads=[b_lg], writes=[b_lg])
                yield
                P.op("dve", lambda e: e.tensor_scalar(out=gates4[:, 4 * i:4 * i + 4], in0=e4, scalar1=mxv[:, 10:11], scalar2=None, op0=ALU.mult), reads=[b_lg, b_rt], writes=[b_gatesl[i]])
                P.op("pe", lambda e: e.transpose(out=ps[bg6][0:NE, 0:128], in_=G, identity=ident[:]), reads=[b_lg, b_init], writes=[bps[bg6]])
                yield
                P.op("act", lambda e: e.activation(out=GTb[:, tsl], in_=ps[bg6][0:NE, 0:128], func=AF.Copy), reads=[bps[bg6]], writes=[b_GT])
                for i2 in range(i + 1):
                    lhs = trib if i2 == i else onesb
                    P.op("pe", lambda e, i2=i2, lhs=lhs: e.matmul(out=ps[br5][:, 0:NE], lhsT=lhs[:], rhs=Mv[:, i2, :], start=(i2 == 0), stop=(i2 == i)), reads=[b_M, b_c, b_init], writes=[bps[br5]])
                yield
                P.op("dve", lambda e: e.tensor_tensor(out=TB, in0=OH, in1=ps[br5][:, 0:NE].unsqueeze(1).to_broadcast([128, 4, NE]), op=ALU.mult), reads=[b_rt, bps[br5]], writes=[b_rt])
                yield
                P.op("dve", lambda e: e.reduce_sum(out=rank4, in_=TB, axis=AX.X), reads=[b_rt], writes=[b_rt])
                yield
                P.op("dve", lambda e: e.scalar_tensor_tensor(out=pos4, in0=eid4, scalar=float(S), in1=rank4, op0=ALU.mult, op1=ALU.add), reads=[b_rt], writes=[b_rt])
                yield
                P.op("dve", lambda e: e.tensor_copy(out=posI[:, 4 * i:4 * i + 4], in_=pos4), reads=[b_rt], writes=[b_posl[i]])
                yield

            for p_ in range(8):
                g0, g1 = tile_gen(2 * p_), tile_gen(2 * p_ + 1)
                done0 = done1 = False
                while not (done0 and done1):
                    if not done0:
                        try:
                            next(g0)
                        except StopIteration:
                            done0 = True
                    if not done1:
                        try:
                            next(g1)
                        except StopIteration:
                            done1 = True
            for i in range(16):
                P.op("pe", lambda e, i=i: e.matmul(out=ps[4][0:1, 0:NE], lhsT=onesb[:, 0:1], rhs=Mv[:, i, :], start=(i == 0), stop=(i == 15)), reads=[b_M, b_c], writes=[bps[4]])
            P.op("dve", lambda e: e.tensor_copy(out=cntF[:], in_=ps[4][0:1, 0:NE]), reads=[bps[4]], writes=[b_cnt])
            P.op("dve", lambda e: e.tensor_copy(out=cntI[:], in_=cntF[:]), reads=[b_cnt], writes=[b_cnt])
            ring = [R_w[:, 0:4096], R_w[:, 4096:8192], R_w[:, 8192:12288], R_h[:, 0:4096], R_h[:, 4096:8192]]
            b_ring = [Buf(f"ring{q}") for q in range(5)]
            claim("Rw", b_ring[0:3])
            ring_claimed_h = [False]

            def issue_piece(e_, p):
                q = (3 * e_ + p) % 5
                if q >= 3 and not ring_claimed_h[0]:
                    claim("Rh", b_ring[3:5])
                    ring_claimed_h[0] = True
                wv = ring[q].bitcast(BF16).rearrange("p (k n) -> p k n", k=8)
                if p == 0:
                    src = wgu_d[l, e_, :, 0:D]
                elif p == 1:
                    src = wgu_d[l, e_, :, D:2 * D]
                else:
                    src = wdn_d[l, e_, :, :]
                P.dma("pool", wv, src.rearrange("(k p) n -> p k n", p=128), f"rg{q}", writes=[b_ring[q]])
                return q, wv

            pieces = {}
            pieces[(0, 0)] = issue_piece(0, 0)
            pieces[(0, 1)] = issue_piece(0, 1)
            pieces[(0, 2)] = issue_piece(0, 2)
            for tb in range(NBLK):
                for d in range(8):
                    bank = 2 + d % 2
                    P.op("pe", lambda e, d=d, tb=tb, bank=bank: e.matmul(out=ps[bank][:], lhsT=bdn_s[:, l * D + d * 128:l * D + (d + 1) * 128], rhs=GTb[:, blk(tb)], start=True, stop=True), reads=[b_init, b_GT], writes=[bps[bank]])
                    P.op("dve", lambda e, d=d, tb=tb, bank=bank: e.scalar_tensor_tensor(out=xT[:, d, blk(tb)], in0=ps[bank][:], scalar=mcol(l, 40, d), in1=xT[:, d, blk(tb)], op0=ALU.mult, op1=ALU.add),
                         reads=[bps[bank], b_mods, bxT[tb]], writes=[bxT[tb]])
            htok = [R_a[:, 0:512].bitcast(BF16), R_a[:, 512:1024].bitcast(BF16)]
            b_htok = [Buf("htok0"), Buf("htok1")]
            actT = [R_a[:, 1024:1536].bitcast(BF16).rearrange("p (f s) -> p f s", f=8), R_a[:, 1536:2048].bitcast(BF16).rearrange("p (f s) -> p f s", f=8)]
            b_actT = [Buf("actT0"), Buf("actT1")]
            claim("Ra", b_htok + b_actT)
            for i in range(16):
                sl = i % 2
                bank = sl
                pv = ps[bank][:].bitcast(BF16)
                for c in range(8):
                    P.op("pe", lambda e, c=c, i=i, pv=pv: e.transpose(out=pv[:, c * 128:(c + 1) * 128], in_=hT[:, c, i * 128:(i + 1) * 128], identity=identb[:]), reads=[b_hT[i // 4], b_c], writes=[bps[bank]])
                P.op("act", lambda e, sl=sl, pv=pv: e.activation(out=htok[sl], in_=pv, func=AF.Copy), reads=[bps[bank]], writes=[b_htok[sl]])
                for k in range(4):
                    col = 4 * i + k
                    P.dma("pool", None, None, f"xsc{sl}", reads=[b_htok[sl], b_posl[i]], writes=[b_XS[sl * 4 + k]],
                          fn=lambda e, sl=sl, col=col: e.indirect_dma_start(out=xs_d[:, :], out_offset=bass.IndirectOffsetOnAxis(ap=posI[:, col:col + 1], axis=0), in_=htok[sl], in_offset=None))
            xsb = R_s[:, 0:512].bitcast(BF16)
            xsT = [R_s[:, 512:1024].bitcast(BF16).rearrange("p (k s) -> p k s", k=8), R_s[:, 1024:1536].bitcast(BF16).rearrange("p (k s) -> p k s", k=8)]
            act_tt = [R_s[:, 1536:2048].bitcast(BF16), R_s[:, 2048:2560].bitcast(BF16)]
            b_xsb, b_xsT, b_acttt = Buf("xsb"), [Buf("xsT0"), Buf("xsT1")], [Buf("act_t0"), Buf("act_t1")]
            ys = [R_m[:, 0:1024], R_m[:, 1024:2048]]
            b_ys = [Buf("ys0"), Buf("ys1")]
            bgu_row = [R_m[0:1, 2048:3072].bitcast(BF16), R_m[0:1, 3072:4096].bitcast(BF16)]
            b_bgu = [Buf("bgu0"), Buf("bgu1")]
            gc, sg_, ln_, t1 = R_m[:, 4096:4608], R_m[:, 4608:5120], R_m[:, 5120:5632], R_m[:, 5632:6144]
            b_gc, b_sg2, b_ln, b_t1 = Buf("gc"), Buf("sg2"), Buf("ln"), Buf("t1")

            def issue_bias(e_):
                P.dma("pool", bgu_row[e_ % 2], bgu_d[l, e_:e_ + 1, :], f"bgu{e_ % 2}", writes=[b_bgu[e_ % 2]])

            claim("Rs", [b_xsb, b_xsT[0], b_xsT[1]] + b_acttt)
            claim("Rm", b_ys + b_bgu + [b_gc, b_sg2, b_ln, b_t1])
            issue_bias(0)
            bcnt = [0]
            for e_ in range(NE):
                if e_ + 1 < NE:
                    pieces[(e_ + 1, 0)] = issue_piece(e_ + 1, 0)
                    pieces[(e_ + 1, 1)] = issue_piece(e_ + 1, 1)
                    issue_bias(e_ + 1)
                (qg, wg), (ql, wl), (qd, wd) = pieces[(e_, 0)], pieces[(e_, 1)], pieces[(e_, 2)]
                brow = bgu_row[e_ % 2]
                for eng in ("pe", "act", "dve", "sp"):
                    P.regload(eng, cntI[0:1, e_:e_ + 1], reads=[b_cnt])
                def stage_a(j, e_=e_, wg=wg, wl=wl, brow=brow, qg=qg, ql=ql):
                    pr = ((l, e_), j * 128)
                    s_ = j % 2
                    act_t, b_actt = act_tt[s_], b_acttt[s_]
                    r0 = e_ * S + j * 128
                    P.dma("sp", xsb, xs_d[r0:r0 + 128, :], "xsl", reads=b_XS, writes=[b_xsb], pred=pr)
                    pv = ps[0][:].bitcast(BF16)
                    for k in range(8):
                        P.op("pe", lambda e, k=k, pv=pv: e.transpose(out=pv[:, k * 128:(k + 1) * 128], in_=xsb[:, k * 128:(k + 1) * 128], identity=identb[:]), reads=[b_xsb, b_c], writes=[bps[0]], pred=pr)
                    P.op("dve", lambda e, s_=s_, pv=pv: e.tensor_copy(out=xsT[s_], in_=pv.rearrange("p (k s) -> p k s", k=8)), reads=[bps[0]], writes=[b_xsT[s_]], pred=pr)
                    for hf in range(2):
                        bg_, bl_ = 2 + 2 * hf, 3 + 2 * hf
                        fs = slice(hf * 512, (hf + 1) * 512)
                        for k in range(8):
                            P.op("pe", lambda e, k=k, s_=s_, bg_=bg_, fs=fs: e.matmul(out=ps[bg_][:], lhsT=xsT[s_][:, k, :], rhs=wg[:, k, fs], start=(k == 0), stop=False), reads=[b_ring[qg], b_xsT[s_]], writes=[bps[bg_]], pred=pr)
                        P.op("pe", lambda e, bg_=bg_, hf=hf: e.matmul(out=ps[bg_][:], lhsT=onesb[0:1, :], rhs=brow[0:1, hf * 512:(hf + 1) * 512], start=False, stop=True), reads=[b_bgu[e_ % 2], b_c], writes=[bps[bg_]], pred=pr)
                        for k in range(8):
                            P.op("pe", lambda e, k=k, s_=s_, bl_=bl_, fs=fs: e.matmul(out=ps[bl_][:], lhsT=xsT[s_][:, k, :], rhs=wl[:, k, fs], start=(k == 0), stop=False), reads=[b_ring[ql], b_xsT[s_]], writes=[bps[bl_]], pred=pr)
                        P.op("pe", lambda e, bl_=bl_, hf=hf: e.matmul(out=ps[bl_][:], lhsT=onesb[0:1, :], rhs=brow[0:1, D + hf * 512:D + (hf + 1) * 512], start=False, stop=True), reads=[b_bgu[e_ % 2], b_c], writes=[bps[bl_]], pred=pr)
                        P.op("dve", lambda e, bg_=bg_: e.tensor_scalar_min(out=gc, in0=ps[bg_][:], scalar1=7.0), reads=[bps[bg_]], writes=[b_gc], pred=pr)
                        P.op("act", lambda e: e.activation(out=sg_, in_=gc, func=AF.Sigmoid, scale=1.702), reads=[b_gc], writes=[b_sg2], pred=pr)
                        P.op("dve", lambda e, bl_=bl_: e.tensor_scalar(out=ln_, in0=ps[bl_][:], scalar1=1.0, scalar2=8.0, op0=ALU.add, op1=ALU.min), reads=[bps[bl_]], writes=[b_ln], pred=pr)
                        P.op("dve", lambda e: e.tensor_tensor(out=t1, in0=gc, in1=sg_, op=ALU.mult), reads=[b_gc, b_sg2], writes=[b_t1], pred=pr)
                        P.op("dve", lambda e, fs=fs, act_t=act_t: e.scalar_tensor_tensor(out=act_t[:, fs], in0=ln_, scalar=-6.0, in1=t1, op0=ALU.max, op1=ALU.mult), reads=[b_ln, b_t1], writes=[b_actt], pred=pr)

                def stage_b(j, e_=e_, wd=wd, qd=qd):
                    pr = ((l, e_), j * 128)
                    s_ = j % 2
                    act_t, b_actt = act_tt[s_], b_acttt[s_]
                    r0 = e_ * S + j * 128
                    pv1 = ps[1][:].bitcast(BF16)
                    for f in range(8):
                        P.op("pe", lambda e, f=f, pv1=pv1, act_t=act_t: e.transpose(out=pv1[:, f * 128:(f + 1) * 128], in_=act_t[:, f * 128:(f + 1) * 128], identity=identb[:]), reads=[b_actt, b_c], writes=[bps[1]], pred=pr)
                    P.op("dve", lambda e, s_=s_, pv1=pv1: e.tensor_copy(out=actT[s_], in_=pv1.rearrange("p (f s) -> p f s", f=8)), reads=[bps[1]], writes=[b_actT[s_]], pred=pr)
                    for n in range(2):
                        for f in range(8):
                            P.op("pe", lambda e, f=f, n=n, s_=s_: e.matmul(out=ps[6 + n][:], lhsT=actT[s_][:, f, :], rhs=wd[:, f, n * 512:(n + 1) * 512], start=(f == 0), stop=(f == 7)), reads=[b_ring[qd], b_actT[s_]], writes=[bps[6 + n]], pred=pr)
                        P.op("dve", lambda e, n=n, s_=s_: e.tensor_copy(out=ys[s_][:, n * 512:(n + 1) * 512], in_=ps[6 + n][:]), reads=[bps[6 + n]], writes=[b_ys[s_]], pred=pr)
                    P.dma("act", ys_d[r0:r0 + 128, :], ys[s_], "yst", reads=[b_ys[s_]], writes=[b_YS[s_]], pred=pr)

                stage_a(0)
                for j in range(16):
                    if j + 1 < 16:
                        stage_a(j + 1)
                    stage_b(j)
                if e_ + 1 < NE:
                    pieces[(e_ + 1, 2)] = issue_piece(e_ + 1, 2)
            yb = [[R_w[:, (s2 * 4 + k) * 1024:(s2 * 4 + k + 1) * 1024] for k in range(4)] for s2 in range(2)]
            acc = [R_w[:, 8192:9216], R_w[:, 9216:10240]]
            b_yb = [[Buf(f"yb{s2}{k}") for k in range(4)] for s2 in range(2)]
            b_acc = [Buf("acc0"), Buf("acc1")]
            claim("Rw", b_yb[0] + b_yb[1] + b_acc)
            for i in range(16):
                s2 = i % 2
                for k in range(4):
                    col = 4 * i + k
                    P.dma("pool", None, None, f"yg{s2}", reads=b_YS + [b_posl[i]], writes=[b_yb[s2][k]],
                          fn=lambda e, s2=s2, k=k, col=col: e.indirect_dma_start(out=yb[s2][k], out_offset=None, in_=ys_d[:, :], in_offset=bass.IndirectOffsetOnAxis(ap=posI[:, col:col + 1], axis=0)))
                P.op("dve", lambda e, s2=s2, i=i: e.tensor_scalar(out=acc[s2], in0=yb[s2][0], scalar1=gates4[:, 4 * i:4 * i + 1], scalar2=None, op0=ALU.mult), reads=[b_yb[s2][0], b_gatesl[i]], writes=[b_acc[s2]])
                for k in range(1, 4):
                    P.op("dve", lambda e, s2=s2, i=i, k=k: e.scalar_tensor_tensor(out=acc[s2], in0=yb[s2][k], scalar=gates4[:, 4 * i + k:4 * i + k + 1], in1=acc[s2], op0=ALU.mult, op1=ALU.add), reads=[b_yb[s2][k], b_gatesl[i], b_acc[s2]], writes=[b_acc[s2]])
                for g in range(2):
                    bank = 2 * s2 + g
                    for cc in range(4):
                        d = g * 4 + cc
                        P.op("pe", lambda e, d=d, cc=cc, s2=s2, bank=bank: e.transpose(out=ps[bank][:, cc * 128:(cc + 1) * 128], in_=acc[s2][:, d * 128:(d + 1) * 128], identity=ident[:]), reads=[b_acc[s2], b_init], writes=[bps[bank]])
                    for cc in range(4):
                        d = g * 4 + cc
                        P.op("dve", lambda e, d=d, cc=cc, i=i, bank=bank: e.scalar_tensor_tensor(out=xT[:, d, i * 128:(i + 1) * 128], in0=ps[bank][:, cc * 128:(cc + 1) * 128], scalar=mcol(l, 40, d), in1=xT[:, d, i * 128:(i + 1) * 128], op0=ALU.mult, op1=ALU.add),
                             reads=[bps[bank], b_mods, bxT[i // 4]], writes=[bxT[i // 4]])
            pg = PT["post_g"] + (l * 2 + 1) * 8
            pb = PT["post_b"] + (l * 2 + 1) * 8
            claim("Rs", [b_sq[0], b_sq[1], b_stat])
            for tb in range(NBLK):
                layer_norm_block(lambda c, tb=tb: xT[:, c, blk(tb)], bxT[tb], lambda c, tb=tb: xT[:, c, blk(tb)], bxT[tb], pg, pb, AF.Identity, 4, 5)

        moe = moe_sparse if SPARSE else moe_sublayer
        phases = [("conv", conv_sublayer), ("moe0", lambda: moe(0)), ("attn", attn_sublayer), ("moe1", lambda: moe(1))]
        for name, fn in phases:
            fn()
            if stop_after == name:
                break

        b_stage = [Buf("ostage0"), Buf("ostage1")]
        claim("Ra", b_stage)
        for i in range(16):
            sl = i % 2
            sv = stage[:, sl * D:(sl + 1) * D]
            for g in range(2):
                bank = 2 + g
                for cc in range(4):
                    c = g * 4 + cc
                    P.op("pe", lambda e, c=c, cc=cc, i=i, bank=bank: e.transpose(out=ps[bank][:, cc * 128:(cc + 1) * 128], in_=xT[:, c, i * 128:(i + 1) * 128], identity=ident[:]),
                         reads=[bxT[i // 4], b_init], writes=[bps[bank]])
                if g == 0:
                    P.op("act", lambda e, sv=sv, bank=bank: e.activation(out=sv[:, 0:512], in_=ps[bank][:], func=AF.Copy), reads=[bps[bank]], writes=[b_stage[sl]])
                else:
                    P.op("dve", lambda e, sv=sv, bank=bank: e.tensor_copy(out=sv[:, 512:1024], in_=ps[bank][:]), reads=[bps[bank]], writes=[b_stage[sl]])
            P.dma("sp", out_d[i * 128:(i + 1) * 128, :], sv, "out", reads=[b_stage[sl]])
        P.final_wait("sp", ["out"])
        P.emit()
    return nc


_CACHE = {}


def _t5_bucket(rel):
    nb = 16
    ret = np.where(rel > 0, nb, 0)
    n = np.abs(rel)
    max_exact = 8
    nf = np.maximum(n, 1).astype(np.float32) / np.float32(max_exact)
    large = max_exact + (np.log(nf) / np.float32(math.log(128 / max_exact)) * np.float32(nb - max_exact)).astype(np.int32)
    large = np.minimum(large, nb - 1)
    return ret + np.where(n < max_exact, n, large)


def _attn_consts():
    rel = 511 - np.arange(RG)
    bk = _t5_bucket(rel)
    ohg = np.zeros((32, RG), np.float32)
    ohg[bk, np.arange(RG)] = 1.0
    ohg[15, :] -= 1.0
    masks = np.zeros((4, 128, BLK), np.float32)
    kl = np.arange(128)[:, None] // 64
    ql = np.arange(BLK)[None, :] // 64
    for o in range(4):
        masks[o] = np.where(kl - ql <= -(o * 128) // 64, 0.0, NEG)
    return ohg, masks


def make_in_maps(inp):
    f = lambda a: np.ascontiguousarray(np.asarray(a, np.float32))
    pt = build_pt(inp)
    shared = {
        "pt": pt,
        "ident": np.eye(128, dtype=np.float32),
        "ada_w": f(inp["ada_w"]),
        "w_pw1": f(inp["conv_w_pw1"][0]),
        "w_pw2": f(inp["conv_w_pw2"][0]),
        "w_r": f(inp["router_w"]),
        "b_r": f(inp["router_b"]).reshape(2, 1, NE),
        "w_gu": f(inp["expert_w_gate_up"]),
        "w_dn": f(inp["expert_w_down"]),
        "b_dn": f(inp["expert_b_down"]),
        "w_kv": f(inp["w_kv"]),
        "w_q": f(inp["attn_w_q"][0]),
        "w_o": f(inp["attn_w_o"][0]),
        "lpt": np.ascontiguousarray(f(inp["attn_lambda"][0]).T),
        "tabs": f(inp["rel_bias_table"]),
        "b_gu": f(inp["expert_b_gate_up"]),
        "tri": np.triu(np.ones((128, 128), np.float32), 1),
        "iota": np.tile(np.arange(NE, dtype=np.float32)[None, :], (128, 1)),
        "ohg": _attn_consts()[0],
        "masks": _attn_consts()[1],
    }
    maps = []
    for b in range(8):
        m = dict(shared)
        m["x"] = f(inp["x"][b])
        m["ct"] = np.ascontiguousarray(f(inp["c"][b]).reshape(8, 128).T)
        maps.append(m)
    return maps


def kernel(**inputs):
    if "nc" not in _CACHE:
        _CACHE["nc"] = build()
    nc = _CACHE["nc"]
    maps = make_in_maps(inputs)
    res = run_bass_kernel_spmd(nc, maps, core_ids=list(range(8)))
    return np.stack([np.asarray(r["out"], np.float32) for r in res.results], axis=0)
```
